# Optimizing a Trainium2 kernel written in Bass

```python
import math
import jax
import jax.numpy as jnp
from jax import lax
import numpy as np

D_MODEL = 1024
BATCH = 8
SEQ = 4096
DEPTH = 4

N_MIXERS = 2
N_MLA_LAYERS = (DEPTH + N_MIXERS - 1) // N_MIXERS
N_LRU_LAYERS = DEPTH // N_MIXERS

MLA_HEADS = 8
QK_NOPE_DIM = 128
QK_ROPE_DIM = 64
V_HEAD_DIM = 128
Q_LORA_RANK = 768
KV_LORA_RANK = 256
MLA_LATENT_DIM = Q_LORA_RANK + KV_LORA_RANK + QK_ROPE_DIM
ROPE_THETA = 10000.0
Q_BLOCK = 128

LRU_WIDTH = D_MODEL
LRU_BLOCKS = 4
LRU_BLOCK_DIM = LRU_WIDTH // LRU_BLOCKS
CONV_WIDTH = 4
LRU_C = 8.0

N_EXPERTS = 32
TOP_K = 4
D_FF = D_MODEL
SWIGLU_LIMIT = 7.0
SWIGLU_ALPHA = 1.702
MOE_BLOCK = 256

PLE_DIM = 256

DEEPNORM_ALPHA = (2.0 * DEPTH) ** 0.25
DEEPNORM_BETA = (8.0 * DEPTH) ** -0.25
LN_EPS = 1e-5
RMS_EPS = 1e-6

kernel_name = 'hybrid_mla_rglru_moe_deepnorm'


def _layer_norm(x, g, b):
    xf = x.astype(jnp.float32)
    mu = jnp.mean(xf, axis=-1, keepdims=True)
    xc = xf - mu
    var = jnp.mean(xc * xc, axis=-1, keepdims=True)
    return (xc * lax.rsqrt(var + LN_EPS) * g.astype(jnp.float32) + b.astype(jnp.float32)).astype(x.dtype)


def _rms_norm(x, g):
    xf = x.astype(jnp.float32)
    ms = jnp.mean(xf * xf, axis=-1, keepdims=True)
    return (xf * lax.rsqrt(ms + RMS_EPS) * g.astype(jnp.float32)).astype(x.dtype)


def _rope(x, positions):
    half = x.shape[-1] // 2
    inv_freq = jnp.exp(-math.log(ROPE_THETA) * jnp.arange(half, dtype=jnp.float32) / half)
    ang = positions.astype(jnp.float32)[..., None] * inv_freq
    cos = jnp.cos(ang)[:, :, None, :]
    sin = jnp.sin(ang)[:, :, None, :]
    xf = x.astype(jnp.float32)
    x1, x2 = xf[..., :half], xf[..., half:]
    return jnp.concatenate([x1 * cos - x2 * sin, x2 * cos + x1 * sin], axis=-1).astype(x.dtype)


def _mla(x, positions, w_in, q_norm, kv_norm, w_uq, w_ukv, w_o):
    bsz, seq, _ = x.shape
    lat = x @ w_in
    c_q = _rms_norm(lat[..., :Q_LORA_RANK], q_norm)
    c_kv = _rms_norm(lat[..., Q_LORA_RANK:Q_LORA_RANK + KV_LORA_RANK], kv_norm)
    k_rope = _rope(lat[..., Q_LORA_RANK + KV_LORA_RANK:][:, :, None, :], positions)[:, :, 0, :]
    q = (c_q @ w_uq).reshape(bsz, seq, MLA_HEADS, QK_NOPE_DIM + QK_ROPE_DIM)
    q_nope = q[..., :QK_NOPE_DIM]
    q_rope = _rope(q[..., QK_NOPE_DIM:], positions)
    kv = (c_kv @ w_ukv).reshape(bsz, seq, MLA_HEADS, QK_NOPE_DIM + V_HEAD_DIM)
    k_nope = kv[..., :QK_NOPE_DIM]
    v = kv[..., QK_NOPE_DIM:]

    n_blocks = seq // Q_BLOCK

    def to_blocks(t):
        return jnp.moveaxis(t.reshape(bsz, n_blocks, Q_BLOCK, *t.shape[2:]), 1, 0)

    scale = 1.0 / math.sqrt(QK_NOPE_DIM + QK_ROPE_DIM)
    key_pos = jnp.arange(seq, dtype=jnp.int32)

    def attend(args):
        qn, qr, blk = args
        s = (jnp.einsum('bqhd,bkhd->bhqk', qn, k_nope)
             + jnp.einsum('bqhr,bkr->bhqk', qr, k_rope)).astype(jnp.float32) * scale
        q_pos = blk * Q_BLOCK + jnp.arange(Q_BLOCK, dtype=jnp.int32)
        s = jnp.where(key_pos[None, :] <= q_pos[:, None], s, -jnp.inf)
        pr = jax.nn.softmax(s, axis=-1).astype(v.dtype)
        return jnp.einsum('bhqk,bkhd->bqhd', pr, v)

    o = lax.map(attend, (to_blocks(q_nope), to_blocks(q_rope), jnp.arange(n_blocks, dtype=jnp.int32)))
    o = jnp.moveaxis(o, 0, 1).reshape(bsz, seq, MLA_HEADS * V_HEAD_DIM)
    return o @ w_o


def _linear_combine(c1, c2):
    a1, b1 = c1
    a2, b2 = c2
    return a1 * a2, a2 * b1 + b2


def _rglru_block(x, w_in, conv_w, conv_b, w_a, b_a, w_x, b_x, lam, w_out):
    bsz, seq, _ = x.shape
    gu = x @ w_in
    gate, u = gu[..., :LRU_WIDTH], gu[..., LRU_WIDTH:]
    u = lax.conv_general_dilated(u, conv_w[:, None, :], window_strides=(1,),
                                 padding=[(CONV_WIDTH - 1, 0)],
                                 dimension_numbers=('NWC', 'WIO', 'NWC'),
                                 feature_group_count=LRU_WIDTH) + conv_b
    ub = u.reshape(bsz, seq, LRU_BLOCKS, LRU_BLOCK_DIM)
    r = jax.nn.sigmoid((jnp.einsum('bsnc,ncd->bsnd', ub, w_a).reshape(bsz, seq, LRU_WIDTH) + b_a).astype(jnp.float32))
    i = jax.nn.sigmoid((jnp.einsum('bsnc,ncd->bsnd', ub, w_x).reshape(bsz, seq, LRU_WIDTH) + b_x).astype(jnp.float32))
    log_a = -LRU_C * r * jax.nn.softplus(-lam.astype(jnp.float32))
    a = jnp.exp(log_a)
    mult = jnp.sqrt(-jnp.expm1(2.0 * log_a))
    b_in = mult * i * u.astype(jnp.float32)
    _, h = lax.associative_scan(_linear_combine, (a, b_in), axis=1)
    y = jax.nn.gelu(gate, approximate=True) * h.astype(x.dtype)
    return y @ w_out


def _moe(x, w_router, b_router, w_up, b_up, w_down, b_down):
    bsz, seq, d = x.shape
    t = bsz * seq
    xf = x.reshape(t, d)
    logits = (xf @ w_router + b_router).astype(jnp.float32)
    top_logits, top_idx = lax.top_k(logits, TOP_K)
    gates = jax.nn.softmax(top_logits, axis=-1)
    n = t * TOP_K
    flat_e = top_idx.reshape(n).astype(jnp.int32)
    flat_tok = jnp.arange(n, dtype=jnp.int32) // TOP_K
    flat_g = gates.reshape(n)
    order = jnp.argsort(flat_e)
    sorted_e = flat_e[order]
    counts = jnp.zeros((N_EXPERTS,), jnp.int32).at[flat_e].add(1)
    padded = (counts + MOE_BLOCK - 1) // MOE_BLOCK * MOE_BLOCK
    starts = jnp.cumsum(counts) - counts
    pad_ends = jnp.cumsum(padded)
    pad_starts = pad_ends - padded
    rank = jnp.arange(n, dtype=jnp.int32) - starts[sorted_e]
    dest = pad_starts[sorted_e] + rank
    n_pad = n + N_EXPERTS * MOE_BLOCK
    n_blocks = n_pad // MOE_BLOCK
    row_tok = jnp.zeros((n_pad,), jnp.int32).at[dest].set(flat_tok[order])
    row_gate = jnp.zeros((n_pad,), x.dtype).at[dest].set(flat_g[order].astype(x.dtype))
    block_e = jnp.searchsorted(pad_ends, jnp.arange(n_blocks, dtype=jnp.int32) * MOE_BLOCK, side='right')
    block_e = jnp.minimum(block_e, N_EXPERTS - 1).astype(jnp.int32)
    xs = xf[row_tok].reshape(n_blocks, MOE_BLOCK, d)

    def expert_block(args):
        xb, e = args
        hb = xb @ w_up[e] + b_up[e]
        g = jnp.minimum(hb[:, :D_FF], SWIGLU_LIMIT)
        up = jnp.clip(hb[:, D_FF:], -SWIGLU_LIMIT, SWIGLU_LIMIT)
        yb = (up + 1.0) * (g * jax.nn.sigmoid(SWIGLU_ALPHA * g))
        return yb @ w_down[e] + b_down[e]

    ys = lax.map(expert_block, (xs, block_e)).reshape(n_pad, d)
    out = jax.ops.segment_sum(ys * row_gate[:, None], row_tok, num_segments=t)
    return out.reshape(bsz, seq, d)


def setup_inputs(seed: int = 0) -> dict:
    key = jax.random.key(seed)
    ks = jax.random.split(key, 30)
    na, nl = N_MLA_LAYERS, N_LRU_LAYERS
    f32 = jnp.float32

    def nrm(k, shape, scale):
        return jax.random.normal(k, shape, f32) * scale

    x = nrm(ks[0], (BATCH, SEQ, D_MODEL), 1.0)
    p = nrm(ks[1], (DEPTH, BATCH, SEQ, PLE_DIM), 1.0)
    positions = (jax.random.randint(ks[2], (BATCH, 1), 0, 1024, dtype=jnp.int32)
                 + jnp.arange(SEQ, dtype=jnp.int32)[None, :])
    mla_w_in = nrm(ks[3], (na, D_MODEL, MLA_LATENT_DIM), D_MODEL ** -0.5)
    mla_q_norm = 1.0 + nrm(ks[4], (na, Q_LORA_RANK), 0.02)
    mla_kv_norm = 1.0 + nrm(ks[5], (na, KV_LORA_RANK), 0.02)
    mla_w_uq = nrm(ks[6], (na, Q_LORA_RANK, MLA_HEADS * (QK_NOPE_DIM + QK_ROPE_DIM)), Q_LORA_RANK ** -0.5)
    mla_w_ukv = nrm(ks[7], (na, KV_LORA_RANK, MLA_HEADS * (QK_NOPE_DIM + V_HEAD_DIM)), KV_LORA_RANK ** -0.5)
    mla_w_o = nrm(ks[8], (na, MLA_HEADS * V_HEAD_DIM, D_MODEL), DEEPNORM_BETA * (MLA_HEADS * V_HEAD_DIM) ** -0.5)
    lru_w_in = nrm(ks[9], (nl, D_MODEL, 2 * LRU_WIDTH), D_MODEL ** -0.5)
    lru_conv_w = nrm(ks[10], (nl, CONV_WIDTH, LRU_WIDTH), CONV_WIDTH ** -0.5)
    lru_conv_b = nrm(ks[11], (nl, LRU_WIDTH), 0.01)
    lru_w_a = nrm(ks[12], (nl, LRU_BLOCKS, LRU_BLOCK_DIM, LRU_BLOCK_DIM), LRU_BLOCK_DIM ** -0.5)
    lru_b_a = nrm(ks[13], (nl, LRU_WIDTH), 0.01)
    lru_w_x = nrm(ks[14], (nl, LRU_BLOCKS, LRU_BLOCK_DIM, LRU_BLOCK_DIM), LRU_BLOCK_DIM ** -0.5)
    lru_b_x = nrm(ks[15], (nl, LRU_WIDTH), 0.01)
    a_c = jax.random.uniform(ks[16], (nl, LRU_WIDTH), f32, 0.9, 0.999)
    sig = a_c ** (1.0 / LRU_C)
    lru_lambda = jnp.log(sig) - jnp.log1p(-sig)
    lru_w_out = nrm(ks[17], (nl, LRU_WIDTH, D_MODEL), DEEPNORM_BETA * LRU_WIDTH ** -0.5)
    ln1_g = 1.0 + nrm(ks[18], (DEPTH, D_MODEL), 0.02)
    ln1_b = nrm(ks[19], (DEPTH, D_MODEL), 0.02)
    ln2_g = 1.0 + nrm(ks[20], (DEPTH, D_MODEL), 0.02)
    ln2_b = nrm(ks[21], (DEPTH, D_MODEL), 0.02)
    moe_w_router = nrm(ks[22], (DEPTH, D_MODEL, N_EXPERTS), D_MODEL ** -0.5)
    moe_b_router = nrm(ks[23], (DEPTH, N_EXPERTS), 0.01)
    moe_w_up = nrm(ks[24], (DEPTH, N_EXPERTS, D_MODEL, 2 * D_FF), D_MODEL ** -0.5)
    moe_b_up = nrm(ks[25], (DEPTH, N_EXPERTS, 2 * D_FF), 0.01)
    moe_w_down = nrm(ks[26], (DEPTH, N_EXPERTS, D_FF, D_MODEL), DEEPNORM_BETA * D_FF ** -0.5)
    moe_b_down = nrm(ks[27], (DEPTH, N_EXPERTS, D_MODEL), 0.01)
    ple_w_gate = nrm(ks[28], (DEPTH, D_MODEL, D_MODEL), D_MODEL ** -0.5)
    ple_w_proj = nrm(ks[29], (DEPTH, PLE_DIM, D_MODEL), DEEPNORM_BETA * PLE_DIM ** -0.5)
    return {
        'x': x, 'p': p, 'positions': positions,
        'mla_w_in': mla_w_in, 'mla_q_norm': mla_q_norm, 'mla_kv_norm': mla_kv_norm,
        'mla_w_uq': mla_w_uq, 'mla_w_ukv': mla_w_ukv, 'mla_w_o': mla_w_o,
        'lru_w_in': lru_w_in, 'lru_conv_w': lru_conv_w, 'lru_conv_b': lru_conv_b,
        'lru_w_a': lru_w_a, 'lru_b_a': lru_b_a, 'lru_w_x': lru_w_x, 'lru_b_x': lru_b_x,
        'lru_lambda': lru_lambda, 'lru_w_out': lru_w_out,
        'ln1_g': ln1_g, 'ln1_b': ln1_b, 'ln2_g': ln2_g, 'ln2_b': ln2_b,
        'moe_w_router': moe_w_router, 'moe_b_router': moe_b_router,
        'moe_w_up': moe_w_up, 'moe_b_up': moe_b_up, 'moe_w_down': moe_w_down, 'moe_b_down': moe_b_down,
        'ple_w_gate': ple_w_gate, 'ple_w_proj': ple_w_proj,
    }


def reference(x, p, positions,
              mla_w_in, mla_q_norm, mla_kv_norm, mla_w_uq, mla_w_ukv, mla_w_o,
              lru_w_in, lru_conv_w, lru_conv_b, lru_w_a, lru_b_a, lru_w_x, lru_b_x,
              lru_lambda, lru_w_out,
              ln1_g, ln1_b, ln2_g, ln2_b,
              moe_w_router, moe_b_router, moe_w_up, moe_b_up, moe_w_down, moe_b_down,
              ple_w_gate, ple_w_proj):
    for layer in range(DEPTH):
        j = layer // N_MIXERS
        if layer % N_MIXERS == 0:
            mix = _mla(x, positions, mla_w_in[j], mla_q_norm[j], mla_kv_norm[j],
                       mla_w_uq[j], mla_w_ukv[j], mla_w_o[j])
        else:
            mix = _rglru_block(x, lru_w_in[j], lru_conv_w[j], lru_conv_b[j], lru_w_a[j], lru_b_a[j],
                               lru_w_x[j], lru_b_x[j], lru_lambda[j], lru_w_out[j])
        x = _layer_norm(DEEPNORM_ALPHA * x + mix, ln1_g[layer], ln1_b[layer])
        ffn = _moe(x, moe_w_router[layer], moe_b_router[layer], moe_w_up[layer], moe_b_up[layer],
                   moe_w_down[layer], moe_b_down[layer])
        x = _layer_norm(DEEPNORM_ALPHA * x + ffn, ln2_g[layer], ln2_b[layer])
        x = x + jax.nn.sigmoid(x @ ple_w_gate[layer]) * (p[layer] @ ple_w_proj[layer])
    return x
```

```python
from contextlib import ExitStack
import math
import os
STOP = int(os.environ.get('K_STOP', '0'))
import numpy as np
import ml_dtypes
import concourse.bass as bass
import concourse.mybir as mybir
from concourse.bass_utils import run_bass_kernel_spmd

F32 = mybir.dt.float32
BF16 = mybir.dt.bfloat16
I32 = mybir.dt.int32
AF = mybir.ActivationFunctionType
ALU = mybir.AluOpType
AX = mybir.AxisListType

D = 1024
T = 4096
DEPTH = 4
TT = 512
NT = T // TT
KC = D // 128
H = 8
QL, KVL, RD = 768, 256, 64
NE = 32
DFF = 1024
PLE = 256
ALPHA = (2.0 * DEPTH) ** 0.25
LN_EPS = 1e-5
RMS_EPS = 1e-6
ATT_SCALE = 1.0 / math.sqrt(192.0)
MASK_NEG = -30000.0
TWO_PI = 2.0 * math.pi
CAP = 768
NSLOT = NE * CAP
BIGV = 65536.0
U32 = mybir.dt.uint32


class Res:
    __slots__ = ("w", "rs", "name")

    def __init__(self, name=""):
        self.w = []
        self.rs = []
        self.name = name


class TL:
    def __init__(self, h, name=""):
        self.h = h
        self.res = Res(name)

    def __getitem__(self, idx):
        return self.h[idx]


N_DMA_SEMS = 12


class KB:
    ENGS = ("pe", "act", "dve", "pool", "sp")

    def __init__(self, nc):
        self.nc = nc
        self.es = ExitStack()
        self.q = {e: [] for e in self.ENGS}
        self.cnt = {e: 0 for e in self.ENGS}
        self.sem = {e: self.es.enter_context(nc.semaphore("s_" + e)) for e in self.ENGS}
        self.dsem, self.dval, self.dnext = {}, {}, {}
        for qn in ("sp", "pool", "act"):
            self.dsem[qn] = [self.es.enter_context(nc.semaphore(f"d_{qn}{i}")) for i in range(N_DMA_SEMS)]
            self.dval[qn] = [0] * N_DMA_SEMS
            self.dnext[qn] = 0
        self.waited = {e: {} for e in self.ENGS}
        self.semobj = {}
        self.n_ops = 0
        self.phase_stack = None

    def begin_phase(self):
        self.phase_stack = ExitStack()
        if getattr(self, "sb_base", None) is None:
            self.sb_base = (self.nc._sbuf_addr_for_side(None) + 63) // 64 * 64
        self.sb_ptr = self.sb_base

    def end_phase(self):
        self.barrier()
        self.phase_stack.close()
        self.phase_stack = None

    def _stack(self):
        return self.phase_stack if self.phase_stack is not None else self.es

    def _nm(self, name):
        self.uid = getattr(self, "uid", 0) + 1
        return f"{name}_{self.uid}"

    def sb(self, name, shape, dt):
        name = self._nm(name)
        if self.phase_stack is not None:
            nbytes = int(np.prod(shape[1:])) * (2 if dt == BF16 else 4)
            off = (self.sb_ptr + 63) // 64 * 64
            self.sb_ptr = off + nbytes
            assert self.sb_ptr <= 229344, f"SBUF phase arena overflow at {name}: {self.sb_ptr}"
            return TL(self.nc.alloc_sbuf_tensor_at(name, list(shape), dt, offset=off), name)
        return TL(self._stack().enter_context(self.nc.sbuf_tensor(name, list(shape), dt)), name)

    def ps(self, name, shape, dt=F32):
        name = self._nm(name)
        return TL(self._stack().enter_context(self.nc.psum_tensor(name, list(shape), dt)), name)

    def dram(self, name, shape, dt, kind="Internal"):
        return TL(self.nc.dram_tensor(name, list(shape), dt, kind=kind), name)

    @staticmethod
    def _r(x):
        return x.res if isinstance(x, TL) else x

    @staticmethod
    def _compact(lst):
        best = {}
        for (s, v) in lst:
            k = id(s)
            if k not in best or best[k][1] < v:
                best[k] = (s, v)
        return list(best.values())

    def _deps(self, eng, reads, writes, joins=()):
        need = {}
        own = self.sem["pe"] if eng == "pe" else None

        def add(tok):
            s, v = tok
            if s is own:
                return
            k = id(s)
            self.semobj[k] = s
            if need.get(k, 0) < v:
                need[k] = v

        for r in reads:
            for t in self._r(r).w:
                add(t)
        for w in writes:
            rr = self._r(w)
            for t in rr.w:
                add(t)
            for t in rr.rs:
                add(t)
        for w in joins:
            rr = self._r(w)
            for t in rr.rs:
                add(t)
        out = []
        wd = self.waited[eng]
        for k, v in need.items():
            if wd.get(k, 0) < v:
                wd[k] = v
                out.append((self.semobj[k], v))
        return out

    def _commit(self, tok, reads, writes, joins=()):
        for r in reads:
            rr = self._r(r)
            rr.rs.append(tok)
            if len(rr.rs) > 48:
                rr.rs = self._compact(rr.rs)
        for w in writes:
            rr = self._r(w)
            rr.w = [tok]
            rr.rs = []
        for w in joins:
            rr = self._r(w)
            rr.w.append(tok)
            if len(rr.w) > 48:
                rr.w = self._compact(rr.w)

    def op(self, eng, fn, reads=(), writes=(), joins=()):
        waits = self._deps(eng, reads, writes, joins)
        self.cnt[eng] += 1
        tok = (self.sem[eng], self.cnt[eng])
        self.q[eng].append((waits, fn, (self.sem[eng], 1)))
        self._commit(tok, reads, writes, joins)
        self.n_ops += 1
        return tok

    def dma(self, qn, out_ap, in_ap, reads=(), writes=(), joins=()):
        waits = self._deps(qn, reads, writes, joins)
        i = self.dnext[qn]
        self.dnext[qn] = (i + 1) % N_DMA_SEMS
        s = self.dsem[qn][i]
        prev = self.dval[qn][i]
        self.semobj[id(s)] = s
        if prev > 0:
            wd = self.waited[qn]
            if wd.get(id(s), 0) < prev:
                wd[id(s)] = prev
                waits.append((s, prev))
        self.dval[qn][i] = prev + 16
        tok = (s, prev + 16)

        def fn(e, out_ap=out_ap, in_ap=in_ap):
            return e.dma_start(out=out_ap, in_=in_ap)

        self.q[qn].append((waits, fn, (s, 16)))
        self._commit(tok, reads, writes, joins)
        self.n_ops += 1
        return tok

    def idma(self, out_ap, out_off, in_ap, in_off, bounds, reads=(), writes=(), joins=()):
        qn = "pool"
        waits = self._deps(qn, reads, writes, joins)
        i = self.dnext[qn]
        self.dnext[qn] = (i + 1) % N_DMA_SEMS
        s = self.dsem[qn][i]
        prev = self.dval[qn][i]
        self.semobj[id(s)] = s
        if prev > 0:
            wd = self.waited[qn]
            if wd.get(id(s), 0) < prev:
                wd[id(s)] = prev
                waits.append((s, prev))
        self.dval[qn][i] = prev + 16
        tok = (s, prev + 16)

        def fn(e):
            if getattr(self, "_breg", None) is None:
                self._breg = e.to_reg(bounds)
            return e.indirect_dma_start(out=out_ap, out_offset=out_off, in_=in_ap, in_offset=in_off,
                                        bounds_check=self._breg, oob_is_err=False)

        self.q[qn].append((waits, fn, (s, 16)))
        self._commit(tok, reads, writes, joins)
        self.n_ops += 1
        return tok

    def barrier(self):
        toks = [(self.sem[e], self.cnt[e]) for e in self.ENGS if self.cnt[e] > 0]
        for qn in self.dsem:
            for s, v in zip(self.dsem[qn], self.dval[qn]):
                if v > 0:
                    toks.append((s, v))
        for e in self.ENGS:
            waits = []
            wd = self.waited[e]
            for (s, v) in toks:
                if s is self.sem[e]:
                    continue
                if wd.get(id(s), 0) < v:
                    wd[id(s)] = v
                    waits.append((s, v))
            if waits:
                self.q[e].append((waits, None, None))

    def emit(self):
        q = self.q

        def run(e, lst):
            for waits, fn, inc in lst:
                for (s, v) in waits:
                    e.wait_ge(s, v)
                if fn is None:
                    continue
                if isinstance(fn, (list, tuple)):
                    ins = None
                    for f in fn:
                        ins = f(e)
                else:
                    ins = fn(e)
                ins.then_inc(inc[0], inc[1])

        with self.nc.Block() as block:
            @block.tensor
            def _(e):
                run(e, q["pe"])

            @block.scalar
            def _(e):
                run(e, q["act"])

            @block.vector
            def _(e):
                run(e, q["dve"])

            @block.gpsimd
            def _(e):
                run(e, q["pool"])

            @block.sync
            def _(e):
                run(e, q["sp"])

    def close(self):
        self.es.close()


def I(name, *a, **k):
    return lambda e: getattr(e, name)(*a, **k)


class Rot:
    def __init__(self, tiles):
        self.tiles = tiles
        self.i = 0

    def next(self):
        t = self.tiles[self.i]
        self.i = (self.i + 1) % len(self.tiles)
        return t


class Prog:
    def __init__(self, n_layers=DEPTH, debug=(), phases=None):
        self.n_layers = n_layers
        self.debug = set(debug)
        self.phases = phases
        nc = bass.Bass("TRN2", target_bir_lowering=False)
        self.nc = nc
        kb = self.kb = KB(nc)
        IN = "ExternalInput"
        NL = n_layers
        na = max(1, (NL + 1) // 2)
        nl = max(1, NL // 2)
        self.in_shapes = {
            "x": ([T, D], F32), "p": ([NL, T, PLE], F32), "positions": ([1, T], I32),
            "mla_w_in": ([na, D, QL + KVL + RD], F32), "mla_q_norm": ([na, QL], F32), "mla_kv_norm": ([na, KVL], F32),
            "mla_w_uq": ([na, QL, H * 192], F32), "mla_w_ukv": ([na, KVL, H * 256], F32), "mla_w_o": ([na, D, D], F32),
            "lru_w_in": ([nl, D, 2 * D], F32), "lru_conv_w": ([nl, 4, D], F32), "lru_conv_b": ([nl, D], F32),
            "lru_w_a": ([nl, 4, 256, 256], F32), "lru_b_a": ([nl, D], F32), "lru_w_x": ([nl, 4, 256, 256], F32),
            "lru_b_x": ([nl, D], F32), "lru_lambda": ([nl, D], F32), "lru_w_out": ([nl, D, D], F32),
            "ln1_g": ([NL, D], F32), "ln1_b": ([NL, D], F32), "ln2_g": ([NL, D], F32), "ln2_b": ([NL, D], F32),
            "moe_w_router": ([NL, D, NE], F32), "moe_b_router": ([NL, NE], F32),
            "moe_w_up": ([NL, NE, D, 2 * DFF], F32), "moe_b_up": ([NL, NE, 2 * DFF], F32),
            "moe_w_down": ([NL, NE, DFF, D], F32), "moe_b_down": ([NL, NE, D], F32),
            "ple_w_gate": ([NL, D, D], F32), "ple_w_proj": ([NL, PLE, D], F32),
            "c_ident_f": ([128, 128], F32), "c_ident_b": ([128, 128], BF16), "c_ones_f": ([128, 128], F32),
            "c_mask_b": ([128, 128], BF16), "c_rt_b": ([64, 128], BF16), "c_invf": ([64, 1], F32),
            "c_u_b": ([128, 128], BF16), "c_ones_b": ([128, 128], BF16), "c_ebase": ([128, NE], F32),
        }
        self.used_inputs = []

        class LazyD(dict):
            def __missing__(dself, name):
                shape, dt = self.in_shapes[name]
                t = kb.dram(name, shape, dt, kind=IN)
                dself[name] = t
                self.used_inputs.append(name)
                return t

        d = self.d = LazyD()

        def scr(name, shape, dt=F32):
            kind = "ExternalOutput" if name in self.debug else "Internal"
            d[name] = kb.dram(name, shape, dt, kind=kind)

        scr("xT", [D, T]); scr("x1T", [D, T]); scr("x1b", [D, T], BF16)
        scr("cs", [2, 64, T])
        scr("qn", [H, 128, T], BF16); scr("qr", [H, 64, T], BF16); scr("kn", [H, 128, T], BF16)
        scr("kr", [64, T], BF16); scr("v", [T, H * 128], BF16); scr("oT", [H, 128, T], BF16)
        scr("y", [T, D]); scr("xg", [NSLOT + 128, D], BF16); scr("ys", [NSLOT + 128, D], BF16)
        d["out"] = kb.dram("out", [T, D], F32, kind="ExternalOutput")
        self.ln_nl = NL

        self.ident_f = kb.sb("ident_f", [128, 128], F32)
        self.ident_b = kb.sb("ident_b", [128, 128], BF16)
        self.ones_f = kb.sb("ones_f", [128, 128], F32)
        self.mask_b = kb.sb("mask_b", [128, 128], BF16)
        self.rt_b = kb.sb("rt_b", [64, 128], BF16)
        self.u_b = kb.sb("u_b", [128, 128], BF16)
        self.ones_b = kb.sb("ones_b", [128, 128], BF16)
        self.ebase = kb.sb("ebase", [128, NE], F32)
        self.slots_all = kb.sb("slots_all", [128, T // 128, 4], I32)
        self.gk_all = kb.sb("gk_all", [128, T // 128, 4], F32)
        self.cnt = kb.sb("cnt", [128, NE], F32)
        for t_, n_ in ((self.ident_f, "c_ident_f"), (self.ident_b, "c_ident_b"), (self.ones_f, "c_ones_f"),
                       (self.mask_b, "c_mask_b"), (self.rt_b, "c_rt_b"), (self.u_b, "c_u_b"),
                       (self.ones_b, "c_ones_b"), (self.ebase, "c_ebase")):
            kb.dma("sp", t_[:], d[n_][:], reads=[d[n_]], writes=[t_])
        self.lncols = kb.sb("lncols", [128, 4 * 32], F32)
        self.eng_rr = 0

        self.build()
        if not d["out"].res.w:
            zt = kb.sb("zt", [128, D], F32)
            kb.op("dve", I("memset", zt[:], 0.0), reads=[], writes=[zt])
            kb.dma("sp", d["out"][0:128, :], zt[:], reads=[zt], joins=[d["out"]])
        outs = list(d["out"].res.w)
        for n in self.debug:
            outs += list(d[n].res.w)
        kb.q["sp"].append((outs, None, None))
        kb.emit()
        kb.close()

    def cp_eng(self):
        self.eng_rr ^= 1
        return "act" if self.eng_rr else "dve"

    def copy(self, eng, out_t, out_ap, in_t, in_ap):
        if eng == "act":
            self.kb.op("act", I("activation", out=out_ap, in_=in_ap, func=AF.Identity), reads=[in_t], joins=[out_t])
        else:
            if eng == "dve":
                self.kb.op(eng, I("tensor_scalar", out_ap, in_ap, 1.0, None, op0=ALU.mult), reads=[in_t], joins=[out_t])
            else:
                self.kb.op(eng, I("tensor_copy", out=out_ap, in_=in_ap), reads=[in_t], joins=[out_t])

    def mm(self, out_t, out_ap, pairs, reads):
        n = len(pairs)
        fns = []
        for i, (l, r) in enumerate(pairs):
            fns.append(I("matmul", out_ap, lhsT=l, rhs=r, start=(i == 0), stop=(i == n - 1)))
        self.kb.op("pe", fns, reads=reads, writes=[out_t])

    def load_cols(self, dst_t, dst_ap, src_t, src_ap, n, ps_t, tmp_t):
        kb = self.kb
        kb.dma("sp", tmp_t[0:n, :], src_ap, reads=[src_t], writes=[tmp_t])
        kb.op("pe", I("transpose", ps_t[:, 0:n], tmp_t[0:n, :], self.ident_f[0:n, 0:n]),
              reads=[tmp_t, self.ident_f], writes=[ps_t])
        self.copy("dve", dst_t, dst_ap, ps_t, ps_t[:, 0:n])

    def want(self, name):
        return self.phases is None or name in self.phases

    def build(self):
        if self.want("setup"):
            self.phase_setup()
        for l in range(self.n_layers):
            if l % 2 == 0:
                if self.want(f"proj{l}"):
                    self.phase_mla_proj(l)
                if self.want(f"attn{l}"):
                    self.phase_attn(l)
                if self.want(f"mix{l}"):
                    self.phase_mix_ln1(l, "mla")
            else:
                if self.want(f"mix{l}"):
                    self.phase_mix_ln1(l, "lru")
            if self.want(f"moe{l}"):
                self.phase_moe(l)
            if self.want(f"comb{l}"):
                self.phase_combine(l)
            if self.want(f"ple{l}"):
                self.phase_ln2_ple(l, last=(l == self.n_layers - 1))

    def phase_setup(self):
        kb, d = self.kb, self.d
        kb.begin_phase()
        ps = Rot([kb.ps(f"su_ps{i}", [128, 512]) for i in range(4)])
        tmp = kb.sb("su_tmp", [128, 128], F32)
        for v, nm in enumerate(("ln1_g", "ln1_b", "ln2_g", "ln2_b")):
            src = d[nm][:].rearrange("l (c p) -> (l c) p", p=128)
            nrow = self.ln_nl * 8
            self.load_cols(self.lncols, self.lncols[:, v * 32:v * 32 + nrow], d[nm], src, nrow, ps.next(), tmp)
        invf = kb.sb("su_invf", [64, 1], F32)
        kb.dma("sp", invf[:], d["c_invf"][:], reads=[d["c_invf"]], writes=[invf])
        pos_i = kb.sb("su_posi", [64, T], I32)
        kb.dma("sp", pos_i[:], d["positions"][0:1, :].partition_broadcast(64), reads=[d["positions"]], writes=[pos_i])
        ang = kb.sb("su_ang", [64, T], F32)
        kb.op("dve", I("tensor_copy", out=ang[:], in_=pos_i[:]), reads=[pos_i], writes=[ang])
        kb.op("dve", I("tensor_scalar", ang[:], ang[:], invf[:, 0:1], None, op0=ALU.mult), reads=[ang, invf], writes=[ang])
        kf_i = kb.sb("su_kfi", [64, T], I32)
        kf = kb.sb("su_kf", [64, T], F32)
        r = kb.sb("su_r", [64, T], F32)
        fx = kb.sb("su_fx", [64, T], F32)
        C1 = 6.28125
        C2 = TWO_PI - C1
        for which, shift in ((1, 0.0), (0, math.pi / 2)):
            kb.op("dve", I("tensor_scalar", kf[:], ang[:], shift, 1.0 / TWO_PI, op0=ALU.add, op1=ALU.mult), reads=[ang], writes=[kf])
            kb.op("dve", I("tensor_copy", out=kf_i[:], in_=kf[:]), reads=[kf], writes=[kf_i])
            kb.op("dve", I("tensor_copy", out=kf[:], in_=kf_i[:]), reads=[kf_i], writes=[kf])
            kb.op("dve", I("scalar_tensor_tensor", out=r[:], in0=kf[:], scalar=-C1, in1=ang[:], op0=ALU.mult, op1=ALU.add), reads=[kf, ang], writes=[r])
            kb.op("dve", I("scalar_tensor_tensor", out=r[:], in0=kf[:], scalar=-C2, in1=r[:], op0=ALU.mult, op1=ALU.add), reads=[kf, r], writes=[r])
            if shift != 0.0:
                kb.op("dve", I("tensor_scalar", r[:], r[:], shift, None, op0=ALU.add), reads=[r], writes=[r])
            kb.op("dve", I("tensor_scalar", fx[:], r[:], math.pi, -TWO_PI, op0=ALU.is_gt, op1=ALU.mult), reads=[r], writes=[fx])
            kb.op("dve", I("tensor_tensor", out=r[:], in0=r[:], in1=fx[:], op=ALU.add), reads=[r, fx], writes=[r])
            kb.op("dve", I("tensor_scalar", fx[:], r[:], -math.pi, TWO_PI, op0=ALU.is_lt, op1=ALU.mult), reads=[r], writes=[fx])
            kb.op("dve", I("tensor_tensor", out=r[:], in0=r[:], in1=fx[:], op=ALU.add), reads=[r, fx], writes=[r])
            kb.op("dve", I("tensor_scalar", r[:], r[:], math.pi, -math.pi, op0=ALU.min, op1=ALU.max), reads=[r], writes=[r])
            kb.op("act", I("activation", out=fx[:], in_=r[:], func=AF.Sin), reads=[r], writes=[fx])
            kb.dma("sp", d["cs"][which], fx[:], reads=[fx], joins=[d["cs"]])
        zt = kb.sb("su_zt", [128, 6, D], BF16)
        kb.op("dve", I("memset", zt[:], 0.0), reads=[], writes=[zt])
        for r0 in range(0, NSLOT + 128, 768):
            nr = min(768, NSLOT + 128 - r0)
            kb.dma("sp", d["xg"][r0:r0 + nr, :].rearrange("(b p) f -> p b f", p=128), zt[:, 0:nr // 128, :], reads=[zt], joins=[d["xg"]])
        xin = Rot([kb.sb(f"su_xin{i}", [128, 4, D], F32) for i in range(2)])
        xo = Rot([kb.sb(f"su_xo{i}", [128, KC, TT], F32) for i in range(2)])
        xTv = d["xT"][:].rearrange("(c p) t -> p c t", p=128)
        for j in range(NT):
            xi = xin.next()
            kb.dma("sp", xi[:], d["x"][j * TT:(j + 1) * TT, :].rearrange("(b p) f -> p b f", p=128), reads=[d["x"]], writes=[xi])
            xo_ = xo.next()
            for c in range(KC):
                pt = ps.next()
                fns = [I("transpose", pt[:, b * 128:(b + 1) * 128], xi[:, b, c * 128:(c + 1) * 128], self.ident_f[:]) for b in range(4)]
                kb.op("pe", fns, reads=[xi, self.ident_f], writes=[pt])
                self.copy(self.cp_eng(), xo_, xo_[:, c, :], pt, pt[:])
            kb.dma("sp", xTv[:, :, j * TT:(j + 1) * TT], xo_[:], reads=[xo_], joins=[d["xT"]])
        kb.end_phase()

    def rope(self, src_ps_t, src_ps_ap, cs_t, out_t, out_ap, ps_rot, tmp_f, tmp_b, tmp2, n=TT):
        kb = self.kb
        if STOP == 311:
            return
        kb.op("act", I("activation", out=tmp_f[0:64, :n], in_=src_ps_ap, func=AF.Identity), reads=[src_ps_t], writes=[tmp_f])
        if STOP == 312:
            return
        kb.op("act", I("activation", out=tmp_b[0:64, :n], in_=src_ps_ap, func=AF.Identity), reads=[src_ps_t], writes=[tmp_b])
        if STOP == 31:
            return
        rp = ps_rot.next()
        self.mm(rp, rp[:, :n], [(self.rt_b[:, :], tmp_b[0:64, :n])], reads=[self.rt_b, tmp_b])
        if STOP == 32:
            return
        kb.op("dve", I("tensor_tensor", out=tmp2[0:64, :n], in0=rp[0:64, :n], in1=cs_t[0:64, 1, :n], op=ALU.mult), reads=[rp, cs_t], writes=[tmp2])
        kb.op("dve", I("tensor_tensor", out=tmp_f[0:64, :n], in0=tmp_f[0:64, :n], in1=cs_t[0:64, 0, :n], op=ALU.mult), reads=[tmp_f, cs_t], writes=[tmp_f])
        kb.op("dve", I("tensor_tensor", out=out_ap, in0=tmp_f[0:64, :n], in1=tmp2[0:64, :n], op=ALU.add), reads=[tmp_f, tmp2], writes=[out_t])

    def phase_mla_proj(self, l):
        kb, d = self.kb, self.d
        j_ = l // 2
        kb.begin_phase()
        w_in = kb.sb("mp_win", [128, KC, QL + KVL + 128], BF16)
        w_uq = kb.sb("mp_wuq", [128, 6, H * 192 + 64], BF16)
        w_ukv = kb.sb("mp_wukv", [128, 2, H * 256], BF16)
        kb.op("dve", I("memset", w_in[:, :, 1088:1152], 0.0), reads=[], joins=[w_in])
        kb.op("dve", I("memset", w_uq[:, :, H * 192:H * 192 + 64], 0.0), reads=[], joins=[w_uq])
        kb.dma("pool", w_in[:, :, 0:1088], d["mla_w_in"][j_].rearrange("(c p) f -> p c f", p=128), reads=[d["mla_w_in"]], joins=[w_in])
        kb.dma("pool", w_uq[:, :, 0:H * 192], d["mla_w_uq"][j_].rearrange("(c p) f -> p c f", p=128), reads=[d["mla_w_uq"]], joins=[w_uq])
        kb.dma("pool", w_ukv[:], d["mla_w_ukv"][j_].rearrange("(c p) f -> p c f", p=128), reads=[d["mla_w_ukv"]], writes=[w_ukv])
        ps = Rot([kb.ps(f"mp_ps{i}", [128, 512]) for i in range(6)])
        tmp = kb.sb("mp_tmp", [128, 128], F32)
        ncol = kb.sb("mp_ncol", [128, 8], F32)
        self.load_cols(ncol, ncol[:, 0:6], d["mla_q_norm"], d["mla_q_norm"][j_].rearrange("(c p) -> c p", p=128), 6, ps.next(), tmp)
        self.load_cols(ncol, ncol[:, 6:8], d["mla_kv_norm"], d["mla_kv_norm"][j_].rearrange("(c p) -> c p", p=128), 2, ps.next(), tmp)

        if STOP == 1:
            kb.end_phase(); return
        xa = Rot([kb.sb(f"mp_xa{i}", [128, KC, TT], F32) for i in range(1)])
        xb = Rot([kb.sb(f"mp_xb{i}", [128, KC, TT], BF16) for i in range(2)])
        lat = kb.sb("mp_lat", [128, 8, TT], F32)
        sq = kb.sb("mp_sq", [128, 8, TT], F32)
        rstd = [kb.sb(f"mp_rstd{i}", [128, TT], F32) for i in range(2)]
        cqn = kb.sb("mp_cqn", [128, 8, TT], BF16)
        cs = Rot([kb.sb(f"mp_cs{i}", [64, 2, TT], F32) for i in range(2)])
        tf = kb.sb("mp_tf", [64, TT], F32)
        tb = kb.sb("mp_tb", [64, TT], BF16)
        t2 = kb.sb("mp_t2", [64, TT], F32)
        kr_o = Rot([kb.sb(f"mp_kro{i}", [64, TT], BF16) for i in range(2)])
        qn_o = Rot([kb.sb(f"mp_qno{i}", [128, H, TT], BF16) for i in range(1)])
        qr_o = Rot([kb.sb(f"mp_qro{i}", [64, H, TT], BF16) for i in range(1)])
        kn_o = Rot([kb.sb(f"mp_kno{i}", [128, H, TT], BF16) for i in range(1)])
        v_o = Rot([kb.sb(f"mp_vo{i}", [128, 4, H * 128], BF16) for i in range(1)])
        xTv = d["xT"][:].rearrange("(c p) t -> p c t", p=128)
        wv = w_ukv[:].rearrange("p c (h two e) -> p c h two e", two=2, e=128)
        for j in range(NT):
            ts = slice(j * TT, (j + 1) * TT)
            xa_ = xa.next(); xb_ = xb.next()
            kb.dma("sp", xa_[:], xTv[:, :, ts], reads=[d["xT"]], writes=[xa_])
            for c in range(KC):
                self.copy(self.cp_eng(), xb_, xb_[:, c, :], xa_, xa_[:, c, :])
            cs_ = cs.next()
            kb.dma("sp", cs_[:], d["cs"][:, :, ts].rearrange("a p t -> p a t"), reads=[d["cs"]], writes=[cs_])
            for oc in range(8):
                pt = ps.next()
                self.mm(pt, pt[:], [(w_in[:, kc, oc * 128:(oc + 1) * 128], xb_[:, kc, :]) for kc in range(KC)], reads=[w_in, xb_])
                kb.op("act", I("activation", out=lat[:, oc, :], in_=pt[:], func=AF.Identity), reads=[pt], joins=[lat])
                kb.op("act", I("activation", out=sq[:, oc, :], in_=pt[:], func=AF.Square), reads=[pt], joins=[sq])
            if STOP == 2:
                break
            ptk = ps.next()
            self.mm(ptk, ptk[:, :], [(w_in[:, kc, 1024:1152], xb_[:, kc, :]) for kc in range(KC)], reads=[w_in, xb_])
            kr_ = kr_o.next()
            self.rope(ptk, ptk[0:64, :], cs_, kr_, kr_[:, :], ps, tf, tb, t2)
            if STOP in (31, 32, 33, 311, 312):
                break
            kb.dma("sp", d["kr"][:, ts], kr_[:], reads=[kr_], joins=[d["kr"]])
            if STOP == 3:
                break
            for which, (c0, c1, dim) in enumerate(((0, 6, QL), (6, 8, KVL))):
                pt = ps.next()
                self.mm(pt, pt[:], [(self.ones_f[:], sq[:, c, :]) for c in range(c0, c1)], reads=[self.ones_f, sq])
                rs_ = rstd[which]
                kb.op("act", I("activation", out=rs_[:], in_=pt[:], func=AF.Sqrt, scale=1.0 / dim, bias=RMS_EPS), reads=[pt], writes=[rs_])
                kb.op("dve", I("reciprocal", out=rs_[:], in_=rs_[:]), reads=[rs_], writes=[rs_])
                for c in range(c0, c1):
                    kb.op("dve", I("scalar_tensor_tensor", out=cqn[:, c, :], in0=lat[:, c, :], scalar=ncol[:, c:c + 1], in1=rs_[:], op0=ALU.mult, op1=ALU.mult),
                          reads=[lat, ncol, rs_], joins=[cqn])
            if STOP == 4:
                break
            qn_ = qn_o.next(); qr_ = qr_o.next()
            for h in range(H):
                pt = ps.next()
                self.mm(pt, pt[:], [(w_uq[:, kc, h * 192:h * 192 + 128], cqn[:, kc, :]) for kc in range(6)], reads=[w_uq, cqn])
                self.copy(self.cp_eng(), qn_, qn_[:, h, :], pt, pt[:])
                pt2 = ps.next()
                self.mm(pt2, pt2[:, :], [(w_uq[:, kc, h * 192 + 128:h * 192 + 256], cqn[:, kc, :]) for kc in range(6)], reads=[w_uq, cqn])
                self.rope(pt2, pt2[0:64, :], cs_, qr_, qr_[:, h, :], ps, tf, tb, t2)
            kb.dma("sp", d["qn"][:, :, ts].rearrange("h p t -> p h t"), qn_[:], reads=[qn_], joins=[d["qn"]])
            kb.dma("sp", d["qr"][:, :, ts].rearrange("h p t -> p h t"), qr_[:], reads=[qr_], joins=[d["qr"]])
            if STOP == 5:
                break
            kn_ = kn_o.next()
            for h in range(H):
                pt = ps.next()
                self.mm(pt, pt[:], [(w_ukv[:, kc, h * 256:h * 256 + 128], cqn[:, 6 + kc, :]) for kc in range(2)], reads=[w_ukv, cqn])
                self.copy(self.cp_eng(), kn_, kn_[:, h, :], pt, pt[:])
            kb.dma("sp", d["kn"][:, :, ts].rearrange("h p t -> p h t"), kn_[:], reads=[kn_], joins=[d["kn"]])
            if STOP == 6:
                break
            v_ = v_o.next()
            for b in range(4):
                for hf in range(2):
                    pt = ps.next()
                    o_ap = pt[:].rearrange("p (a e) -> p a e", a=4)
                    self.mm(pt, o_ap, [(cqn[:, 6 + kc, b * 128:(b + 1) * 128], wv[:, kc, hf * 4:(hf + 1) * 4, 1, :]) for kc in range(2)], reads=[w_ukv, cqn])
                    self.copy(self.cp_eng(), v_, v_[:, b, hf * 512:(hf + 1) * 512], pt, pt[:])
            kb.dma("sp", d["v"][ts, :].rearrange("(b p) f -> p b f", p=128), v_[:], reads=[v_], joins=[d["v"]])
        kb.end_phase()

    def phase_attn(self, l):
        kb, d = self.kb, self.d
        kb.begin_phase()
        NQ = T // 128
        NST = 3
        kr = kb.sb("at_kr", [64, T], BF16)
        kb.dma("sp", kr[:], d["kr"][:], reads=[d["kr"]], writes=[kr])
        hd = [dict(kn=kb.sb(f"at_kn{i}", [128, T], BF16), qn=kb.sb(f"at_qn{i}", [128, T], BF16), qr=kb.sb(f"at_qr{i}", [64, T], BF16),
                   v=kb.sb(f"at_v{i}", [128, NQ, 128], BF16), oT=kb.sb(f"at_oT{i}", [128, T], BF16)) for i in range(2)]
        S = [kb.sb(f"at_S{i}", [128, T], F32) for i in range(NST)]
        P = [kb.sb(f"at_P{i}", [128, T], BF16) for i in range(NST)]
        PT = [kb.sb(f"at_PT{i}", [128, NQ, 128], BF16) for i in range(NST)]
        small = [kb.sb(f"at_sm{i}", [128, 4], F32) for i in range(NST)]
        bm = [kb.sb(f"at_bm{i}", [128, 8], F32) for i in range(4)]
        osb = Rot([kb.sb(f"at_osb{i}", [128, 128], BF16) for i in range(3)])
        sc = Rot([kb.ps(f"at_sc{i}", [128, 512]) for i in range(3)])
        ptp = Rot([kb.ps(f"at_pt{i}", [128, 1024], BF16) for i in range(2)])
        ops = Rot([kb.ps(f"at_o{i}", [128, 512]) for i in range(2)])
        otp = kb.ps("at_oTp", [128, 1024], BF16)
        vview = d["v"][:].rearrange("(c p) (h e) -> h p c e", p=128, e=128)

        def load_head(h):
            t_ = hd[h % 2]
            kb.dma("sp", t_["kn"][:], d["kn"][h], reads=[d["kn"]], writes=[t_["kn"]])
            kb.dma("sp", t_["qn"][:], d["qn"][h], reads=[d["qn"]], writes=[t_["qn"]])
            kb.dma("sp", t_["qr"][:], d["qr"][h], reads=[d["qr"]], writes=[t_["qr"]])
            for q4 in range(4):
                kb.dma("sp", t_["v"][:, q4 * 8:(q4 + 1) * 8, :], vview[h][:, q4 * 8:(q4 + 1) * 8, :], reads=[d["v"]], joins=[t_["v"]])

        items = [(h, qi) for h in range(H) for qi in range(NQ)]

        def stage_a(i):
            h, qi = items[i]
            if qi == 0 and h == 0:
                load_head(0)
            if qi == 3 and h + 1 < H:
                load_head(h + 1)
            t_ = hd[h % 2]
            qn_, kn_, qr_ = t_["qn"], t_["kn"], t_["qr"]
            S_ = S[i % NST]
            bm_ = bm[i % 4]
            qs = slice(qi * 128, (qi + 1) * 128)
            nk = (qi + 1) * 128
            nblk = (nk + 511) // 512
            for b in range(nblk):
                k0 = b * 512
                kw = min(512, nk - k0)
                pt = sc.next()
                last = (b == nblk - 1)
                fns = [I("matmul", pt[:, :kw], lhsT=qn_[:, qs], rhs=kn_[:, k0:k0 + kw], start=True, stop=False),
                       I("matmul", pt[:, :kw], lhsT=qr_[:, qs], rhs=kr[:, k0:k0 + kw], start=False, stop=(not last))]
                if last:
                    fns.append(I("matmul", pt[:, kw - 128:kw], lhsT=self.ident_b[:], rhs=self.mask_b[:], start=False, stop=True))
                kb.op("pe", fns, reads=[qn_, kn_, qr_, kr, self.ident_b, self.mask_b], writes=[pt])
                self.copy("act", S_, S_[:, k0:k0 + kw], pt, pt[:, :kw])
                kb.op("dve", I("reduce_max", out=bm_[:, b:b + 1], in_=S_[:, k0:k0 + kw], axis=AX.X), reads=[S_], joins=[bm_])

        def stage_b(i):
            h, qi = items[i]
            nk = (qi + 1) * 128
            S_, P_, sm = S[i % NST], P[i % NST], small[i % NST]
            nblk = (nk + 511) // 512
            kb.op("dve", I("reduce_max", out=sm[:, 0:1], in_=bm[i % 4][:, 0:nblk], axis=AX.X), reads=[bm[i % 4]], writes=[sm])
            kb.op("dve", I("tensor_scalar", sm[:, 1:2], sm[:, 0:1], -ATT_SCALE, None, op0=ALU.mult), reads=[sm], writes=[sm])
            kb.op("act", I("activation", out=P_[:, :nk], in_=S_[:, :nk], func=AF.Exp, scale=ATT_SCALE, bias=sm[:, 1:2], accum_out=sm[:, 2:3]),
                  reads=[S_, sm], writes=[P_, sm])

        opsum = {}

        def stage_t(i):
            h, qi = items[i]
            nk = (qi + 1) * 128
            P_, PT_ = P[i % NST], PT[i % NST]
            nc_ = nk // 128
            for g0 in range(0, nc_, 8):
                g1 = min(nc_, g0 + 8)
                tp = ptp.next()
                fns = [I("transpose", tp[:, (c - g0) * 128:(c - g0 + 1) * 128], P_[:, c * 128:(c + 1) * 128], self.ident_b[:]) for c in range(g0, g1)]
                kb.op("pe", fns, reads=[P_, self.ident_b], writes=[tp])
                self.copy("dve", PT_, PT_[:, g0:g1, :], tp, tp[:, 0:(g1 - g0) * 128].rearrange("p (c e) -> p c e", e=128))

        def stage_pv(i):
            h, qi = items[i]
            v_ = hd[h % 2]["v"]
            PT_ = PT[i % NST]
            nc_ = qi + 1
            op_ = ops.next()
            opsum[i] = op_
            fns = [I("matmul", op_[:, 0:128], lhsT=PT_[:, c, :], rhs=v_[:, c, :], start=(c == 0), stop=(c == nc_ - 1)) for c in range(nc_)]
            kb.op("pe", fns, reads=[PT_, v_], writes=[op_])

        def stage_o(i):
            h, qi = items[i]
            oT_ = hd[h % 2]["oT"]
            sm = small[i % NST]
            op_ = opsum.pop(i)
            kb.op("dve", I("reciprocal", out=sm[:, 3:4], in_=sm[:, 2:3]), reads=[sm], writes=[sm])
            os_ = osb.next()
            kb.op("dve", I("tensor_scalar", os_[:], op_[:, 0:128], sm[:, 3:4], None, op0=ALU.mult), reads=[op_, sm], writes=[os_])
            g = qi % 8
            kb.op("pe", I("transpose", otp[:, g * 128:(g + 1) * 128], os_[:], self.ident_b[:]), reads=[os_, self.ident_b], joins=[otp])
            if g == 7:
                q0 = (qi - 7) * 128
                self.copy("act", oT_, oT_[:, q0:q0 + 1024], otp, otp[:])
            if qi == NQ - 1:
                kb.dma("sp", d["oT"][h], oT_[:], reads=[oT_], joins=[d["oT"]])

        n = len(items)
        for step in range(n + 4):
            if 0 <= step - 2 < n:
                stage_t(step - 2)
            if step < n:
                stage_a(step)
            if 0 <= step - 2 < n:
                stage_pv(step - 2)
            if 0 <= step - 3 < n:
                stage_o(step - 3)
            if 0 <= step - 1 < n:
                stage_b(step - 1)
        kb.end_phase()

    def layernorm(self, pre, sq, ps, mean, rstd, tmp, gcol, bcol, out_f, out_b):
        kb = self.kb
        for c in range(KC):
            kb.op("act", I("activation", out=sq[:, c, :], in_=pre[:, c, :], func=AF.Square), reads=[pre], joins=[sq])
        p1 = ps.next()
        self.mm(p1, p1[:], [(self.ones_f[:], pre[:, c, :]) for c in range(KC)], reads=[self.ones_f, pre])
        p2 = ps.next()
        self.mm(p2, p2[:], [(self.ones_f[:], sq[:, c, :]) for c in range(KC)], reads=[self.ones_f, sq])
        kb.op("act", I("activation", out=mean[:], in_=p1[:], func=AF.Identity, scale=1.0 / D), reads=[p1], writes=[mean])
        kb.op("dve", I("tensor_tensor", out=tmp[:], in0=mean[:], in1=mean[:], op=ALU.mult), reads=[mean], writes=[tmp])
        kb.op("dve", I("scalar_tensor_tensor", out=rstd[:], in0=p2[:], scalar=1.0 / D, in1=tmp[:], op0=ALU.mult, op1=ALU.subtract), reads=[p2, tmp], writes=[rstd])
        kb.op("act", I("activation", out=rstd[:], in_=rstd[:], func=AF.Sqrt, bias=LN_EPS), reads=[rstd], writes=[rstd])
        kb.op("dve", I("reciprocal", out=rstd[:], in_=rstd[:]), reads=[rstd], writes=[rstd])
        for c in range(KC):
            kb.op("dve", I("tensor_tensor", out=sq[:, c, :], in0=pre[:, c, :], in1=mean[:], op=ALU.subtract), reads=[pre, mean], joins=[sq])
            kb.op("pool", I("tensor_tensor", out=sq[:, c, :], in0=sq[:, c, :], in1=rstd[:], op=ALU.mult), reads=[sq, rstd], joins=[sq])
            kb.op("act", I("activation", out=out_f[:, c, :], in_=sq[:, c, :], func=AF.Identity, scale=gcol[:, c:c + 1], bias=bcol[:, c:c + 1]),
                  reads=[sq, self.lncols], joins=[out_f])
            if out_b is not None:
                kb.op("pool", I("tensor_copy", out=out_b[:, c, :], in_=out_f[:, c, :]), reads=[out_f], joins=[out_b])

    def phase_mix_ln1(self, l, kind):
        kb, d = self.kb, self.d
        j_ = l // 2
        kb.begin_phase()
        ps = Rot([kb.ps(f"ml_ps{i}", [128, 512]) for i in range(7)])
        tmp = kb.sb("ml_tmp", [128, 128], F32)
        w_o = kb.sb("ml_wo", [128, KC, D], BF16)
        wsrc = d["mla_w_o"] if kind == "mla" else d["lru_w_out"]
        kb.dma("pool", w_o[:], wsrc[j_].rearrange("(c p) f -> p c f", p=128), reads=[wsrc], writes=[w_o])
        wr = kb.sb("ml_wr", [128, KC, NE], F32)
        kb.dma("sp", wr[:], d["moe_w_router"][l].rearrange("(c p) e -> p c e", p=128), reads=[d["moe_w_router"]], writes=[wr])
        br = kb.sb("ml_br", [1, NE], F32)
        kb.dma("sp", br[:], d["moe_b_router"][l:l + 1, :], reads=[d["moe_b_router"]], writes=[br])
        xa = Rot([kb.sb(f"ml_xa{i}", [128, KC, TT], F32) for i in range(1 if kind == "lru" else 2)])
        yin = Rot([kb.sb(f"ml_yin{i}", [128, KC, TT], BF16) for i in range(1 if kind == "lru" else 2)])
        pres = Rot([kb.sb(f"ml_pre{i}", [128, KC, TT], F32) for i in range(1 if kind == "lru" else 2)])
        sq = kb.sb("ml_sq", [128, KC, TT], F32)
        mean = kb.sb("ml_mean", [128, TT], F32)
        rstd = kb.sb("ml_rstd", [128, TT], F32)
        tmp2 = kb.sb("ml_tmp2", [128, TT], F32)
        x1f = Rot([kb.sb(f"ml_x1f{i}", [128, KC, TT], F32) for i in range(1 if kind == "lru" else 2)])
        x1b = Rot([kb.sb(f"ml_x1b{i}", [128, KC, TT], BF16) for i in range(1 if kind == "lru" else 2)])
        lg = kb.sb("ml_lg", [128, NE], F32)
        ee = kb.sb("ml_ee", [128, NE], F32)
        msk = kb.sb("ml_msk", [128, NE], F32)
        top8 = kb.sb("ml_top8", [128, 8], F32)
        sm = kb.sb("ml_sm", [128, 4], F32)
        mskb = kb.sb("ml_mskb", [128, NE], BF16)
        gts = kb.sb("ml_gts", [128, NE], F32)
        pos = kb.sb("ml_pos", [128, NE], F32)
        okm = kb.sb("ml_okm", [128, NE], F32)
        val = kb.sb("ml_val", [128, NE], F32)
        junk = kb.sb("ml_junk", [128, NE], F32)
        sl4 = kb.sb("ml_sl4", [128, 4], F32)
        x1tok = Rot([kb.sb(f"ml_xtok{i}", [128, 4, D], BF16) for i in range(1 if kind == "lru" else 2)])
        ptb = Rot([kb.ps(f"ml_ptb{i}", [128, 1024], BF16) for i in range(1)])
        kb.op("dve", I("memset", self.cnt[:], 0.0), reads=[], writes=[self.cnt])
        g1 = self.lncols[:, 0 * 32 + l * 8:0 * 32 + l * 8 + 8]
        b1 = self.lncols[:, 1 * 32 + l * 8:1 * 32 + l * 8 + 8]
        xTv = d["xT"][:].rearrange("(c p) t -> p c t", p=128)
        x1Tv = d["x1T"][:].rearrange("(c p) t -> p c t", p=128)
        x1bv = d["x1b"][:].rearrange("(c p) t -> p c t", p=128)
        if kind == "lru":
            L = self.lru_setup(l, ps, tmp)
        for j in range(NT):
            ts = slice(j * TT, (j + 1) * TT)
            xa_ = xa.next()
            kb.dma("sp", xa_[:], xTv[:, :, ts], reads=[d["xT"]], writes=[xa_])
            yin_ = yin.next()
            if kind == "mla":
                kb.dma("sp", yin_[:], d["oT"][:, :, ts].rearrange("h p t -> p h t"), reads=[d["oT"]], writes=[yin_])
            else:
                self.lru_tile(L, j, xa_, yin_, ps)
            pre = pres.next()
            for mc in range(KC):
                pt = ps.next()
                self.mm(pt, pt[:], [(w_o[:, kc, mc * 128:(mc + 1) * 128], yin_[:, kc, :]) for kc in range(KC)], reads=[w_o, yin_])
                kb.op("dve", I("scalar_tensor_tensor", out=pre[:, mc, :], in0=xa_[:, mc, :], scalar=ALPHA, in1=pt[:], op0=ALU.mult, op1=ALU.add),
                      reads=[xa_, pt], joins=[pre])
            x1f_, x1b_ = x1f.next(), x1b.next()
            self.layernorm(pre, sq, ps, mean, rstd, tmp2, g1, b1, x1f_, x1b_)
            kb.dma("sp", x1Tv[:, :, ts], x1f_[:], reads=[x1f_], joins=[d["x1T"]])
            xtok = x1tok.next()
            for b in range(4):
                tp = ptb.next()
                fns = [I("transpose", tp[:, kc * 128:(kc + 1) * 128], x1b_[:, kc, b * 128:(b + 1) * 128], self.ident_b[:]) for kc in range(KC)]
                kb.op("pe", fns, reads=[x1b_, self.ident_b], writes=[tp])
                self.copy(self.cp_eng(), xtok, xtok[:, b, :], tp, tp[:])
            for b in range(4):
                gb = j * 4 + b
                pt = ps.next()
                pairs = [(x1f_[:, kc, b * 128:(b + 1) * 128], wr[:, kc, :]) for kc in range(KC)]
                pairs.append((self.ones_f[0:1, 0:128], br[0:1, :]))
                self.mm(pt, pt[:, 0:NE], pairs, reads=[x1f_, wr, self.ones_f, br])
                self.copy("act", lg, lg[:], pt, pt[:, 0:NE])
                kb.op("dve", I("max", out=top8[:], in_=lg[:]), reads=[lg], writes=[top8])
                kb.op("dve", I("tensor_scalar", msk[:], lg[:], top8[:, 3:4], None, op0=ALU.is_ge), reads=[lg, top8], writes=[msk])
                kb.op("dve", I("tensor_scalar", mskb[:], lg[:], top8[:, 3:4], None, op0=ALU.is_ge), reads=[lg, top8], writes=[mskb])
                kb.op("dve", I("tensor_scalar", sm[:, 0:1], top8[:, 0:1], -1.0, None, op0=ALU.mult), reads=[top8], writes=[sm])
                kb.op("act", I("activation", out=ee[:], in_=lg[:], func=AF.Exp, bias=sm[:, 0:1]), reads=[lg, sm], writes=[ee])
                kb.op("dve", I("tensor_tensor", out=ee[:], in0=ee[:], in1=msk[:], op=ALU.mult), reads=[ee, msk], writes=[ee])
                kb.op("dve", I("reduce_sum", out=sm[:, 1:2], in_=ee[:], axis=AX.X), reads=[ee], writes=[sm])
                kb.op("dve", I("reciprocal", out=sm[:, 2:3], in_=sm[:, 1:2]), reads=[sm], writes=[sm])
                kb.op("dve", I("tensor_scalar", gts[:], ee[:], sm[:, 2:3], None, op0=ALU.mult), reads=[ee, sm], writes=[gts])
                pr = ps.next()
                self.mm(pr, pr[:, 0:NE], [(self.u_b[:], mskb[:])], reads=[self.u_b, mskb])
                pc = ps.next()
                self.mm(pc, pc[:, 0:NE], [(self.ones_b[:], mskb[:])], reads=[self.ones_b, mskb])
                kb.op("dve", I("tensor_tensor", out=pos[:], in0=pr[:, 0:NE], in1=self.cnt[:], op=ALU.add), reads=[pr, self.cnt], writes=[pos])
                kb.op("dve", I("tensor_tensor", out=self.cnt[:], in0=pc[:, 0:NE], in1=self.cnt[:], op=ALU.add), reads=[pc, self.cnt], writes=[self.cnt])
                kb.op("dve", I("tensor_scalar", okm[:], pos[:], float(CAP), None, op0=ALU.is_lt), reads=[pos], writes=[okm])
                kb.op("dve", I("tensor_tensor", out=okm[:], in0=okm[:], in1=msk[:], op=ALU.mult), reads=[okm, msk], writes=[okm])
                kb.op("dve", I("tensor_tensor", out=pos[:], in0=pos[:], in1=self.ebase[:], op=ALU.add), reads=[pos, self.ebase], writes=[pos])
                kb.op("dve", I("tensor_scalar", pos[:], pos[:], -1.0, BIGV, op0=ALU.mult, op1=ALU.add), reads=[pos], writes=[pos])
                kb.op("dve", I("tensor_tensor", out=val[:], in0=pos[:], in1=okm[:], op=ALU.mult), reads=[pos, okm], writes=[val])
                kb.op("dve", I("max", out=top8[:], in_=val[:]), reads=[val], writes=[top8])
                kb.op("dve", I("tensor_scalar", sl4[:], top8[:, 0:4], -1.0, BIGV, op0=ALU.mult, op1=ALU.add), reads=[top8], writes=[sl4])
                kb.op("dve", I("tensor_copy", out=self.slots_all[:, gb, :], in_=sl4[:]), reads=[sl4], joins=[self.slots_all])
                for k in range(4):
                    kb.op("dve", I("scalar_tensor_tensor", out=junk[:], in0=val[:], scalar=top8[:, k:k + 1], in1=gts[:], op0=ALU.is_equal, op1=ALU.mult,
                                   accum_out=self.gk_all[:, gb, k:k + 1]), reads=[val, top8, gts], writes=[junk], joins=[self.gk_all])
                for k in range(4):
                    kb.idma(d["xg"][:, :], bass.IndirectOffsetOnAxis(self.slots_all[:, gb, k:k + 1], 0), xtok[:, b, :], None, NSLOT - 1,
                            reads=[xtok, self.slots_all], joins=[d["xg"]])
        kb.end_phase()

    def lru_setup(self, l, ps, tmp):
        kb, d = self.kb, self.d
        j_ = l // 2
        L = {}
        L["w_in"] = kb.sb("lr_win", [128, KC, 2 * D], BF16)
        kb.dma("pool", L["w_in"][:], d["lru_w_in"][j_].rearrange("(c p) f -> p c f", p=128), reads=[d["lru_w_in"]], writes=[L["w_in"]])
        for nm, src in (("w_a", "lru_w_a"), ("w_x", "lru_w_x")):
            L[nm] = kb.sb("lr_" + nm, [128, 4, 2, 256], BF16)
            kb.dma("pool", L[nm][:], d[src][j_].rearrange("n (c p) f -> p n c f", p=128), reads=[d[src]], writes=[L[nm]])
        cols = L["cols"] = kb.sb("lr_cols", [128, 80], F32)
        self.load_cols(cols, cols[:, 0:32], d["lru_conv_w"], d["lru_conv_w"][j_].rearrange("k (c p) -> (k c) p", p=128), 32, ps.next(), tmp)
        for i, nm in enumerate(("lru_conv_b", "lru_b_a", "lru_b_x", "lru_lambda")):
            self.load_cols(cols, cols[:, 32 + 8 * i:40 + 8 * i], d[nm], d[nm][j_].rearrange("(c p) -> c p", p=128), 8, ps.next(), tmp)
        kb.op("act", I("activation", out=cols[:, 64:72], in_=cols[:, 56:64], func=AF.Exp, scale=-1.0), reads=[cols], writes=[cols])
        kb.op("act", I("activation", out=cols[:, 64:72], in_=cols[:, 64:72], func=AF.Ln, bias=1.0), reads=[cols], writes=[cols])
        kb.op("dve", I("tensor_scalar", cols[:, 64:72], cols[:, 64:72], -8.0, None, op0=ALU.mult), reads=[cols], writes=[cols])
        kb.op("dve", I("tensor_scalar", cols[:, 72:80], cols[:, 64:72], 2.0, None, op0=ALU.mult), reads=[cols], writes=[cols])
        L["halo"] = kb.sb("lr_halo", [128, KC, 4], F32)
        kb.op("dve", I("memset", L["halo"][:], 0.0), reads=[], writes=[L["halo"]])
        L["carry"] = kb.sb("lr_carry", [128, KC], F32)
        kb.op("dve", I("memset", L["carry"][:], 0.0), reads=[], writes=[L["carry"]])
        L["u"] = Rot([kb.sb(f"lr_u{i}", [128, 3 + TT], F32) for i in range(2)])
        L["gate"] = Rot([kb.sb(f"lr_gate{i}", [128, TT], BF16) for i in range(4)])
        L["uc"] = Rot([kb.sb(f"lr_uc{i}", [128, TT], F32) for i in range(4)])
        L["ucb"] = Rot([kb.sb(f"lr_ucb{i}", [128, TT], BF16) for i in range(4)])
        L["a"] = Rot([kb.sb(f"lr_a{i}", [128, TT], F32) for i in range(2)])
        L["a2"] = Rot([kb.sb(f"lr_a2{i}", [128, TT], F32) for i in range(2)])
        L["i"] = Rot([kb.sb(f"lr_i{i}", [128, TT], F32) for i in range(2)])
        L["r"] = Rot([kb.sb(f"lr_r{i}", [128, TT], F32) for i in range(2)])
        L["h"] = Rot([kb.sb(f"lr_h{i}", [128, TT], F32) for i in range(2)])
        L["xb"] = kb.sb("lr_xb", [128, KC, TT], BF16)
        return L

    def lru_tile(self, L, j, xa_, y_out, ps):
        kb = self.kb
        cols, halo, carry, w_in, xb = L["cols"], L["halo"], L["carry"], L["w_in"], L["xb"]
        for c in range(KC):
            self.copy(self.cp_eng(), xb, xb[:, c, :], xa_, xa_[:, c, :])
        chunks = {}

        def s1(n):
            chunk = chunks[n] = {}
            for oc in (2 * n, 2 * n + 1):
                pt = ps.next()
                self.mm(pt, pt[:], [(w_in[:, kc, oc * 128:(oc + 1) * 128], xb[:, kc, :]) for kc in range(KC)], reads=[w_in, xb])
                gate = L["gate"].next()
                kb.op("act", I("activation", out=gate[:], in_=pt[:], func=AF.Gelu_apprx_tanh), reads=[pt], writes=[gate])
                pt2 = ps.next()
                self.mm(pt2, pt2[:], [(w_in[:, kc, D + oc * 128:D + (oc + 1) * 128], xb[:, kc, :]) for kc in range(KC)], reads=[w_in, xb])
                u = L["u"].next()
                kb.op("dve", I("tensor_copy", out=u[:, 0:3], in_=halo[:, oc, 0:3]), reads=[halo], writes=[u])
                kb.op("act", I("activation", out=u[:, 3:3 + TT], in_=pt2[:], func=AF.Identity), reads=[pt2], joins=[u])
                kb.op("dve", I("tensor_copy", out=halo[:, oc, 0:3], in_=u[:, TT:TT + 3]), reads=[u], writes=[halo])
                uc = L["uc"].next()
                ucb = L["ucb"].next()
                kb.op("dve", I("tensor_scalar", uc[:], u[:, 0:TT], cols[:, oc:oc + 1], cols[:, 32 + oc:33 + oc], op0=ALU.mult, op1=ALU.add),
                      reads=[u, cols], writes=[uc])
                for k in range(1, 4):
                    kb.op("dve", I("scalar_tensor_tensor", out=uc[:], in0=u[:, k:k + TT], scalar=cols[:, k * 8 + oc:k * 8 + oc + 1], in1=uc[:], op0=ALU.mult, op1=ALU.add),
                          reads=[u, cols, uc], writes=[uc])
                kb.op("pool", I("tensor_copy", out=ucb[:], in_=uc[:]), reads=[uc], writes=[ucb])
                chunk[oc] = (gate, uc, ucb)

        def s2(n):
            chunk = chunks.pop(n)
            for oc in (2 * n, 2 * n + 1):
                co = (oc % 2) * 128
                a, a2, ii, rr = L["a"].next(), L["a2"].next(), L["i"].next(), L["r"].next()
                gate, uc, _ = chunk[oc]
                ucbs = [chunk[2 * n][2], chunk[2 * n + 1][2]]
                pa = ps.next()
                self.mm(pa, pa[:], [(L["w_a"][:, n, kc, co:co + 128], ucbs[kc][:]) for kc in range(2)], reads=[L["w_a"]] + ucbs)
                px = ps.next()
                self.mm(px, px[:], [(L["w_x"][:, n, kc, co:co + 128], ucbs[kc][:]) for kc in range(2)], reads=[L["w_x"]] + ucbs)
                kb.op("act", I("activation", out=rr[:], in_=pa[:], func=AF.Sigmoid, bias=cols[:, 40 + oc:41 + oc]), reads=[pa, cols], writes=[rr])
                kb.op("act", I("activation", out=ii[:], in_=px[:], func=AF.Sigmoid, bias=cols[:, 48 + oc:49 + oc]), reads=[px, cols], writes=[ii])
                kb.op("act", I("activation", out=a[:], in_=rr[:], func=AF.Exp, scale=cols[:, 64 + oc:65 + oc]), reads=[rr, cols], writes=[a])
                kb.op("act", I("activation", out=a2[:], in_=rr[:], func=AF.Exp, scale=cols[:, 72 + oc:73 + oc]), reads=[rr, cols], writes=[a2])
                kb.op("dve", I("tensor_scalar", a2[:], a2[:], -1.0, 1.0, op0=ALU.mult, op1=ALU.add), reads=[a2], writes=[a2])
                kb.op("act", I("activation", out=a2[:], in_=a2[:], func=AF.Sqrt), reads=[a2], writes=[a2])
                kb.op("dve", I("tensor_tensor", out=ii[:], in0=ii[:], in1=uc[:], op=ALU.mult), reads=[ii, uc], writes=[ii])
                kb.op("dve", I("tensor_tensor", out=ii[:], in0=ii[:], in1=a2[:], op=ALU.mult), reads=[ii, a2], writes=[ii])
                h = L["h"].next()
                kb.op("dve", I("tensor_tensor_scan", out=h[:], data0=a[:], data1=ii[:], initial=carry[:, oc:oc + 1], op0=ALU.mult, op1=ALU.add),
                      reads=[a, ii, carry], writes=[h])
                kb.op("dve", I("tensor_copy", out=carry[:, oc:oc + 1], in_=h[:, TT - 1:TT]), reads=[h], writes=[carry])
                kb.op("dve", I("tensor_tensor", out=y_out[:, oc, :], in0=h[:], in1=gate[:], op=ALU.mult), reads=[h, gate], joins=[y_out])

        s1(0)
        for n in range(4):
            if n + 1 < 4:
                s1(n + 1)
            s2(n)

    def phase_moe(self, l):
        kb, d = self.kb, self.d
        kb.begin_phase()
        NBLK = CAP // 128
        PARTS = ((0, 512), (512, CAP - 512))
        ps = Rot([kb.ps(f"mo_ps{i}", [128, 512]) for i in range(5)])
        ptb = Rot([kb.ps(f"mo_ptb{i}", [128, 1024], BF16) for i in range(3)])
        tmp = kb.sb("mo_tmp", [128, 128], F32)
        wu = Rot([kb.sb(f"mo_wu{i}", [128, KC, 2 * DFF], BF16) for i in range(2)])
        wd = Rot([kb.sb(f"mo_wd{i}", [128, KC, D], BF16) for i in range(2)])
        bdb = Rot([kb.sb(f"mo_bd{i}", [128, D], F32) for i in range(2)])
        bup = kb.sb("mo_bup", [128, NE * 16], F32)
        for e4 in range(0, NE, 8):
            self.load_cols(bup, bup[:, e4 * 16:(e4 + 8) * 16], d["moe_b_up"],
                           d["moe_b_up"][l, e4:e4 + 8, :].rearrange("e (c p) -> (e c) p", p=128), 128, ps.next(), tmp)
        xg = Rot([kb.sb(f"mo_xg{i}", [128, NBLK, D], BF16) for i in range(2)])
        xgT = kb.sb("mo_xgT", [128, KC, CAP], BF16)
        hT = kb.sb("mo_hT", [128, KC, CAP], BF16)
        yst = Rot([kb.sb(f"mo_ys{i}", [128, NBLK, D], BF16) for i in range(2)])
        g_ = Rot([kb.sb(f"mo_g{i}", [128, CAP], F32) for i in range(2)])
        sg_ = Rot([kb.sb(f"mo_sg{i}", [128, CAP], F32) for i in range(2)])
        u_ = Rot([kb.sb(f"mo_u{i}", [128, CAP], F32) for i in range(2)])
        bufs = {}

        def load(ex):
            wu_, wd_, bd_, xg_ = wu.next(), wd.next(), bdb.next(), xg.next()
            bufs[ex] = (wu_, wd_, bd_, xg_)
            rows = slice(ex * CAP, (ex + 1) * CAP)
            kb.dma("sp", xg_[:], d["xg"][rows, :].rearrange("(b p) f -> p b f", p=128), reads=[d["xg"]], writes=[xg_])
            kb.dma("sp", bd_[:], d["moe_b_down"][l, ex:ex + 1, :].partition_broadcast(128), reads=[d["moe_b_down"]], writes=[bd_])
            for q4 in range(4):
                kb.dma("pool", wu_[:, 2 * q4:2 * q4 + 2, :], d["moe_w_up"][l, ex, q4 * 256:(q4 + 1) * 256, :].rearrange("(c p) f -> p c f", p=128),
                       reads=[d["moe_w_up"]], joins=[wu_])
            for q2 in range(2):
                kb.dma("pool", wd_[:, 4 * q2:4 * q2 + 4, :], d["moe_w_down"][l, ex, q2 * 512:(q2 + 1) * 512, :].rearrange("(c p) f -> p c f", p=128),
                       reads=[d["moe_w_down"]], joins=[wd_])

        def transposes(ex):
            xg_ = bufs[ex][3]
            for kc in range(KC):
                tp = ptb.next()
                fns = [I("transpose", tp[:, b * 128:(b + 1) * 128], xg_[:, b, kc * 128:(kc + 1) * 128], self.ident_b[:]) for b in range(NBLK)]
                kb.op("pe", fns, reads=[xg_, self.ident_b], writes=[tp])
                self.copy(self.cp_eng(), xgT, xgT[:, kc, :], tp, tp[:, 0:CAP])

        def up(ex):
            wu_ = bufs[ex][0]
            for fc in range(KC):
                gg, sg, uu = g_.next(), sg_.next(), u_.next()
                cg = ex * 16 + fc
                cu = ex * 16 + 8 + fc
                for (s0, sw) in PARTS:
                    pg = ps.next()
                    self.mm(pg, pg[:, 0:sw], [(wu_[:, kc, fc * 128:(fc + 1) * 128], xgT[:, kc, s0:s0 + sw]) for kc in range(KC)], reads=[wu_, xgT])
                    kb.op("dve", I("tensor_scalar", gg[:, s0:s0 + sw], pg[:, 0:sw], bup[:, cg:cg + 1], 7.0, op0=ALU.add, op1=ALU.min), reads=[pg, bup], joins=[gg])
                    pu = ps.next()
                    self.mm(pu, pu[:, 0:sw], [(wu_[:, kc, DFF + fc * 128:DFF + (fc + 1) * 128], xgT[:, kc, s0:s0 + sw]) for kc in range(KC)], reads=[wu_, xgT])
                    kb.op("act", I("activation", out=uu[:, s0:s0 + sw], in_=pu[:, 0:sw], func=AF.Identity, bias=bup[:, cu:cu + 1]), reads=[pu, bup], joins=[uu])
                kb.op("act", I("activation", out=sg[:], in_=gg[:], func=AF.Sigmoid, scale=1.702), reads=[gg], writes=[sg])
                kb.op("pool", I("tensor_scalar", uu[:], uu[:], 7.0, -7.0, op0=ALU.min, op1=ALU.max), reads=[uu], writes=[uu])
                kb.op("pool", I("tensor_tensor", out=gg[:], in0=gg[:], in1=sg[:], op=ALU.mult), reads=[gg, sg], writes=[gg])
                kb.op("dve", I("scalar_tensor_tensor", out=hT[:, fc, :], in0=uu[:], scalar=1.0, in1=gg[:], op0=ALU.add, op1=ALU.mult), reads=[uu, gg], joins=[hT])

        def down(ex):
            _, wd_, bd_, _ = bufs[ex]
            rows = slice(ex * CAP, (ex + 1) * CAP)
            ys_ = yst.next()
            for b in range(NBLK):
                for hf in range(2):
                    pt = ps.next()
                    pairs = [(hT[:, fc, b * 128:(b + 1) * 128], wd_[:, fc, hf * 512:(hf + 1) * 512]) for fc in range(KC)]
                    self.mm(pt, pt[:], pairs, reads=[hT, wd_])
                    kb.op("dve", I("scalar_tensor_tensor", out=ys_[:, b, hf * 512:(hf + 1) * 512], in0=pt[:], scalar=1.0, in1=bd_[:, hf * 512:(hf + 1) * 512],
                                   op0=ALU.mult, op1=ALU.add), reads=[pt, bd_], joins=[ys_])
            kb.dma("sp", d["ys"][rows, :].rearrange("(b p) f -> p b f", p=128), ys_[:], reads=[ys_], joins=[d["ys"]])

        load(0)
        transposes(0)
        for ex in range(NE):
            if ex + 1 < NE:
                load(ex + 1)
            up(ex)
            if ex + 1 < NE:
                transposes(ex + 1)
            down(ex)
        kb.end_phase()

    def phase_combine(self, l):
        kb, d = self.kb, self.d
        kb.begin_phase()
        rows = [Rot([kb.sb(f"cb_r{k}_{i}", [128, D], BF16) for i in range(2)]) for k in range(4)]
        for k in range(4):
            for t_ in rows[k].tiles:
                kb.op("dve", I("memset", t_[:], 0.0), reads=[], writes=[t_])
        yt = Rot([kb.sb(f"cb_y{i}", [128, 4, D], F32) for i in range(2)])
        for j in range(NT):
            ts = slice(j * TT, (j + 1) * TT)
            yt_ = yt.next()
            for b in range(4):
                gb = j * 4 + b
                rk = [rows[k].next() for k in range(4)]
                for k in range(4):
                    kb.idma(rk[k][:], None, d["ys"][:, :], bass.IndirectOffsetOnAxis(self.slots_all[:, gb, k:k + 1], 0), NSLOT - 1,
                            reads=[d["ys"], self.slots_all], writes=[rk[k]])
                kb.op("dve", I("tensor_scalar", yt_[:, b, :], rk[0][:], self.gk_all[:, gb, 0:1], None, op0=ALU.mult), reads=[rk[0], self.gk_all], joins=[yt_])
                for k in range(1, 4):
                    kb.op("dve", I("scalar_tensor_tensor", out=yt_[:, b, :], in0=rk[k][:], scalar=self.gk_all[:, gb, k:k + 1], in1=yt_[:, b, :], op0=ALU.mult, op1=ALU.add),
                          reads=[rk[k], self.gk_all, yt_], joins=[yt_])
            kb.dma("sp", d["y"][ts, :].rearrange("(b p) f -> p b f", p=128), yt_[:], reads=[yt_], joins=[d["y"]])
        kb.end_phase()

    def phase_ln2_ple(self, l, last):
        kb, d = self.kb, self.d
        kb.begin_phase()
        ps = Rot([kb.ps(f"lp_ps{i}", [128, 512]) for i in range(7)])
        wg = kb.sb("lp_wg", [128, KC, D], BF16)
        kb.dma("pool", wg[:], d["ple_w_gate"][l].rearrange("(c p) f -> p c f", p=128), reads=[d["ple_w_gate"]], writes=[wg])
        wp = kb.sb("lp_wp", [128, 2, D], BF16)
        kb.dma("pool", wp[:], d["ple_w_proj"][l].rearrange("(c p) f -> p c f", p=128), reads=[d["ple_w_proj"]], writes=[wp])
        yt = Rot([kb.sb(f"lp_yt{i}", [128, 4, D], F32) for i in range(2)])
        x1f = Rot([kb.sb(f"lp_x1f{i}", [128, KC, TT], F32) for i in range(1)])
        pin = Rot([kb.sb(f"lp_pin{i}", [128, 4, PLE], F32) for i in range(2)])
        pTb = kb.sb("lp_pTb", [128, 2, TT], BF16)
        pres = Rot([kb.sb(f"lp_pre{i}", [128, KC, TT], F32) for i in range(2)])
        sq = kb.sb("lp_sq", [128, KC, TT], F32)
        mean = kb.sb("lp_mean", [128, TT], F32)
        rstd = kb.sb("lp_rstd", [128, TT], F32)
        tmp2 = kb.sb("lp_tmp2", [128, TT], F32)
        x2f = kb.sb("lp_x2f", [128, KC, TT], F32)
        x2b = kb.sb("lp_x2b", [128, KC, TT], BF16)
        sgm = Rot([kb.sb(f"lp_sg{i}", [128, TT], F32) for i in range(2)])
        xo = Rot([kb.sb(f"lp_xo{i}", [128, KC, TT], F32) for i in range(1)])
        otok = Rot([kb.sb(f"lp_ot{i}", [128, 4, D], F32) for i in range(1)]) if last else None
        g2 = self.lncols[:, 2 * 32 + l * 8:2 * 32 + l * 8 + 8]
        b2 = self.lncols[:, 3 * 32 + l * 8:3 * 32 + l * 8 + 8]
        x1Tv = d["x1T"][:].rearrange("(c p) t -> p c t", p=128)
        xTv = d["xT"][:].rearrange("(c p) t -> p c t", p=128)
        for j in range(NT):
            ts = slice(j * TT, (j + 1) * TT)
            yt_, x1f_, pin_ = yt.next(), x1f.next(), pin.next()
            kb.dma("sp", yt_[:], d["y"][ts, :].rearrange("(b p) f -> p b f", p=128), reads=[d["y"]], writes=[yt_])
            kb.dma("sp", x1f_[:], x1Tv[:, :, ts], reads=[d["x1T"]], writes=[x1f_])
            kb.dma("sp", pin_[:], d["p"][l, ts, :].rearrange("(b p) f -> p b f", p=128), reads=[d["p"]], writes=[pin_])
            for c in range(2):
                pt = ps.next()
                fns = [I("transpose", pt[:, b * 128:(b + 1) * 128], pin_[:, b, c * 128:(c + 1) * 128], self.ident_f[:]) for b in range(4)]
                kb.op("pe", fns, reads=[pin_, self.ident_f], writes=[pt])
                self.copy(self.cp_eng(), pTb, pTb[:, c, :], pt, pt[:])
            pre = pres.next()
            for mc in range(KC):
                pt = ps.next()
                fns = [I("transpose", pt[:, b * 128:(b + 1) * 128], yt_[:, b, mc * 128:(mc + 1) * 128], self.ident_f[:]) for b in range(4)]
                kb.op("pe", fns, reads=[yt_, self.ident_f], writes=[pt])
                kb.op("dve", I("scalar_tensor_tensor", out=pre[:, mc, :], in0=x1f_[:, mc, :], scalar=ALPHA, in1=pt[:], op0=ALU.mult, op1=ALU.add),
                      reads=[x1f_, pt], joins=[pre])
            self.layernorm(pre, sq, ps, mean, rstd, tmp2, g2, b2, x2f, x2b)
            xo_ = xo.next()
            for mc in range(KC):
                pg = ps.next()
                self.mm(pg, pg[:], [(wg[:, kc, mc * 128:(mc + 1) * 128], x2b[:, kc, :]) for kc in range(KC)], reads=[wg, x2b])
                pp = ps.next()
                self.mm(pp, pp[:], [(wp[:, kc, mc * 128:(mc + 1) * 128], pTb[:, kc, :]) for kc in range(2)], reads=[wp, pTb])
                sg = sgm.next()
                kb.op("act", I("activation", out=sg[:], in_=pg[:], func=AF.Sigmoid), reads=[pg], writes=[sg])
                kb.op("dve", I("tensor_tensor", out=sg[:], in0=sg[:], in1=pp[:], op=ALU.mult), reads=[sg, pp], writes=[sg])
                kb.op("pool", I("tensor_tensor", out=xo_[:, mc, :], in0=x2f[:, mc, :], in1=sg[:], op=ALU.add), reads=[x2f, sg], joins=[xo_])
            if not last:
                kb.dma("sp", xTv[:, :, ts], xo_[:], reads=[xo_], joins=[d["xT"]])
            else:
                if "xT" in self.debug:
                    kb.dma("sp", xTv[:, :, ts], xo_[:], reads=[xo_], joins=[d["xT"]])
                ot = otok.next()
                for b in range(4):
                    for hf in range(2):
                        pt = ps.next()
                        fns = [I("transpose", pt[:, c4 * 128:(c4 + 1) * 128], xo_[:, hf * 4 + c4, b * 128:(b + 1) * 128], self.ident_f[:]) for c4 in range(4)]
                        kb.op("pe", fns, reads=[xo_, self.ident_f], writes=[pt])
                        self.copy(self.cp_eng(), ot, ot[:, b, hf * 512:(hf + 1) * 512], pt, pt[:])
                kb.dma("sp", d["out"][ts, :].rearrange("(b p) f -> p b f", p=128), ot[:], reads=[ot], joins=[d["out"]])
        kb.end_phase()


def make_consts():
    bf = ml_dtypes.bfloat16
    c = {}
    c["c_ident_f"] = np.eye(128, dtype=np.float32)
    c["c_ident_b"] = np.eye(128, dtype=np.float32).astype(bf)
    c["c_ones_f"] = np.ones((128, 128), np.float32)
    q = np.arange(128)[:, None]
    k = np.arange(128)[None, :]
    c["c_mask_b"] = np.where(k <= q, 0.0, MASK_NEG).astype(np.float32).astype(bf)
    rt = np.zeros((64, 128), np.float32)
    for m in range(32):
        rt[m + 32, m] = -1.0
        rt[m, m + 32] = 1.0
    c["c_rt_b"] = rt.astype(bf)
    c["c_u_b"] = (np.arange(128)[:, None] < np.arange(128)[None, :]).astype(np.float32).astype(bf)
    c["c_ones_b"] = np.ones((128, 128), np.float32).astype(bf)
    c["c_ebase"] = np.tile((np.arange(NE, dtype=np.float32) * CAP)[None, :], (128, 1)).astype(np.float32)
    half = 32
    invf = np.exp(-math.log(10000.0) * np.arange(half, dtype=np.float32) / half).astype(np.float32)
    c["c_invf"] = np.concatenate([invf, invf]).reshape(64, 1).astype(np.float32)
    return c


_PROG_CACHE = {}


def get_prog(n_layers=DEPTH, debug=()):
    key = (n_layers, tuple(sorted(debug)))
    if key not in _PROG_CACHE:
        _PROG_CACHE[key] = Prog(n_layers, debug)
    return _PROG_CACHE[key]


WEIGHT_NAMES = ["mla_w_in", "mla_q_norm", "mla_kv_norm", "mla_w_uq", "mla_w_ukv", "mla_w_o",
                "lru_w_in", "lru_conv_w", "lru_conv_b", "lru_w_a", "lru_b_a", "lru_w_x", "lru_b_x", "lru_lambda", "lru_w_out",
                "ln1_g", "ln1_b", "ln2_g", "ln2_b", "moe_w_router", "moe_b_router", "moe_w_up", "moe_b_up",
                "moe_w_down", "moe_b_down", "ple_w_gate", "ple_w_proj"]


def make_in_map(inputs, b, consts, prog, shared=None):
    m = {}
    for n in prog.used_inputs:
        shape = prog.in_shapes[n][0]
        if n == "x":
            m[n] = np.ascontiguousarray(np.asarray(inputs["x"])[b], dtype=np.float32)
        elif n == "p":
            m[n] = np.ascontiguousarray(np.asarray(inputs["p"])[:shape[0], b], dtype=np.float32)
        elif n == "positions":
            m[n] = np.ascontiguousarray(np.asarray(inputs["positions"])[b:b + 1], dtype=np.int32)
        elif n.startswith("c_"):
            m[n] = consts[n]
        else:
            if shared is not None and n in shared:
                m[n] = shared[n]
            else:
                a = np.ascontiguousarray(np.asarray(inputs[n])[:shape[0]], dtype=np.float32)
                if shared is not None:
                    shared[n] = a
                m[n] = a
    return m


def kernel(**inputs):
    prog = get_prog()
    consts = make_consts()
    nb = np.asarray(inputs["x"]).shape[0]
    shared = {}
    in_maps = [make_in_map(inputs, b, consts, prog, shared) for b in range(nb)]
    res = run_bass_kernel_spmd(prog.nc, in_maps, core_ids=list(range(nb)))
    return np.stack([np.asarray(r["out"], dtype=np.float32) for r in res.results], axis=0)
```

```python
from contextlib import ExitStack
import math
import os
STOP = int(os.environ.get('K_STOP', '0'))
import numpy as np
import ml_dtypes
import concourse.bass as bass
import concourse.mybir as mybir
from concourse.bass_utils import run_bass_kernel_spmd

F32 = mybir.dt.float32
BF16 = mybir.dt.bfloat16
I32 = mybir.dt.int32
AF = mybir.ActivationFunctionType
ALU = mybir.AluOpType
AX = mybir.AxisListType

D = 1024
T = 4096
DEPTH = 4
TT = 512
NT = T // TT
KC = D // 128
H = 8
QL, KVL, RD = 768, 256, 64
NE = 32
DFF = 1024
PLE = 256
ALPHA = (2.0 * DEPTH) ** 0.25
LN_EPS = 1e-5
RMS_EPS = 1e-6
ATT_SCALE = 1.0 / math.sqrt(192.0)
MASK_NEG = -30000.0
TWO_PI = 2.0 * math.pi
CAP = 768
NSLOT = NE * CAP
BIGV = 65536.0
U32 = mybir.dt.uint32


class Res:
    __slots__ = ("w", "rs", "name")

    def __init__(self, name=""):
        self.w = []
        self.rs = []
        self.name = name


class TL:
    def __init__(self, h, name=""):
        self.h = h
        self.res = Res(name)

    def __getitem__(self, idx):
        return self.h[idx]


N_DMA_SEMS = 12


class KB:
    ENGS = ("pe", "act", "dve", "pool", "sp")

    def __init__(self, nc):
        self.nc = nc
        self.es = ExitStack()
        self.q = {e: [] for e in self.ENGS}
        self.cnt = {e: 0 for e in self.ENGS}
        self.sem = {e: self.es.enter_context(nc.semaphore("s_" + e)) for e in self.ENGS}
        self.dsem, self.dval, self.dnext = {}, {}, {}
        for qn in ("sp", "pool", "act"):
            self.dsem[qn] = [self.es.enter_context(nc.semaphore(f"d_{qn}{i}")) for i in range(N_DMA_SEMS)]
            self.dval[qn] = [0] * N_DMA_SEMS
            self.dnext[qn] = 0
        self.waited = {e: {} for e in self.ENGS}
        self.semobj = {}
        self.n_ops = 0
        self.phase_stack = None

    def begin_phase(self):
        self.phase_stack = ExitStack()
        if getattr(self, "sb_base", None) is None:
            self.sb_base = (self.nc._sbuf_addr_for_side(None) + 63) // 64 * 64
        self.sb_ptr = self.sb_base

    def end_phase(self):
        self.barrier()
        self.phase_stack.close()
        self.phase_stack = None

    def _stack(self):
        return self.phase_stack if self.phase_stack is not None else self.es

    def _nm(self, name):
        self.uid = getattr(self, "uid", 0) + 1
        return f"{name}_{self.uid}"

    def sb(self, name, shape, dt):
        name = self._nm(name)
        if self.phase_stack is not None:
            nbytes = int(np.prod(shape[1:])) * (2 if dt == BF16 else 4)
            off = (self.sb_ptr + 63) // 64 * 64
            self.sb_ptr = off + nbytes
            assert self.sb_ptr <= 229344, f"SBUF phase arena overflow at {name}: {self.sb_ptr}"
            return TL(self.nc.alloc_sbuf_tensor_at(name, list(shape), dt, offset=off), name)
        return TL(self._stack().enter_context(self.nc.sbuf_tensor(name, list(shape), dt)), name)

    def ps(self, name, shape, dt=F32):
        name = self._nm(name)
        return TL(self._stack().enter_context(self.nc.psum_tensor(name, list(shape), dt)), name)

    def dram(self, name, shape, dt, kind="Internal"):
        return TL(self.nc.dram_tensor(name, list(shape), dt, kind=kind), name)

    @staticmethod
    def _r(x):
        return x.res if isinstance(x, TL) else x

    @staticmethod
    def _compact(lst):
        best = {}
        for (s, v) in lst:
            k = id(s)
            if k not in best or best[k][1] < v:
                best[k] = (s, v)
        return list(best.values())

    def _deps(self, eng, reads, writes, joins=()):
        need = {}
        own = self.sem["pe"] if eng == "pe" else None

        def add(tok):
            s, v = tok
            if s is own:
                return
            k = id(s)
            self.semobj[k] = s
            if need.get(k, 0) < v:
                need[k] = v

        for r in reads:
            for t in self._r(r).w:
                add(t)
        for w in writes:
            rr = self._r(w)
            for t in rr.w:
                add(t)
            for t in rr.rs:
                add(t)
        for w in joins:
            rr = self._r(w)
            for t in rr.rs:
                add(t)
        out = []
        wd = self.waited[eng]
        for k, v in need.items():
            if wd.get(k, 0) < v:
                wd[k] = v
                out.append((self.semobj[k], v))
        return out

    def _commit(self, tok, reads, writes, joins=()):
        for r in reads:
            rr = self._r(r)
            rr.rs.append(tok)
            if len(rr.rs) > 48:
                rr.rs = self._compact(rr.rs)
        for w in writes:
            rr = self._r(w)
            rr.w = [tok]
            rr.rs = []
        for w in joins:
            rr = self._r(w)
            rr.w.append(tok)
            if len(rr.w) > 48:
                rr.w = self._compact(rr.w)

    def op(self, eng, fn, reads=(), writes=(), joins=()):
        waits = self._deps(eng, reads, writes, joins)
        self.cnt[eng] += 1
        tok = (self.sem[eng], self.cnt[eng])
        self.q[eng].append((waits, fn, (self.sem[eng], 1)))
        self._commit(tok, reads, writes, joins)
        self.n_ops += 1
        return tok

    def dma(self, qn, out_ap, in_ap, reads=(), writes=(), joins=()):
        waits = self._deps(qn, reads, writes, joins)
        i = self.dnext[qn]
        self.dnext[qn] = (i + 1) % N_DMA_SEMS
        s = self.dsem[qn][i]
        prev = self.dval[qn][i]
        self.semobj[id(s)] = s
        if prev > 0:
            wd = self.waited[qn]
            if wd.get(id(s), 0) < prev:
                wd[id(s)] = prev
                waits.append((s, prev))
        self.dval[qn][i] = prev + 16
        tok = (s, prev + 16)

        def fn(e, out_ap=out_ap, in_ap=in_ap):
            return e.dma_start(out=out_ap, in_=in_ap)

        self.q[qn].append((waits, fn, (s, 16)))
        self._commit(tok, reads, writes, joins)
        self.n_ops += 1
        return tok

    def idma(self, out_ap, out_off, in_ap, in_off, bounds, reads=(), writes=(), joins=()):
        qn = "pool"
        waits = self._deps(qn, reads, writes, joins)
        i = self.dnext[qn]
        self.dnext[qn] = (i + 1) % N_DMA_SEMS
        s = self.dsem[qn][i]
        prev = self.dval[qn][i]
        self.semobj[id(s)] = s
        if prev > 0:
            wd = self.waited[qn]
            if wd.get(id(s), 0) < prev:
                wd[id(s)] = prev
                waits.append((s, prev))
        self.dval[qn][i] = prev + 16
        tok = (s, prev + 16)

        def fn(e):
            if getattr(self, "_breg", None) is None:
                self._breg = e.to_reg(bounds)
            return e.indirect_dma_start(out=out_ap, out_offset=out_off, in_=in_ap, in_offset=in_off,
                                        bounds_check=self._breg, oob_is_err=False)

        self.q[qn].append((waits, fn, (s, 16)))
        self._commit(tok, reads, writes, joins)
        self.n_ops += 1
        return tok

    def barrier(self):
        toks = [(self.sem[e], self.cnt[e]) for e in self.ENGS if self.cnt[e] > 0]
        for qn in self.dsem:
            for s, v in zip(self.dsem[qn], self.dval[qn]):
                if v > 0:
                    toks.append((s, v))
        for e in self.ENGS:
            waits = []
            wd = self.waited[e]
            for (s, v) in toks:
                if s is self.sem[e]:
                    continue
                if wd.get(id(s), 0) < v:
                    wd[id(s)] = v
                    waits.append((s, v))
            if waits:
                self.q[e].append((waits, None, None))

    def emit(self):
        q = self.q

        def run(e, lst):
            for waits, fn, inc in lst:
                for (s, v) in waits:
                    e.wait_ge(s, v)
                if fn is None:
                    continue
                if isinstance(fn, (list, tuple)):
                    ins = None
                    for f in fn:
                        ins = f(e)
                else:
                    ins = fn(e)
                ins.then_inc(inc[0], inc[1])

        with self.nc.Block() as block:
            @block.tensor
            def _(e):
                run(e, q["pe"])

            @block.scalar
            def _(e):
                run(e, q["act"])

            @block.vector
            def _(e):
                run(e, q["dve"])

            @block.gpsimd
            def _(e):
                run(e, q["pool"])

            @block.sync
            def _(e):
                run(e, q["sp"])

    def close(self):
        self.es.close()


def I(name, *a, **k):
    return lambda e: getattr(e, name)(*a, **k)


class Rot:
    def __init__(self, tiles):
        self.tiles = tiles
        self.i = 0

    def next(self):
        t = self.tiles[self.i]
        self.i = (self.i + 1) % len(self.tiles)
        return t


class Prog:
    def __init__(self, n_layers=DEPTH, debug=(), phases=None):
        self.n_layers = n_layers
        self.debug = set(debug)
        self.phases = phases
        nc = bass.Bass("TRN2", target_bir_lowering=False)
        self.nc = nc
        kb = self.kb = KB(nc)
        IN = "ExternalInput"
        NL = n_layers
        na = max(1, (NL + 1) // 2)
        nl = max(1, NL // 2)
        self.in_shapes = {
            "x": ([T, D], F32), "p": ([NL, T, PLE], F32), "positions": ([1, T], I32),
            "mla_w_in": ([na, D, QL + KVL + RD], F32), "mla_q_norm": ([na, QL], F32), "mla_kv_norm": ([na, KVL], F32),
            "mla_w_uq": ([na, QL, H * 192], F32), "mla_w_ukv": ([na, KVL, H * 256], F32), "mla_w_o": ([na, D, D], F32),
            "lru_w_in": ([nl, D, 2 * D], F32), "lru_conv_w": ([nl, 4, D], F32), "lru_conv_b": ([nl, D], F32),
            "lru_w_a": ([nl, 4, 256, 256], F32), "lru_b_a": ([nl, D], F32), "lru_w_x": ([nl, 4, 256, 256], F32),
            "lru_b_x": ([nl, D], F32), "lru_lambda": ([nl, D], F32), "lru_w_out": ([nl, D, D], F32),
            "ln1_g": ([NL, D], F32), "ln1_b": ([NL, D], F32), "ln2_g": ([NL, D], F32), "ln2_b": ([NL, D], F32),
            "moe_w_router": ([NL, D, NE], F32), "moe_b_router": ([NL, NE], F32),
            "moe_w_up": ([NL, NE, D, 2 * DFF], F32), "moe_b_up": ([NL, NE, 2 * DFF], F32),
            "moe_w_down": ([NL, NE, DFF, D], F32), "moe_b_down": ([NL, NE, D], F32),
            "ple_w_gate": ([NL, D, D], F32), "ple_w_proj": ([NL, PLE, D], F32),
            "c_ident_f": ([128, 128], F32), "c_ident_b": ([128, 128], BF16), "c_ones_f": ([128, 128], F32),
            "c_mask_b": ([128, 128], BF16), "c_rt_b": ([64, 128], BF16), "c_invf": ([64, 1], F32),
            "c_u_b": ([128, 128], BF16), "c_ones_b": ([128, 128], BF16), "c_ebase": ([128, NE], F32),
        }
        self.used_inputs = []

        class LazyD(dict):
            def __missing__(dself, name):
                shape, dt = self.in_shapes[name]
                t = kb.dram(name, shape, dt, kind=IN)
                dself[name] = t
                self.used_inputs.append(name)
                return t

        d = self.d = LazyD()

        def scr(name, shape, dt=F32):
            kind = "ExternalOutput" if name in self.debug else "Internal"
            d[name] = kb.dram(name, shape, dt, kind=kind)

        scr("xT", [D, T]); scr("x1T", [D, T]); scr("x1b", [D, T], BF16)
        scr("cs", [2, 64, T])
        scr("qn", [H, 128, T], BF16); scr("qr", [H, 64, T], BF16); scr("kn", [H, 128, T], BF16)
        scr("kr", [64, T], BF16); scr("v", [T, H * 128], BF16); scr("oT", [H, 128, T], BF16)
        scr("y", [T, D]); scr("xg", [NSLOT + 128, D], BF16); scr("ys", [NSLOT + 128, D], BF16)
        d["out"] = kb.dram("out", [T, D], F32, kind="ExternalOutput")
        self.ln_nl = NL

        self.ident_f = kb.sb("ident_f", [128, 128], F32)
        self.ident_b = kb.sb("ident_b", [128, 128], BF16)
        self.ones_f = kb.sb("ones_f", [128, 128], F32)
        self.mask_b = kb.sb("mask_b", [128, 128], BF16)
        self.rt_b = kb.sb("rt_b", [64, 128], BF16)
        self.u_b = kb.sb("u_b", [128, 128], BF16)
        self.ones_b = kb.sb("ones_b", [128, 128], BF16)
        self.ebase = kb.sb("ebase", [128, NE], F32)
        self.slots_all = kb.sb("slots_all", [128, T // 128, 4], I32)
        self.gk_all = kb.sb("gk_all", [128, T // 128, 4], F32)
        self.cnt = kb.sb("cnt", [128, NE], F32)
        for t_, n_ in ((self.ident_f, "c_ident_f"), (self.ident_b, "c_ident_b"), (self.ones_f, "c_ones_f"),
                       (self.mask_b, "c_mask_b"), (self.rt_b, "c_rt_b"), (self.u_b, "c_u_b"),
                       (self.ones_b, "c_ones_b"), (self.ebase, "c_ebase")):
            kb.dma("sp", t_[:], d[n_][:], reads=[d[n_]], writes=[t_])
        self.lncols = kb.sb("lncols", [128, 4 * 32], F32)
        self.eng_rr = 0

        self.build()
        if not d["out"].res.w:
            zt = kb.sb("zt", [128, D], F32)
            kb.op("dve", I("memset", zt[:], 0.0), reads=[], writes=[zt])
            kb.dma("sp", d["out"][0:128, :], zt[:], reads=[zt], joins=[d["out"]])
        outs = list(d["out"].res.w)
        for n in self.debug:
            outs += list(d[n].res.w)
        kb.q["sp"].append((outs, None, None))
        kb.emit()
        kb.close()

    def cp_eng(self):
        self.eng_rr ^= 1
        return "act" if self.eng_rr else "dve"

    def copy(self, eng, out_t, out_ap, in_t, in_ap):
        if eng == "act":
            self.kb.op("act", I("activation", out=out_ap, in_=in_ap, func=AF.Identity), reads=[in_t], joins=[out_t])
        else:
            if eng == "dve":
                self.kb.op(eng, I("tensor_scalar", out_ap, in_ap, 1.0, None, op0=ALU.mult), reads=[in_t], joins=[out_t])
            else:
                self.kb.op(eng, I("tensor_copy", out=out_ap, in_=in_ap), reads=[in_t], joins=[out_t])

    def mm(self, out_t, out_ap, pairs, reads):
        n = len(pairs)
        fns = []
        for i, (l, r) in enumerate(pairs):
            fns.append(I("matmul", out_ap, lhsT=l, rhs=r, start=(i == 0), stop=(i == n - 1)))
        self.kb.op("pe", fns, reads=reads, writes=[out_t])

    def load_cols(self, dst_t, dst_ap, src_t, src_ap, n, ps_t, tmp_t):
        kb = self.kb
        kb.dma("sp", tmp_t[0:n, :], src_ap, reads=[src_t], writes=[tmp_t])
        kb.op("pe", I("transpose", ps_t[:, 0:n], tmp_t[0:n, :], self.ident_f[0:n, 0:n]),
              reads=[tmp_t, self.ident_f], writes=[ps_t])
        self.copy("dve", dst_t, dst_ap, ps_t, ps_t[:, 0:n])

    def want(self, name):
        return self.phases is None or name in self.phases

    def build(self):
        if self.want("setup"):
            self.phase_setup()
        for l in range(self.n_layers):
            if l % 2 == 0:
                if self.want(f"proj{l}"):
                    self.phase_mla_proj(l)
                if self.want(f"attn{l}"):
                    self.phase_attn(l)
                if self.want(f"mix{l}"):
                    self.phase_mix_ln1(l, "mla")
            else:
                if self.want(f"mix{l}"):
                    self.phase_mix_ln1(l, "lru")
            if self.want(f"moe{l}"):
                self.phase_moe(l)
            if self.want(f"comb{l}"):
                self.phase_combine(l)
            if self.want(f"ple{l}"):
                self.phase_ln2_ple(l, last=(l == self.n_layers - 1))

    def phase_setup(self):
        kb, d = self.kb, self.d
        kb.begin_phase()
        ps = Rot([kb.ps(f"su_ps{i}", [128, 512]) for i in range(4)])
        tmp = kb.sb("su_tmp", [128, 128], F32)
        for v, nm in enumerate(("ln1_g", "ln1_b", "ln2_g", "ln2_b")):
            src = d[nm][:].rearrange("l (c p) -> (l c) p", p=128)
            nrow = self.ln_nl * 8
            self.load_cols(self.lncols, self.lncols[:, v * 32:v * 32 + nrow], d[nm], src, nrow, ps.next(), tmp)
        invf = kb.sb("su_invf", [64, 1], F32)
        kb.dma("sp", invf[:], d["c_invf"][:], reads=[d["c_invf"]], writes=[invf])
        pos_i = kb.sb("su_posi", [64, T], I32)
        kb.dma("sp", pos_i[:], d["positions"][0:1, :].partition_broadcast(64), reads=[d["positions"]], writes=[pos_i])
        ang = kb.sb("su_ang", [64, T], F32)
        kb.op("dve", I("tensor_copy", out=ang[:], in_=pos_i[:]), reads=[pos_i], writes=[ang])
        kb.op("dve", I("tensor_scalar", ang[:], ang[:], invf[:, 0:1], None, op0=ALU.mult), reads=[ang, invf], writes=[ang])
        kf_i = kb.sb("su_kfi", [64, T], I32)
        kf = kb.sb("su_kf", [64, T], F32)
        r = kb.sb("su_r", [64, T], F32)
        fx = kb.sb("su_fx", [64, T], F32)
        C1 = 6.28125
        C2 = TWO_PI - C1
        for which, shift in ((1, 0.0), (0, math.pi / 2)):
            kb.op("dve", I("tensor_scalar", kf[:], ang[:], shift, 1.0 / TWO_PI, op0=ALU.add, op1=ALU.mult), reads=[ang], writes=[kf])
            kb.op("dve", I("tensor_copy", out=kf_i[:], in_=kf[:]), reads=[kf], writes=[kf_i])
            kb.op("dve", I("tensor_copy", out=kf[:], in_=kf_i[:]), reads=[kf_i], writes=[kf])
            kb.op("dve", I("scalar_tensor_tensor", out=r[:], in0=kf[:], scalar=-C1, in1=ang[:], op0=ALU.mult, op1=ALU.add), reads=[kf, ang], writes=[r])
            kb.op("dve", I("scalar_tensor_tensor", out=r[:], in0=kf[:], scalar=-C2, in1=r[:], op0=ALU.mult, op1=ALU.add), reads=[kf, r], writes=[r])
            if shift != 0.0:
                kb.op("dve", I("tensor_scalar", r[:], r[:], shift, None, op0=ALU.add), reads=[r], writes=[r])
            kb.op("dve", I("tensor_scalar", fx[:], r[:], math.pi, -TWO_PI, op0=ALU.is_gt, op1=ALU.mult), reads=[r], writes=[fx])
            kb.op("dve", I("tensor_tensor", out=r[:], in0=r[:], in1=fx[:], op=ALU.add), reads=[r, fx], writes=[r])
            kb.op("dve", I("tensor_scalar", fx[:], r[:], -math.pi, TWO_PI, op0=ALU.is_lt, op1=ALU.mult), reads=[r], writes=[fx])
            kb.op("dve", I("tensor_tensor", out=r[:], in0=r[:], in1=fx[:], op=ALU.add), reads=[r, fx], writes=[r])
            kb.op("dve", I("tensor_scalar", r[:], r[:], math.pi, -math.pi, op0=ALU.min, op1=ALU.max), reads=[r], writes=[r])
            kb.op("act", I("activation", out=fx[:], in_=r[:], func=AF.Sin), reads=[r], writes=[fx])
            kb.dma("sp", d["cs"][which], fx[:], reads=[fx], joins=[d["cs"]])
        zt = kb.sb("su_zt", [128, 6, D], BF16)
        kb.op("dve", I("memset", zt[:], 0.0), reads=[], writes=[zt])
        for r0 in range(0, NSLOT + 128, 768):
            nr = min(768, NSLOT + 128 - r0)
            kb.dma("sp", d["xg"][r0:r0 + nr, :].rearrange("(b p) f -> p b f", p=128), zt[:, 0:nr // 128, :], reads=[zt], joins=[d["xg"]])
        xin = Rot([kb.sb(f"su_xin{i}", [128, 4, D], F32) for i in range(2)])
        xo = Rot([kb.sb(f"su_xo{i}", [128, KC, TT], F32) for i in range(2)])
        xTv = d["xT"][:].rearrange("(c p) t -> p c t", p=128)
        for j in range(NT):
            xi = xin.next()
            kb.dma("sp", xi[:], d["x"][j * TT:(j + 1) * TT, :].rearrange("(b p) f -> p b f", p=128), reads=[d["x"]], writes=[xi])
            xo_ = xo.next()
            for c in range(KC):
                pt = ps.next()
                fns = [I("transpose", pt[:, b * 128:(b + 1) * 128], xi[:, b, c * 128:(c + 1) * 128], self.ident_f[:]) for b in range(4)]
                kb.op("pe", fns, reads=[xi, self.ident_f], writes=[pt])
                self.copy(self.cp_eng(), xo_, xo_[:, c, :], pt, pt[:])
            kb.dma("sp", xTv[:, :, j * TT:(j + 1) * TT], xo_[:], reads=[xo_], joins=[d["xT"]])
        kb.end_phase()

    def rope(self, src_ps_t, src_ps_ap, cs_t, out_t, out_ap, ps_rot, tmp_f, tmp_b, tmp2, n=TT):
        kb = self.kb
        if STOP == 311:
            return
        kb.op("act", I("activation", out=tmp_f[0:64, :n], in_=src_ps_ap, func=AF.Identity), reads=[src_ps_t], writes=[tmp_f])
        if STOP == 312:
            return
        kb.op("act", I("activation", out=tmp_b[0:64, :n], in_=src_ps_ap, func=AF.Identity), reads=[src_ps_t], writes=[tmp_b])
        if STOP == 31:
            return
        rp = ps_rot.next()
        self.mm(rp, rp[:, :n], [(self.rt_b[:, :], tmp_b[0:64, :n])], reads=[self.rt_b, tmp_b])
        if STOP == 32:
            return
        kb.op("dve", I("tensor_tensor", out=tmp2[0:64, :n], in0=rp[0:64, :n], in1=cs_t[0:64, 1, :n], op=ALU.mult), reads=[rp, cs_t], writes=[tmp2])
        kb.op("dve", I("tensor_tensor", out=tmp_f[0:64, :n], in0=tmp_f[0:64, :n], in1=cs_t[0:64, 0, :n], op=ALU.mult), reads=[tmp_f, cs_t], writes=[tmp_f])
        kb.op("dve", I("tensor_tensor", out=out_ap, in0=tmp_f[0:64, :n], in1=tmp2[0:64, :n], op=ALU.add), reads=[tmp_f, tmp2], writes=[out_t])

    def phase_mla_proj(self, l):
        kb, d = self.kb, self.d
        j_ = l // 2
        kb.begin_phase()
        w_in = kb.sb("mp_win", [128, KC, QL + KVL + 128], BF16)
        w_uq = kb.sb("mp_wuq", [128, 6, H * 192 + 64], BF16)
        w_ukv = kb.sb("mp_wukv", [128, 2, H * 256], BF16)
        kb.op("dve", I("memset", w_in[:, :, 1088:1152], 0.0), reads=[], joins=[w_in])
        kb.op("dve", I("memset", w_uq[:, :, H * 192:H * 192 + 64], 0.0), reads=[], joins=[w_uq])
        kb.dma("pool", w_in[:, :, 0:1088], d["mla_w_in"][j_].rearrange("(c p) f -> p c f", p=128), reads=[d["mla_w_in"]], joins=[w_in])
        kb.dma("pool", w_uq[:, :, 0:H * 192], d["mla_w_uq"][j_].rearrange("(c p) f -> p c f", p=128), reads=[d["mla_w_uq"]], joins=[w_uq])
        kb.dma("pool", w_ukv[:], d["mla_w_ukv"][j_].rearrange("(c p) f -> p c f", p=128), reads=[d["mla_w_ukv"]], writes=[w_ukv])
        ps = Rot([kb.ps(f"mp_ps{i}", [128, 512]) for i in range(6)])
        tmp = kb.sb("mp_tmp", [128, 128], F32)
        ncol = kb.sb("mp_ncol", [128, 8], F32)
        self.load_cols(ncol, ncol[:, 0:6], d["mla_q_norm"], d["mla_q_norm"][j_].rearrange("(c p) -> c p", p=128), 6, ps.next(), tmp)
        self.load_cols(ncol, ncol[:, 6:8], d["mla_kv_norm"], d["mla_kv_norm"][j_].rearrange("(c p) -> c p", p=128), 2, ps.next(), tmp)

        if STOP == 1:
            kb.end_phase(); return
        xa = Rot([kb.sb(f"mp_xa{i}", [128, KC, TT], F32) for i in range(1)])
        xb = Rot([kb.sb(f"mp_xb{i}", [128, KC, TT], BF16) for i in range(2)])
        lat = kb.sb("mp_lat", [128, 8, TT], F32)
        sq = kb.sb("mp_sq", [128, 8, TT], F32)
        rstd = [kb.sb(f"mp_rstd{i}", [128, TT], F32) for i in range(2)]
        cqn = kb.sb("mp_cqn", [128, 8, TT], BF16)
        cs = Rot([kb.sb(f"mp_cs{i}", [64, 2, TT], F32) for i in range(2)])
        tf = kb.sb("mp_tf", [64, TT], F32)
        tb = kb.sb("mp_tb", [64, TT], BF16)
        t2 = kb.sb("mp_t2", [64, TT], F32)
        kr_o = Rot([kb.sb(f"mp_kro{i}", [64, TT], BF16) for i in range(2)])
        qn_o = Rot([kb.sb(f"mp_qno{i}", [128, H, TT], BF16) for i in range(1)])
        qr_o = Rot([kb.sb(f"mp_qro{i}", [64, H, TT], BF16) for i in range(1)])
        kn_o = Rot([kb.sb(f"mp_kno{i}", [128, H, TT], BF16) for i in range(1)])
        v_o = Rot([kb.sb(f"mp_vo{i}", [128, 4, H * 128], BF16) for i in range(1)])
        xTv = d["xT"][:].rearrange("(c p) t -> p c t", p=128)
        wv = w_ukv[:].rearrange("p c (h two e) -> p c h two e", two=2, e=128)
        for j in range(NT):
            ts = slice(j * TT, (j + 1) * TT)
            xa_ = xa.next(); xb_ = xb.next()
            kb.dma("sp", xa_[:], xTv[:, :, ts], reads=[d["xT"]], writes=[xa_])
            for c in range(KC):
                self.copy(self.cp_eng(), xb_, xb_[:, c, :], xa_, xa_[:, c, :])
            cs_ = cs.next()
            kb.dma("sp", cs_[:], d["cs"][:, :, ts].rearrange("a p t -> p a t"), reads=[d["cs"]], writes=[cs_])
            for oc in range(8):
                pt = ps.next()
                self.mm(pt, pt[:], [(w_in[:, kc, oc * 128:(oc + 1) * 128], xb_[:, kc, :]) for kc in range(KC)], reads=[w_in, xb_])
                kb.op("act", I("activation", out=lat[:, oc, :], in_=pt[:], func=AF.Identity), reads=[pt], joins=[lat])
                kb.op("act", I("activation", out=sq[:, oc, :], in_=pt[:], func=AF.Square), reads=[pt], joins=[sq])
            if STOP == 2:
                break
            ptk = ps.next()
            self.mm(ptk, ptk[:, :], [(w_in[:, kc, 1024:1152], xb_[:, kc, :]) for kc in range(KC)], reads=[w_in, xb_])
            kr_ = kr_o.next()
            self.rope(ptk, ptk[0:64, :], cs_, kr_, kr_[:, :], ps, tf, tb, t2)
            if STOP in (31, 32, 33, 311, 312):
                break
            kb.dma("sp", d["kr"][:, ts], kr_[:], reads=[kr_], joins=[d["kr"]])
            if STOP == 3:
                break
            for which, (c0, c1, dim) in enumerate(((0, 6, QL), (6, 8, KVL))):
                pt = ps.next()
                self.mm(pt, pt[:], [(self.ones_f[:], sq[:, c, :]) for c in range(c0, c1)], reads=[self.ones_f, sq])
                rs_ = rstd[which]
                kb.op("act", I("activation", out=rs_[:], in_=pt[:], func=AF.Sqrt, scale=1.0 / dim, bias=RMS_EPS), reads=[pt], writes=[rs_])
                kb.op("dve", I("reciprocal", out=rs_[:], in_=rs_[:]), reads=[rs_], writes=[rs_])
                for c in range(c0, c1):
                    kb.op("dve", I("scalar_tensor_tensor", out=cqn[:, c, :], in0=lat[:, c, :], scalar=ncol[:, c:c + 1], in1=rs_[:], op0=ALU.mult, op1=ALU.mult),
                          reads=[lat, ncol, rs_], joins=[cqn])
            if STOP == 4:
                break
            qn_ = qn_o.next(); qr_ = qr_o.next()
            for h in range(H):
                pt = ps.next()
                self.mm(pt, pt[:], [(w_uq[:, kc, h * 192:h * 192 + 128], cqn[:, kc, :]) for kc in range(6)], reads=[w_uq, cqn])
                self.copy(self.cp_eng(), qn_, qn_[:, h, :], pt, pt[:])
                pt2 = ps.next()
                self.mm(pt2, pt2[:, :], [(w_uq[:, kc, h * 192 + 128:h * 192 + 256], cqn[:, kc, :]) for kc in range(6)], reads=[w_uq, cqn])
                self.rope(pt2, pt2[0:64, :], cs_, qr_, qr_[:, h, :], ps, tf, tb, t2)
            kb.dma("sp", d["qn"][:, :, ts].rearrange("h p t -> p h t"), qn_[:], reads=[qn_], joins=[d["qn"]])
            kb.dma("sp", d["qr"][:, :, ts].rearrange("h p t -> p h t"), qr_[:], reads=[qr_], joins=[d["qr"]])
            if STOP == 5:
                break
            kn_ = kn_o.next()
            for h in range(H):
                pt = ps.next()
                self.mm(pt, pt[:], [(w_ukv[:, kc, h * 256:h * 256 + 128], cqn[:, 6 + kc, :]) for kc in range(2)], reads=[w_ukv, cqn])
                self.copy(self.cp_eng(), kn_, kn_[:, h, :], pt, pt[:])
            kb.dma("sp", d["kn"][:, :, ts].rearrange("h p t -> p h t"), kn_[:], reads=[kn_], joins=[d["kn"]])
            if STOP == 6:
                break
            v_ = v_o.next()
            for b in range(4):
                for hf in range(2):
                    pt = ps.next()
                    o_ap = pt[:].rearrange("p (a e) -> p a e", a=4)
                    self.mm(pt, o_ap, [(cqn[:, 6 + kc, b * 128:(b + 1) * 128], wv[:, kc, hf * 4:(hf + 1) * 4, 1, :]) for kc in range(2)], reads=[w_ukv, cqn])
                    self.copy(self.cp_eng(), v_, v_[:, b, hf * 512:(hf + 1) * 512], pt, pt[:])
            kb.dma("sp", d["v"][ts, :].rearrange("(b p) f -> p b f", p=128), v_[:], reads=[v_], joins=[d["v"]])
        kb.end_phase()

    def phase_attn(self, l):
        kb, d = self.kb, self.d
        kb.begin_phase()
        NQ = T // 128
        NST = 3
        kr = kb.sb("at_kr", [64, T], BF16)
        kb.dma("sp", kr[:], d["kr"][:], reads=[d["kr"]], writes=[kr])
        hd = [dict(kn=kb.sb(f"at_kn{i}", [128, T], BF16), qn=kb.sb(f"at_qn{i}", [128, T], BF16), qr=kb.sb(f"at_qr{i}", [64, T], BF16),
                   v=kb.sb(f"at_v{i}", [128, NQ, 128], BF16), oT=kb.sb(f"at_oT{i}", [128, T], BF16)) for i in range(2)]
        S = [kb.sb(f"at_S{i}", [128, T], F32) for i in range(NST)]
        P = [kb.sb(f"at_P{i}", [128, T], BF16) for i in range(NST)]
        PT = [kb.sb(f"at_PT{i}", [128, NQ, 128], BF16) for i in range(NST)]
        small = [kb.sb(f"at_sm{i}", [128, 4], F32) for i in range(NST)]
        osb = Rot([kb.sb(f"at_osb{i}", [128, 128], BF16) for i in range(3)])
        sc = Rot([kb.ps(f"at_sc{i}", [128, 512]) for i in range(3)])
        ptp = Rot([kb.ps(f"at_pt{i}", [128, 1024], BF16) for i in range(2)])
        ops = Rot([kb.ps(f"at_o{i}", [128, 512]) for i in range(2)])
        otp = kb.ps("at_oTp", [128, 1024], BF16)
        vview = d["v"][:].rearrange("(c p) (h e) -> h p c e", p=128, e=128)

        def load_head(h):
            t_ = hd[h % 2]
            kb.dma("sp", t_["kn"][:], d["kn"][h], reads=[d["kn"]], writes=[t_["kn"]])
            kb.dma("sp", t_["qn"][:], d["qn"][h], reads=[d["qn"]], writes=[t_["qn"]])
            kb.dma("sp", t_["qr"][:], d["qr"][h], reads=[d["qr"]], writes=[t_["qr"]])
            for q4 in range(4):
                kb.dma("sp", t_["v"][:, q4 * 8:(q4 + 1) * 8, :], vview[h][:, q4 * 8:(q4 + 1) * 8, :], reads=[d["v"]], joins=[t_["v"]])

        items = [(h, qi) for h in range(H) for qi in range(NQ)]

        def stage_a(i):
            h, qi = items[i]
            if qi == 0 and h == 0:
                load_head(0)
            if qi == 3 and h + 1 < H:
                load_head(h + 1)
            t_ = hd[h % 2]
            qn_, kn_, qr_ = t_["qn"], t_["kn"], t_["qr"]
            S_ = S[i % NST]
            qs = slice(qi * 128, (qi + 1) * 128)
            nk = (qi + 1) * 128
            nblk = (nk + 511) // 512
            for b in range(nblk):
                k0 = b * 512
                kw = min(512, nk - k0)
                pt = sc.next()
                last = (b == nblk - 1)
                fns = [I("matmul", pt[:, :kw], lhsT=qn_[:, qs], rhs=kn_[:, k0:k0 + kw], start=True, stop=False),
                       I("matmul", pt[:, :kw], lhsT=qr_[:, qs], rhs=kr[:, k0:k0 + kw], start=False, stop=(not last))]
                if last:
                    fns.append(I("matmul", pt[:, kw - 128:kw], lhsT=self.ident_b[:], rhs=self.mask_b[:], start=False, stop=True))
                kb.op("pe", fns, reads=[qn_, kn_, qr_, kr, self.ident_b, self.mask_b], writes=[pt])
                self.copy(self.cp_eng(), S_, S_[:, k0:k0 + kw], pt, pt[:, :kw])

        def stage_b(i):
            h, qi = items[i]
            nk = (qi + 1) * 128
            S_, P_, sm = S[i % NST], P[i % NST], small[i % NST]
            kb.op("dve", I("reduce_max", out=sm[:, 0:1], in_=S_[:, :nk], axis=AX.X), reads=[S_], writes=[sm])
            kb.op("dve", I("tensor_scalar", sm[:, 1:2], sm[:, 0:1], -ATT_SCALE, None, op0=ALU.mult), reads=[sm], writes=[sm])
            kb.op("act", I("activation", out=P_[:, :nk], in_=S_[:, :nk], func=AF.Exp, scale=ATT_SCALE, bias=sm[:, 1:2], accum_out=sm[:, 2:3]),
                  reads=[S_, sm], writes=[P_, sm])

        opsum = {}

        def stage_t(i):
            h, qi = items[i]
            nk = (qi + 1) * 128
            P_, PT_ = P[i % NST], PT[i % NST]
            nc_ = nk // 128
            for g0 in range(0, nc_, 8):
                g1 = min(nc_, g0 + 8)
                tp = ptp.next()
                fns = [I("transpose", tp[:, (c - g0) * 128:(c - g0 + 1) * 128], P_[:, c * 128:(c + 1) * 128], self.ident_b[:]) for c in range(g0, g1)]
                kb.op("pe", fns, reads=[P_, self.ident_b], writes=[tp])
                self.copy(self.cp_eng(), PT_, PT_[:, g0:g1, :], tp, tp[:, 0:(g1 - g0) * 128].rearrange("p (c e) -> p c e", e=128))

        def stage_pv(i):
            h, qi = items[i]
            v_ = hd[h % 2]["v"]
            PT_ = PT[i % NST]
            nc_ = qi + 1
            op_ = ops.next()
            opsum[i] = op_
            fns = [I("matmul", op_[:, 0:128], lhsT=PT_[:, c, :], rhs=v_[:, c, :], start=(c == 0), stop=(c == nc_ - 1)) for c in range(nc_)]
            kb.op("pe", fns, reads=[PT_, v_], writes=[op_])

        def stage_o(i):
            h, qi = items[i]
            oT_ = hd[h % 2]["oT"]
            sm = small[i % NST]
            op_ = opsum.pop(i)
            kb.op("dve", I("reciprocal", out=sm[:, 3:4], in_=sm[:, 2:3]), reads=[sm], writes=[sm])
            os_ = osb.next()
            kb.op("dve", I("tensor_scalar", os_[:], op_[:, 0:128], sm[:, 3:4], None, op0=ALU.mult), reads=[op_, sm], writes=[os_])
            g = qi % 8
            kb.op("pe", I("transpose", otp[:, g * 128:(g + 1) * 128], os_[:], self.ident_b[:]), reads=[os_, self.ident_b], joins=[otp])
            if g == 7:
                q0 = (qi - 7) * 128
                self.copy(self.cp_eng(), oT_, oT_[:, q0:q0 + 1024], otp, otp[:])
            if qi == NQ - 1:
                kb.dma("sp", d["oT"][h], oT_[:], reads=[oT_], joins=[d["oT"]])

        n = len(items)
        for step in range(n + 4):
            if 0 <= step - 2 < n:
                stage_t(step - 2)
            if step < n:
                stage_a(step)
            if 0 <= step - 2 < n:
                stage_pv(step - 2)
            if 0 <= step - 3 < n:
                stage_o(step - 3)
            if 0 <= step - 1 < n:
                stage_b(step - 1)
        kb.end_phase()

    def layernorm(self, pre, sq, ps, mean, rstd, tmp, gcol, bcol, out_f, out_b):
        kb = self.kb
        for c in range(KC):
            kb.op("act", I("activation", out=sq[:, c, :], in_=pre[:, c, :], func=AF.Square), reads=[pre], joins=[sq])
        p1 = ps.next()
        self.mm(p1, p1[:], [(self.ones_f[:], pre[:, c, :]) for c in range(KC)], reads=[self.ones_f, pre])
        p2 = ps.next()
        self.mm(p2, p2[:], [(self.ones_f[:], sq[:, c, :]) for c in range(KC)], reads=[self.ones_f, sq])
        kb.op("act", I("activation", out=mean[:], in_=p1[:], func=AF.Identity, scale=1.0 / D), reads=[p1], writes=[mean])
        kb.op("dve", I("tensor_tensor", out=tmp[:], in0=mean[:], in1=mean[:], op=ALU.mult), reads=[mean], writes=[tmp])
        kb.op("dve", I("scalar_tensor_tensor", out=rstd[:], in0=p2[:], scalar=1.0 / D, in1=tmp[:], op0=ALU.mult, op1=ALU.subtract), reads=[p2, tmp], writes=[rstd])
        kb.op("act", I("activation", out=rstd[:], in_=rstd[:], func=AF.Sqrt, bias=LN_EPS), reads=[rstd], writes=[rstd])
        kb.op("dve", I("reciprocal", out=rstd[:], in_=rstd[:]), reads=[rstd], writes=[rstd])
        for c in range(KC):
            kb.op("dve", I("tensor_tensor", out=sq[:, c, :], in0=pre[:, c, :], in1=mean[:], op=ALU.subtract), reads=[pre, mean], joins=[sq])
            kb.op("pool", I("tensor_tensor", out=sq[:, c, :], in0=sq[:, c, :], in1=rstd[:], op=ALU.mult), reads=[sq, rstd], joins=[sq])
            kb.op("act", I("activation", out=out_f[:, c, :], in_=sq[:, c, :], func=AF.Identity, scale=gcol[:, c:c + 1], bias=bcol[:, c:c + 1]),
                  reads=[sq, self.lncols], joins=[out_f])
            if out_b is not None:
                kb.op("pool", I("tensor_copy", out=out_b[:, c, :], in_=out_f[:, c, :]), reads=[out_f], joins=[out_b])

    def phase_mix_ln1(self, l, kind):
        kb, d = self.kb, self.d
        j_ = l // 2
        kb.begin_phase()
        ps = Rot([kb.ps(f"ml_ps{i}", [128, 512]) for i in range(7)])
        tmp = kb.sb("ml_tmp", [128, 128], F32)
        w_o = kb.sb("ml_wo", [128, KC, D], BF16)
        wsrc = d["mla_w_o"] if kind == "mla" else d["lru_w_out"]
        kb.dma("pool", w_o[:], wsrc[j_].rearrange("(c p) f -> p c f", p=128), reads=[wsrc], writes=[w_o])
        wr = kb.sb("ml_wr", [128, KC, NE], F32)
        kb.dma("sp", wr[:], d["moe_w_router"][l].rearrange("(c p) e -> p c e", p=128), reads=[d["moe_w_router"]], writes=[wr])
        br = kb.sb("ml_br", [1, NE], F32)
        kb.dma("sp", br[:], d["moe_b_router"][l:l + 1, :], reads=[d["moe_b_router"]], writes=[br])
        xa = Rot([kb.sb(f"ml_xa{i}", [128, KC, TT], F32) for i in range(1 if kind == "lru" else 2)])
        yin = Rot([kb.sb(f"ml_yin{i}", [128, KC, TT], BF16) for i in range(1 if kind == "lru" else 2)])
        pres = Rot([kb.sb(f"ml_pre{i}", [128, KC, TT], F32) for i in range(1 if kind == "lru" else 2)])
        sq = kb.sb("ml_sq", [128, KC, TT], F32)
        mean = kb.sb("ml_mean", [128, TT], F32)
        rstd = kb.sb("ml_rstd", [128, TT], F32)
        tmp2 = kb.sb("ml_tmp2", [128, TT], F32)
        x1f = Rot([kb.sb(f"ml_x1f{i}", [128, KC, TT], F32) for i in range(1 if kind == "lru" else 2)])
        x1b = Rot([kb.sb(f"ml_x1b{i}", [128, KC, TT], BF16) for i in range(1 if kind == "lru" else 2)])
        lg = kb.sb("ml_lg", [128, NE], F32)
        ee = kb.sb("ml_ee", [128, NE], F32)
        msk = kb.sb("ml_msk", [128, NE], F32)
        top8 = kb.sb("ml_top8", [128, 8], F32)
        sm = kb.sb("ml_sm", [128, 4], F32)
        mskb = kb.sb("ml_mskb", [128, NE], BF16)
        gts = kb.sb("ml_gts", [128, NE], F32)
        pos = kb.sb("ml_pos", [128, NE], F32)
        okm = kb.sb("ml_okm", [128, NE], F32)
        val = kb.sb("ml_val", [128, NE], F32)
        junk = kb.sb("ml_junk", [128, NE], F32)
        sl4 = kb.sb("ml_sl4", [128, 4], F32)
        x1tok = Rot([kb.sb(f"ml_xtok{i}", [128, 4, D], BF16) for i in range(1 if kind == "lru" else 2)])
        ptb = Rot([kb.ps(f"ml_ptb{i}", [128, 1024], BF16) for i in range(1)])
        kb.op("dve", I("memset", self.cnt[:], 0.0), reads=[], writes=[self.cnt])
        g1 = self.lncols[:, 0 * 32 + l * 8:0 * 32 + l * 8 + 8]
        b1 = self.lncols[:, 1 * 32 + l * 8:1 * 32 + l * 8 + 8]
        xTv = d["xT"][:].rearrange("(c p) t -> p c t", p=128)
        x1Tv = d["x1T"][:].rearrange("(c p) t -> p c t", p=128)
        x1bv = d["x1b"][:].rearrange("(c p) t -> p c t", p=128)
        if kind == "lru":
            L = self.lru_setup(l, ps, tmp)
        for j in range(NT):
            ts = slice(j * TT, (j + 1) * TT)
            xa_ = xa.next()
            kb.dma("sp", xa_[:], xTv[:, :, ts], reads=[d["xT"]], writes=[xa_])
            yin_ = yin.next()
            if kind == "mla":
                kb.dma("sp", yin_[:], d["oT"][:, :, ts].rearrange("h p t -> p h t"), reads=[d["oT"]], writes=[yin_])
            else:
                self.lru_tile(L, j, xa_, yin_, ps)
            pre = pres.next()
            for mc in range(KC):
                pt = ps.next()
                self.mm(pt, pt[:], [(w_o[:, kc, mc * 128:(mc + 1) * 128], yin_[:, kc, :]) for kc in range(KC)], reads=[w_o, yin_])
                kb.op("dve", I("scalar_tensor_tensor", out=pre[:, mc, :], in0=xa_[:, mc, :], scalar=ALPHA, in1=pt[:], op0=ALU.mult, op1=ALU.add),
                      reads=[xa_, pt], joins=[pre])
            x1f_, x1b_ = x1f.next(), x1b.next()
            self.layernorm(pre, sq, ps, mean, rstd, tmp2, g1, b1, x1f_, x1b_)
            kb.dma("sp", x1Tv[:, :, ts], x1f_[:], reads=[x1f_], joins=[d["x1T"]])
            xtok = x1tok.next()
            for b in range(4):
                tp = ptb.next()
                fns = [I("transpose", tp[:, kc * 128:(kc + 1) * 128], x1b_[:, kc, b * 128:(b + 1) * 128], self.ident_b[:]) for kc in range(KC)]
                kb.op("pe", fns, reads=[x1b_, self.ident_b], writes=[tp])
                self.copy(self.cp_eng(), xtok, xtok[:, b, :], tp, tp[:])
            for b in range(4):
                gb = j * 4 + b
                pt = ps.next()
                pairs = [(x1f_[:, kc, b * 128:(b + 1) * 128], wr[:, kc, :]) for kc in range(KC)]
                pairs.append((self.ones_f[0:1, 0:128], br[0:1, :]))
                self.mm(pt, pt[:, 0:NE], pairs, reads=[x1f_, wr, self.ones_f, br])
                self.copy("act", lg, lg[:], pt, pt[:, 0:NE])
                kb.op("dve", I("max", out=top8[:], in_=lg[:]), reads=[lg], writes=[top8])
                kb.op("dve", I("tensor_scalar", msk[:], lg[:], top8[:, 3:4], None, op0=ALU.is_ge), reads=[lg, top8], writes=[msk])
                kb.op("dve", I("tensor_scalar", mskb[:], lg[:], top8[:, 3:4], None, op0=ALU.is_ge), reads=[lg, top8], writes=[mskb])
                kb.op("dve", I("tensor_scalar", sm[:, 0:1], top8[:, 0:1], -1.0, None, op0=ALU.mult), reads=[top8], writes=[sm])
                kb.op("act", I("activation", out=ee[:], in_=lg[:], func=AF.Exp, bias=sm[:, 0:1]), reads=[lg, sm], writes=[ee])
                kb.op("dve", I("tensor_tensor", out=ee[:], in0=ee[:], in1=msk[:], op=ALU.mult), reads=[ee, msk], writes=[ee])
                kb.op("dve", I("reduce_sum", out=sm[:, 1:2], in_=ee[:], axis=AX.X), reads=[ee], writes=[sm])
                kb.op("dve", I("reciprocal", out=sm[:, 2:3], in_=sm[:, 1:2]), reads=[sm], writes=[sm])
                kb.op("dve", I("tensor_scalar", gts[:], ee[:], sm[:, 2:3], None, op0=ALU.mult), reads=[ee, sm], writes=[gts])
                pr = ps.next()
                self.mm(pr, pr[:, 0:NE], [(self.u_b[:], mskb[:])], reads=[self.u_b, mskb])
                pc = ps.next()
                self.mm(pc, pc[:, 0:NE], [(self.ones_b[:], mskb[:])], reads=[self.ones_b, mskb])
                kb.op("dve", I("tensor_tensor", out=pos[:], in0=pr[:, 0:NE], in1=self.cnt[:], op=ALU.add), reads=[pr, self.cnt], writes=[pos])
                kb.op("dve", I("tensor_tensor", out=self.cnt[:], in0=pc[:, 0:NE], in1=self.cnt[:], op=ALU.add), reads=[pc, self.cnt], writes=[self.cnt])
                kb.op("dve", I("tensor_scalar", okm[:], pos[:], float(CAP), None, op0=ALU.is_lt), reads=[pos], writes=[okm])
                kb.op("dve", I("tensor_tensor", out=okm[:], in0=okm[:], in1=msk[:], op=ALU.mult), reads=[okm, msk], writes=[okm])
                kb.op("dve", I("tensor_tensor", out=pos[:], in0=pos[:], in1=self.ebase[:], op=ALU.add), reads=[pos, self.ebase], writes=[pos])
                kb.op("dve", I("tensor_scalar", pos[:], pos[:], -1.0, BIGV, op0=ALU.mult, op1=ALU.add), reads=[pos], writes=[pos])
                kb.op("dve", I("tensor_tensor", out=val[:], in0=pos[:], in1=okm[:], op=ALU.mult), reads=[pos, okm], writes=[val])
                kb.op("dve", I("max", out=top8[:], in_=val[:]), reads=[val], writes=[top8])
                kb.op("dve", I("tensor_scalar", sl4[:], top8[:, 0:4], -1.0, BIGV, op0=ALU.mult, op1=ALU.add), reads=[top8], writes=[sl4])
                kb.op("dve", I("tensor_copy", out=self.slots_all[:, gb, :], in_=sl4[:]), reads=[sl4], joins=[self.slots_all])
                for k in range(4):
                    kb.op("dve", I("scalar_tensor_tensor", out=junk[:], in0=val[:], scalar=top8[:, k:k + 1], in1=gts[:], op0=ALU.is_equal, op1=ALU.mult,
                                   accum_out=self.gk_all[:, gb, k:k + 1]), reads=[val, top8, gts], writes=[junk], joins=[self.gk_all])
                for k in range(4):
                    kb.idma(d["xg"][:, :], bass.IndirectOffsetOnAxis(self.slots_all[:, gb, k:k + 1], 0), xtok[:, b, :], None, NSLOT - 1,
                            reads=[xtok, self.slots_all], joins=[d["xg"]])
        kb.end_phase()

    def lru_setup(self, l, ps, tmp):
        kb, d = self.kb, self.d
        j_ = l // 2
        L = {}
        L["w_in"] = kb.sb("lr_win", [128, KC, 2 * D], BF16)
        kb.dma("pool", L["w_in"][:], d["lru_w_in"][j_].rearrange("(c p) f -> p c f", p=128), reads=[d["lru_w_in"]], writes=[L["w_in"]])
        for nm, src in (("w_a", "lru_w_a"), ("w_x", "lru_w_x")):
            L[nm] = kb.sb("lr_" + nm, [128, 4, 2, 256], BF16)
            kb.dma("pool", L[nm][:], d[src][j_].rearrange("n (c p) f -> p n c f", p=128), reads=[d[src]], writes=[L[nm]])
        cols = L["cols"] = kb.sb("lr_cols", [128, 80], F32)
        self.load_cols(cols, cols[:, 0:32], d["lru_conv_w"], d["lru_conv_w"][j_].rearrange("k (c p) -> (k c) p", p=128), 32, ps.next(), tmp)
        for i, nm in enumerate(("lru_conv_b", "lru_b_a", "lru_b_x", "lru_lambda")):
            self.load_cols(cols, cols[:, 32 + 8 * i:40 + 8 * i], d[nm], d[nm][j_].rearrange("(c p) -> c p", p=128), 8, ps.next(), tmp)
        kb.op("act", I("activation", out=cols[:, 64:72], in_=cols[:, 56:64], func=AF.Exp, scale=-1.0), reads=[cols], writes=[cols])
        kb.op("act", I("activation", out=cols[:, 64:72], in_=cols[:, 64:72], func=AF.Ln, bias=1.0), reads=[cols], writes=[cols])
        kb.op("dve", I("tensor_scalar", cols[:, 64:72], cols[:, 64:72], -8.0, None, op0=ALU.mult), reads=[cols], writes=[cols])
        kb.op("dve", I("tensor_scalar", cols[:, 72:80], cols[:, 64:72], 2.0, None, op0=ALU.mult), reads=[cols], writes=[cols])
        L["halo"] = kb.sb("lr_halo", [128, KC, 4], F32)
        kb.op("dve", I("memset", L["halo"][:], 0.0), reads=[], writes=[L["halo"]])
        L["carry"] = kb.sb("lr_carry", [128, KC], F32)
        kb.op("dve", I("memset", L["carry"][:], 0.0), reads=[], writes=[L["carry"]])
        L["u"] = Rot([kb.sb(f"lr_u{i}", [128, 3 + TT], F32) for i in range(2)])
        L["gate"] = Rot([kb.sb(f"lr_gate{i}", [128, TT], BF16) for i in range(4)])
        L["uc"] = Rot([kb.sb(f"lr_uc{i}", [128, TT], F32) for i in range(4)])
        L["ucb"] = Rot([kb.sb(f"lr_ucb{i}", [128, TT], BF16) for i in range(4)])
        L["a"] = Rot([kb.sb(f"lr_a{i}", [128, TT], F32) for i in range(2)])
        L["a2"] = Rot([kb.sb(f"lr_a2{i}", [128, TT], F32) for i in range(2)])
        L["i"] = Rot([kb.sb(f"lr_i{i}", [128, TT], F32) for i in range(2)])
        L["r"] = Rot([kb.sb(f"lr_r{i}", [128, TT], F32) for i in range(2)])
        L["h"] = Rot([kb.sb(f"lr_h{i}", [128, TT], F32) for i in range(2)])
        L["xb"] = kb.sb("lr_xb", [128, KC, TT], BF16)
        return L

    def lru_tile(self, L, j, xa_, y_out, ps):
        kb = self.kb
        cols, halo, carry, w_in, xb = L["cols"], L["halo"], L["carry"], L["w_in"], L["xb"]
        for c in range(KC):
            self.copy(self.cp_eng(), xb, xb[:, c, :], xa_, xa_[:, c, :])
        chunks = {}

        def s1(n):
            chunk = chunks[n] = {}
            for oc in (2 * n, 2 * n + 1):
                pt = ps.next()
                self.mm(pt, pt[:], [(w_in[:, kc, oc * 128:(oc + 1) * 128], xb[:, kc, :]) for kc in range(KC)], reads=[w_in, xb])
                gate = L["gate"].next()
                kb.op("act", I("activation", out=gate[:], in_=pt[:], func=AF.Gelu_apprx_tanh), reads=[pt], writes=[gate])
                pt2 = ps.next()
                self.mm(pt2, pt2[:], [(w_in[:, kc, D + oc * 128:D + (oc + 1) * 128], xb[:, kc, :]) for kc in range(KC)], reads=[w_in, xb])
                u = L["u"].next()
                kb.op("dve", I("tensor_copy", out=u[:, 0:3], in_=halo[:, oc, 0:3]), reads=[halo], writes=[u])
                kb.op("act", I("activation", out=u[:, 3:3 + TT], in_=pt2[:], func=AF.Identity), reads=[pt2], joins=[u])
                kb.op("dve", I("tensor_copy", out=halo[:, oc, 0:3], in_=u[:, TT:TT + 3]), reads=[u], writes=[halo])
                uc = L["uc"].next()
                ucb = L["ucb"].next()
                kb.op("dve", I("tensor_scalar", uc[:], u[:, 0:TT], cols[:, oc:oc + 1], cols[:, 32 + oc:33 + oc], op0=ALU.mult, op1=ALU.add),
                      reads=[u, cols], writes=[uc])
                for k in range(1, 4):
                    kb.op("dve", I("scalar_tensor_tensor", out=uc[:], in0=u[:, k:k + TT], scalar=cols[:, k * 8 + oc:k * 8 + oc + 1], in1=uc[:], op0=ALU.mult, op1=ALU.add),
                          reads=[u, cols, uc], writes=[uc])
                kb.op("pool", I("tensor_copy", out=ucb[:], in_=uc[:]), reads=[uc], writes=[ucb])
                chunk[oc] = (gate, uc, ucb)

        def s2(n):
            chunk = chunks.pop(n)
            for oc in (2 * n, 2 * n + 1):
                co = (oc % 2) * 128
                a, a2, ii, rr = L["a"].next(), L["a2"].next(), L["i"].next(), L["r"].next()
                gate, uc, _ = chunk[oc]
                ucbs = [chunk[2 * n][2], chunk[2 * n + 1][2]]
                pa = ps.next()
                self.mm(pa, pa[:], [(L["w_a"][:, n, kc, co:co + 128], ucbs[kc][:]) for kc in range(2)], reads=[L["w_a"]] + ucbs)
                px = ps.next()
                self.mm(px, px[:], [(L["w_x"][:, n, kc, co:co + 128], ucbs[kc][:]) for kc in range(2)], reads=[L["w_x"]] + ucbs)
                kb.op("act", I("activation", out=rr[:], in_=pa[:], func=AF.Sigmoid, bias=cols[:, 40 + oc:41 + oc]), reads=[pa, cols], writes=[rr])
                kb.op("act", I("activation", out=ii[:], in_=px[:], func=AF.Sigmoid, bias=cols[:, 48 + oc:49 + oc]), reads=[px, cols], writes=[ii])
                kb.op("act", I("activation", out=a[:], in_=rr[:], func=AF.Exp, scale=cols[:, 64 + oc:65 + oc]), reads=[rr, cols], writes=[a])
                kb.op("act", I("activation", out=a2[:], in_=rr[:], func=AF.Exp, scale=cols[:, 72 + oc:73 + oc]), reads=[rr, cols], writes=[a2])
                kb.op("dve", I("tensor_scalar", a2[:], a2[:], -1.0, 1.0, op0=ALU.mult, op1=ALU.add), reads=[a2], writes=[a2])
                kb.op("act", I("activation", out=a2[:], in_=a2[:], func=AF.Sqrt), reads=[a2], writes=[a2])
                kb.op("dve", I("tensor_tensor", out=ii[:], in0=ii[:], in1=uc[:], op=ALU.mult), reads=[ii, uc], writes=[ii])
                kb.op("dve", I("tensor_tensor", out=ii[:], in0=ii[:], in1=a2[:], op=ALU.mult), reads=[ii, a2], writes=[ii])
                h = L["h"].next()
                kb.op("dve", I("tensor_tensor_scan", out=h[:], data0=a[:], data1=ii[:], initial=carry[:, oc:oc + 1], op0=ALU.mult, op1=ALU.add),
                      reads=[a, ii, carry], writes=[h])
                kb.op("dve", I("tensor_copy", out=carry[:, oc:oc + 1], in_=h[:, TT - 1:TT]), reads=[h], writes=[carry])
                kb.op("dve", I("tensor_tensor", out=y_out[:, oc, :], in0=h[:], in1=gate[:], op=ALU.mult), reads=[h, gate], joins=[y_out])

        s1(0)
        for n in range(4):
            if n + 1 < 4:
                s1(n + 1)
            s2(n)

    def phase_moe(self, l):
        kb, d = self.kb, self.d
        kb.begin_phase()
        NBLK = CAP // 128
        PARTS = ((0, 512), (512, CAP - 512))
        ps = Rot([kb.ps(f"mo_ps{i}", [128, 512]) for i in range(5)])
        ptb = Rot([kb.ps(f"mo_ptb{i}", [128, 1024], BF16) for i in range(3)])
        tmp = kb.sb("mo_tmp", [128, 128], F32)
        wu = Rot([kb.sb(f"mo_wu{i}", [128, KC, 2 * DFF], BF16) for i in range(2)])
        wd = Rot([kb.sb(f"mo_wd{i}", [128, KC, D], BF16) for i in range(2)])
        bdb = Rot([kb.sb(f"mo_bd{i}", [128, D], F32) for i in range(2)])
        bup = kb.sb("mo_bup", [128, NE * 16], F32)
        for e4 in range(0, NE, 8):
            self.load_cols(bup, bup[:, e4 * 16:(e4 + 8) * 16], d["moe_b_up"],
                           d["moe_b_up"][l, e4:e4 + 8, :].rearrange("e (c p) -> (e c) p", p=128), 128, ps.next(), tmp)
        xg = Rot([kb.sb(f"mo_xg{i}", [128, NBLK, D], BF16) for i in range(2)])
        xgT = kb.sb("mo_xgT", [128, KC, CAP], BF16)
        hT = kb.sb("mo_hT", [128, KC, CAP], BF16)
        yst = Rot([kb.sb(f"mo_ys{i}", [128, NBLK, D], BF16) for i in range(2)])
        g_ = Rot([kb.sb(f"mo_g{i}", [128, CAP], F32) for i in range(2)])
        sg_ = Rot([kb.sb(f"mo_sg{i}", [128, CAP], F32) for i in range(2)])
        u_ = Rot([kb.sb(f"mo_u{i}", [128, CAP], F32) for i in range(2)])
        bufs = {}

        def load(ex):
            wu_, wd_, bd_, xg_ = wu.next(), wd.next(), bdb.next(), xg.next()
            bufs[ex] = (wu_, wd_, bd_, xg_)
            rows = slice(ex * CAP, (ex + 1) * CAP)
            kb.dma("sp", xg_[:], d["xg"][rows, :].rearrange("(b p) f -> p b f", p=128), reads=[d["xg"]], writes=[xg_])
            kb.dma("sp", bd_[:], d["moe_b_down"][l, ex:ex + 1, :].partition_broadcast(128), reads=[d["moe_b_down"]], writes=[bd_])
            for q4 in range(4):
                kb.dma("pool", wu_[:, 2 * q4:2 * q4 + 2, :], d["moe_w_up"][l, ex, q4 * 256:(q4 + 1) * 256, :].rearrange("(c p) f -> p c f", p=128),
                       reads=[d["moe_w_up"]], joins=[wu_])
            for q2 in range(2):
                kb.dma("pool", wd_[:, 4 * q2:4 * q2 + 4, :], d["moe_w_down"][l, ex, q2 * 512:(q2 + 1) * 512, :].rearrange("(c p) f -> p c f", p=128),
                       reads=[d["moe_w_down"]], joins=[wd_])

        def transposes(ex):
            xg_ = bufs[ex][3]
            for kc in range(KC):
                tp = ptb.next()
                fns = [I("transpose", tp[:, b * 128:(b + 1) * 128], xg_[:, b, kc * 128:(kc + 1) * 128], self.ident_b[:]) for b in range(NBLK)]
                kb.op("pe", fns, reads=[xg_, self.ident_b], writes=[tp])
                self.copy(self.cp_eng(), xgT, xgT[:, kc, :], tp, tp[:, 0:CAP])

        def up(ex):
            wu_ = bufs[ex][0]
            for fc in range(KC):
                gg, sg, uu = g_.next(), sg_.next(), u_.next()
                cg = ex * 16 + fc
                cu = ex * 16 + 8 + fc
                for (s0, sw) in PARTS:
                    pg = ps.next()
                    self.mm(pg, pg[:, 0:sw], [(wu_[:, kc, fc * 128:(fc + 1) * 128], xgT[:, kc, s0:s0 + sw]) for kc in range(KC)], reads=[wu_, xgT])
                    kb.op("dve", I("tensor_scalar", gg[:, s0:s0 + sw], pg[:, 0:sw], bup[:, cg:cg + 1], 7.0, op0=ALU.add, op1=ALU.min), reads=[pg, bup], joins=[gg])
                    pu = ps.next()
                    self.mm(pu, pu[:, 0:sw], [(wu_[:, kc, DFF + fc * 128:DFF + (fc + 1) * 128], xgT[:, kc, s0:s0 + sw]) for kc in range(KC)], reads=[wu_, xgT])
                    kb.op("act", I("activation", out=uu[:, s0:s0 + sw], in_=pu[:, 0:sw], func=AF.Identity, bias=bup[:, cu:cu + 1]), reads=[pu, bup], joins=[uu])
                kb.op("act", I("activation", out=sg[:], in_=gg[:], func=AF.Sigmoid, scale=1.702), reads=[gg], writes=[sg])
                kb.op("pool", I("tensor_scalar", uu[:], uu[:], 7.0, -7.0, op0=ALU.min, op1=ALU.max), reads=[uu], writes=[uu])
                kb.op("pool", I("tensor_tensor", out=gg[:], in0=gg[:], in1=sg[:], op=ALU.mult), reads=[gg, sg], writes=[gg])
                kb.op("dve", I("scalar_tensor_tensor", out=hT[:, fc, :], in0=uu[:], scalar=1.0, in1=gg[:], op0=ALU.add, op1=ALU.mult), reads=[uu, gg], joins=[hT])

        def down(ex):
            _, wd_, bd_, _ = bufs[ex]
            rows = slice(ex * CAP, (ex + 1) * CAP)
            ys_ = yst.next()
            for b in range(NBLK):
                for hf in range(2):
                    pt = ps.next()
                    pairs = [(hT[:, fc, b * 128:(b + 1) * 128], wd_[:, fc, hf * 512:(hf + 1) * 512]) for fc in range(KC)]
                    self.mm(pt, pt[:], pairs, reads=[hT, wd_])
                    kb.op("dve", I("scalar_tensor_tensor", out=ys_[:, b, hf * 512:(hf + 1) * 512], in0=pt[:], scalar=1.0, in1=bd_[:, hf * 512:(hf + 1) * 512],
                                   op0=ALU.mult, op1=ALU.add), reads=[pt, bd_], joins=[ys_])
            kb.dma("sp", d["ys"][rows, :].rearrange("(b p) f -> p b f", p=128), ys_[:], reads=[ys_], joins=[d["ys"]])

        load(0)
        transposes(0)
        for ex in range(NE):
            if ex + 1 < NE:
                load(ex + 1)
            up(ex)
            if ex + 1 < NE:
                transposes(ex + 1)
            down(ex)
        kb.end_phase()

    def phase_combine(self, l):
        kb, d = self.kb, self.d
        kb.begin_phase()
        rows = [Rot([kb.sb(f"cb_r{k}_{i}", [128, D], BF16) for i in range(3)]) for k in range(4)]
        for k in range(4):
            for t_ in rows[k].tiles:
                kb.op("dve", I("memset", t_[:], 0.0), reads=[], writes=[t_])
        yt = Rot([kb.sb(f"cb_y{i}", [128, 4, D], F32) for i in range(2)])
        for j in range(NT):
            ts = slice(j * TT, (j + 1) * TT)
            yt_ = yt.next()
            for b in range(4):
                gb = j * 4 + b
                rk = [rows[k].next() for k in range(4)]
                for k in range(4):
                    kb.idma(rk[k][:], None, d["ys"][:, :], bass.IndirectOffsetOnAxis(self.slots_all[:, gb, k:k + 1], 0), NSLOT - 1,
                            reads=[d["ys"], self.slots_all], writes=[rk[k]])
                kb.op("dve", I("tensor_scalar", yt_[:, b, :], rk[0][:], self.gk_all[:, gb, 0:1], None, op0=ALU.mult), reads=[rk[0], self.gk_all], joins=[yt_])
                for k in range(1, 4):
                    kb.op("dve", I("scalar_tensor_tensor", out=yt_[:, b, :], in0=rk[k][:], scalar=self.gk_all[:, gb, k:k + 1], in1=yt_[:, b, :], op0=ALU.mult, op1=ALU.add),
                          reads=[rk[k], self.gk_all, yt_], joins=[yt_])
            kb.dma("sp", d["y"][ts, :].rearrange("(b p) f -> p b f", p=128), yt_[:], reads=[yt_], joins=[d["y"]])
        kb.end_phase()

    def phase_ln2_ple(self, l, last):
        kb, d = self.kb, self.d
        kb.begin_phase()
        ps = Rot([kb.ps(f"lp_ps{i}", [128, 512]) for i in range(7)])
        wg = kb.sb("lp_wg", [128, KC, D], BF16)
        kb.dma("pool", wg[:], d["ple_w_gate"][l].rearrange("(c p) f -> p c f", p=128), reads=[d["ple_w_gate"]], writes=[wg])
        wp = kb.sb("lp_wp", [128, 2, D], BF16)
        kb.dma("pool", wp[:], d["ple_w_proj"][l].rearrange("(c p) f -> p c f", p=128), reads=[d["ple_w_proj"]], writes=[wp])
        yt = Rot([kb.sb(f"lp_yt{i}", [128, 4, D], F32) for i in range(2)])
        x1f = Rot([kb.sb(f"lp_x1f{i}", [128, KC, TT], F32) for i in range(1)])
        pin = Rot([kb.sb(f"lp_pin{i}", [128, 4, PLE], F32) for i in range(2)])
        pTb = kb.sb("lp_pTb", [128, 2, TT], BF16)
        pres = Rot([kb.sb(f"lp_pre{i}", [128, KC, TT], F32) for i in range(2)])
        sq = kb.sb("lp_sq", [128, KC, TT], F32)
        mean = kb.sb("lp_mean", [128, TT], F32)
        rstd = kb.sb("lp_rstd", [128, TT], F32)
        tmp2 = kb.sb("lp_tmp2", [128, TT], F32)
        x2f = kb.sb("lp_x2f", [128, KC, TT], F32)
        x2b = kb.sb("lp_x2b", [128, KC, TT], BF16)
        sgm = Rot([kb.sb(f"lp_sg{i}", [128, TT], F32) for i in range(2)])
        xo = Rot([kb.sb(f"lp_xo{i}", [128, KC, TT], F32) for i in range(1)])
        otok = Rot([kb.sb(f"lp_ot{i}", [128, 4, D], F32) for i in range(1)]) if last else None
        g2 = self.lncols[:, 2 * 32 + l * 8:2 * 32 + l * 8 + 8]
        b2 = self.lncols[:, 3 * 32 + l * 8:3 * 32 + l * 8 + 8]
        x1Tv = d["x1T"][:].rearrange("(c p) t -> p c t", p=128)
        xTv = d["xT"][:].rearrange("(c p) t -> p c t", p=128)
        for j in range(NT):
            ts = slice(j * TT, (j + 1) * TT)
            yt_, x1f_, pin_ = yt.next(), x1f.next(), pin.next()
            kb.dma("sp", yt_[:], d["y"][ts, :].rearrange("(b p) f -> p b f", p=128), reads=[d["y"]], writes=[yt_])
            kb.dma("sp", x1f_[:], x1Tv[:, :, ts], reads=[d["x1T"]], writes=[x1f_])
            kb.dma("sp", pin_[:], d["p"][l, ts, :].rearrange("(b p) f -> p b f", p=128), reads=[d["p"]], writes=[pin_])
            for c in range(2):
                pt = ps.next()
                fns = [I("transpose", pt[:, b * 128:(b + 1) * 128], pin_[:, b, c * 128:(c + 1) * 128], self.ident_f[:]) for b in range(4)]
                kb.op("pe", fns, reads=[pin_, self.ident_f], writes=[pt])
                self.copy(self.cp_eng(), pTb, pTb[:, c, :], pt, pt[:])
            pre = pres.next()
            for mc in range(KC):
                pt = ps.next()
                fns = [I("transpose", pt[:, b * 128:(b + 1) * 128], yt_[:, b, mc * 128:(mc + 1) * 128], self.ident_f[:]) for b in range(4)]
                kb.op("pe", fns, reads=[yt_, self.ident_f], writes=[pt])
                kb.op("dve", I("scalar_tensor_tensor", out=pre[:, mc, :], in0=x1f_[:, mc, :], scalar=ALPHA, in1=pt[:], op0=ALU.mult, op1=ALU.add),
                      reads=[x1f_, pt], joins=[pre])
            self.layernorm(pre, sq, ps, mean, rstd, tmp2, g2, b2, x2f, x2b)
            xo_ = xo.next()
            for mc in range(KC):
                pg = ps.next()
                self.mm(pg, pg[:], [(wg[:, kc, mc * 128:(mc + 1) * 128], x2b[:, kc, :]) for kc in range(KC)], reads=[wg, x2b])
                pp = ps.next()
                self.mm(pp, pp[:], [(wp[:, kc, mc * 128:(mc + 1) * 128], pTb[:, kc, :]) for kc in range(2)], reads=[wp, pTb])
                sg = sgm.next()
                kb.op("act", I("activation", out=sg[:], in_=pg[:], func=AF.Sigmoid), reads=[pg], writes=[sg])
                kb.op("dve", I("tensor_tensor", out=sg[:], in0=sg[:], in1=pp[:], op=ALU.mult), reads=[sg, pp], writes=[sg])
                kb.op("pool", I("tensor_tensor", out=xo_[:, mc, :], in0=x2f[:, mc, :], in1=sg[:], op=ALU.add), reads=[x2f, sg], joins=[xo_])
            if not last:
                kb.dma("sp", xTv[:, :, ts], xo_[:], reads=[xo_], joins=[d["xT"]])
            else:
                if "xT" in self.debug:
                    kb.dma("sp", xTv[:, :, ts], xo_[:], reads=[xo_], joins=[d["xT"]])
                ot = otok.next()
                for b in range(4):
                    for hf in range(2):
                        pt = ps.next()
                        fns = [I("transpose", pt[:, c4 * 128:(c4 + 1) * 128], xo_[:, hf * 4 + c4, b * 128:(b + 1) * 128], self.ident_f[:]) for c4 in range(4)]
                        kb.op("pe", fns, reads=[xo_, self.ident_f], writes=[pt])
                        self.copy(self.cp_eng(), ot, ot[:, b, hf * 512:(hf + 1) * 512], pt, pt[:])
                kb.dma("sp", d["out"][ts, :].rearrange("(b p) f -> p b f", p=128), ot[:], reads=[ot], joins=[d["out"]])
        kb.end_phase()


def make_consts():
    bf = ml_dtypes.bfloat16
    c = {}
    c["c_ident_f"] = np.eye(128, dtype=np.float32)
    c["c_ident_b"] = np.eye(128, dtype=np.float32).astype(bf)
    c["c_ones_f"] = np.ones((128, 128), np.float32)
    q = np.arange(128)[:, None]
    k = np.arange(128)[None, :]
    c["c_mask_b"] = np.where(k <= q, 0.0, MASK_NEG).astype(np.float32).astype(bf)
    rt = np.zeros((64, 128), np.float32)
    for m in range(32):
        rt[m + 32, m] = -1.0
        rt[m, m + 32] = 1.0
    c["c_rt_b"] = rt.astype(bf)
    c["c_u_b"] = (np.arange(128)[:, None] < np.arange(128)[None, :]).astype(np.float32).astype(bf)
    c["c_ones_b"] = np.ones((128, 128), np.float32).astype(bf)
    c["c_ebase"] = np.tile((np.arange(NE, dtype=np.float32) * CAP)[None, :], (128, 1)).astype(np.float32)
    half = 32
    invf = np.exp(-math.log(10000.0) * np.arange(half, dtype=np.float32) / half).astype(np.float32)
    c["c_invf"] = np.concatenate([invf, invf]).reshape(64, 1).astype(np.float32)
    return c


_PROG_CACHE = {}


def get_prog(n_layers=DEPTH, debug=()):
    key = (n_layers, tuple(sorted(debug)))
    if key not in _PROG_CACHE:
        _PROG_CACHE[key] = Prog(n_layers, debug)
    return _PROG_CACHE[key]


WEIGHT_NAMES = ["mla_w_in", "mla_q_norm", "mla_kv_norm", "mla_w_uq", "mla_w_ukv", "mla_w_o",
                "lru_w_in", "lru_conv_w", "lru_conv_b", "lru_w_a", "lru_b_a", "lru_w_x", "lru_b_x", "lru_lambda", "lru_w_out",
                "ln1_g", "ln1_b", "ln2_g", "ln2_b", "moe_w_router", "moe_b_router", "moe_w_up", "moe_b_up",
                "moe_w_down", "moe_b_down", "ple_w_gate", "ple_w_proj"]


def make_in_map(inputs, b, consts, prog, shared=None):
    m = {}
    for n in prog.used_inputs:
        shape = prog.in_shapes[n][0]
        if n == "x":
            m[n] = np.ascontiguousarray(np.asarray(inputs["x"])[b], dtype=np.float32)
        elif n == "p":
            m[n] = np.ascontiguousarray(np.asarray(inputs["p"])[:shape[0], b], dtype=np.float32)
        elif n == "positions":
            m[n] = np.ascontiguousarray(np.asarray(inputs["positions"])[b:b + 1], dtype=np.int32)
        elif n.startswith("c_"):
            m[n] = consts[n]
        else:
            if shared is not None and n in shared:
                m[n] = shared[n]
            else:
                a = np.ascontiguousarray(np.asarray(inputs[n])[:shape[0]], dtype=np.float32)
                if shared is not None:
                    shared[n] = a
                m[n] = a
    return m


def kernel(**inputs):
    prog = get_prog()
    consts = make_consts()
    nb = np.asarray(inputs["x"]).shape[0]
    shared = {}
    in_maps = [make_in_map(inputs, b, consts, prog, shared) for b in range(nb)]
    res = run_bass_kernel_spmd(prog.nc, in_maps, core_ids=list(range(nb)))
    return np.stack([np.asarray(r["out"], dtype=np.float32) for r in res.results], axis=0)
```

```python
from contextlib import ExitStack
import math
import os
STOP = int(os.environ.get('K_STOP', '0'))
import numpy as np
import ml_dtypes
import concourse.bass as bass
import concourse.mybir as mybir
from concourse.bass_utils import run_bass_kernel_spmd

F32 = mybir.dt.float32
BF16 = mybir.dt.bfloat16
I32 = mybir.dt.int32
AF = mybir.ActivationFunctionType
ALU = mybir.AluOpType
AX = mybir.AxisListType

D = 1024
T = 4096
DEPTH = 4
TT = 512
NT = T // TT
KC = D // 128
H = 8
QL, KVL, RD = 768, 256, 64
NE = 32
DFF = 1024
PLE = 256
ALPHA = (2.0 * DEPTH) ** 0.25
LN_EPS = 1e-5
RMS_EPS = 1e-6
ATT_SCALE = 1.0 / math.sqrt(192.0)
MASK_NEG = -30000.0
TWO_PI = 2.0 * math.pi
CAP = 768
NSLOT = NE * CAP
BIGV = 65536.0
U32 = mybir.dt.uint32


class Res:
    __slots__ = ("w", "rs", "name")

    def __init__(self, name=""):
        self.w = []
        self.rs = []
        self.name = name


class TL:
    def __init__(self, h, name=""):
        self.h = h
        self.res = Res(name)

    def __getitem__(self, idx):
        return self.h[idx]


N_DMA_SEMS = 12


class KB:
    ENGS = ("pe", "act", "dve", "pool", "sp")

    def __init__(self, nc):
        self.nc = nc
        self.es = ExitStack()
        self.q = {e: [] for e in self.ENGS}
        self.cnt = {e: 0 for e in self.ENGS}
        self.sem = {e: self.es.enter_context(nc.semaphore("s_" + e)) for e in self.ENGS}
        self.dsem, self.dval, self.dnext = {}, {}, {}
        for qn in ("sp", "pool", "act"):
            self.dsem[qn] = [self.es.enter_context(nc.semaphore(f"d_{qn}{i}")) for i in range(N_DMA_SEMS)]
            self.dval[qn] = [0] * N_DMA_SEMS
            self.dnext[qn] = 0
        self.waited = {e: {} for e in self.ENGS}
        self.semobj = {}
        self.n_ops = 0
        self.phase_stack = None

    def begin_phase(self):
        self.phase_stack = ExitStack()
        if getattr(self, "sb_base", None) is None:
            self.sb_base = (self.nc._sbuf_addr_for_side(None) + 63) // 64 * 64
        self.sb_ptr = self.sb_base

    def end_phase(self):
        self.barrier()
        self.phase_stack.close()
        self.phase_stack = None

    def _stack(self):
        return self.phase_stack if self.phase_stack is not None else self.es

    def _nm(self, name):
        self.uid = getattr(self, "uid", 0) + 1
        return f"{name}_{self.uid}"

    def sb(self, name, shape, dt):
        name = self._nm(name)
        if self.phase_stack is not None:
            nbytes = int(np.prod(shape[1:])) * (2 if dt == BF16 else 4)
            off = (self.sb_ptr + 63) // 64 * 64
            self.sb_ptr = off + nbytes
            assert self.sb_ptr <= 229344, f"SBUF phase arena overflow at {name}: {self.sb_ptr}"
            return TL(self.nc.alloc_sbuf_tensor_at(name, list(shape), dt, offset=off), name)
        return TL(self._stack().enter_context(self.nc.sbuf_tensor(name, list(shape), dt)), name)

    def ps(self, name, shape, dt=F32):
        name = self._nm(name)
        return TL(self._stack().enter_context(self.nc.psum_tensor(name, list(shape), dt)), name)

    def dram(self, name, shape, dt, kind="Internal"):
        return TL(self.nc.dram_tensor(name, list(shape), dt, kind=kind), name)

    @staticmethod
    def _r(x):
        return x.res if isinstance(x, TL) else x

    @staticmethod
    def _compact(lst):
        best = {}
        for (s, v) in lst:
            k = id(s)
            if k not in best or best[k][1] < v:
                best[k] = (s, v)
        return list(best.values())

    def _deps(self, eng, reads, writes, joins=()):
        need = {}
        own = self.sem["pe"] if eng == "pe" else None

        def add(tok):
            s, v = tok
            if s is own:
                return
            k = id(s)
            self.semobj[k] = s
            if need.get(k, 0) < v:
                need[k] = v

        for r in reads:
            for t in self._r(r).w:
                add(t)
        for w in writes:
            rr = self._r(w)
            for t in rr.w:
                add(t)
            for t in rr.rs:
                add(t)
        for w in joins:
            rr = self._r(w)
            for t in rr.rs:
                add(t)
        out = []
        wd = self.waited[eng]
        for k, v in need.items():
            if wd.get(k, 0) < v:
                wd[k] = v
                out.append((self.semobj[k], v))
        return out

    def _commit(self, tok, reads, writes, joins=()):
        for r in reads:
            rr = self._r(r)
            rr.rs.append(tok)
            if len(rr.rs) > 48:
                rr.rs = self._compact(rr.rs)
        for w in writes:
            rr = self._r(w)
            rr.w = [tok]
            rr.rs = []
        for w in joins:
            rr = self._r(w)
            rr.w.append(tok)
            if len(rr.w) > 48:
                rr.w = self._compact(rr.w)

    def op(self, eng, fn, reads=(), writes=(), joins=()):
        waits = self._deps(eng, reads, writes, joins)
        self.cnt[eng] += 1
        tok = (self.sem[eng], self.cnt[eng])
        self.q[eng].append((waits, fn, (self.sem[eng], 1)))
        self._commit(tok, reads, writes, joins)
        self.n_ops += 1
        return tok

    def dma(self, qn, out_ap, in_ap, reads=(), writes=(), joins=()):
        waits = self._deps(qn, reads, writes, joins)
        i = self.dnext[qn]
        self.dnext[qn] = (i + 1) % N_DMA_SEMS
        s = self.dsem[qn][i]
        prev = self.dval[qn][i]
        self.semobj[id(s)] = s
        if prev > 0:
            wd = self.waited[qn]
            if wd.get(id(s), 0) < prev:
                wd[id(s)] = prev
                waits.append((s, prev))
        self.dval[qn][i] = prev + 16
        tok = (s, prev + 16)

        def fn(e, out_ap=out_ap, in_ap=in_ap):
            return e.dma_start(out=out_ap, in_=in_ap)

        self.q[qn].append((waits, fn, (s, 16)))
        self._commit(tok, reads, writes, joins)
        self.n_ops += 1
        return tok

    def idma(self, out_ap, out_off, in_ap, in_off, bounds, reads=(), writes=(), joins=()):
        qn = "pool"
        waits = self._deps(qn, reads, writes, joins)
        i = self.dnext[qn]
        self.dnext[qn] = (i + 1) % N_DMA_SEMS
        s = self.dsem[qn][i]
        prev = self.dval[qn][i]
        self.semobj[id(s)] = s
        if prev > 0:
            wd = self.waited[qn]
            if wd.get(id(s), 0) < prev:
                wd[id(s)] = prev
                waits.append((s, prev))
        self.dval[qn][i] = prev + 16
        tok = (s, prev + 16)

        def fn(e):
            if getattr(self, "_breg", None) is None:
                self._breg = e.to_reg(bounds)
            return e.indirect_dma_start(out=out_ap, out_offset=out_off, in_=in_ap, in_offset=in_off,
                                        bounds_check=self._breg, oob_is_err=False)

        self.q[qn].append((waits, fn, (s, 16)))
        self._commit(tok, reads, writes, joins)
        self.n_ops += 1
        return tok

    def barrier(self):
        toks = [(self.sem[e], self.cnt[e]) for e in self.ENGS if self.cnt[e] > 0]
        for qn in self.dsem:
            for s, v in zip(self.dsem[qn], self.dval[qn]):
                if v > 0:
                    toks.append((s, v))
        for e in self.ENGS:
            waits = []
            wd = self.waited[e]
            for (s, v) in toks:
                if s is self.sem[e]:
                    continue
                if wd.get(id(s), 0) < v:
                    wd[id(s)] = v
                    waits.append((s, v))
            if waits:
                self.q[e].append((waits, None, None))

    def emit(self):
        q = self.q

        def run(e, lst):
            for waits, fn, inc in lst:
                for (s, v) in waits:
                    e.wait_ge(s, v)
                if fn is None:
                    continue
                if isinstance(fn, (list, tuple)):
                    ins = None
                    for f in fn:
                        ins = f(e)
                else:
                    ins = fn(e)
                ins.then_inc(inc[0], inc[1])

        with self.nc.Block() as block:
            @block.tensor
            def _(e):
                run(e, q["pe"])

            @block.scalar
            def _(e):
                run(e, q["act"])

            @block.vector
            def _(e):
                run(e, q["dve"])

            @block.gpsimd
            def _(e):
                run(e, q["pool"])

            @block.sync
            def _(e):
                run(e, q["sp"])

    def close(self):
        self.es.close()


def I(name, *a, **k):
    return lambda e: getattr(e, name)(*a, **k)


class Rot:
    def __init__(self, tiles):
        self.tiles = tiles
        self.i = 0

    def next(self):
        t = self.tiles[self.i]
        self.i = (self.i + 1) % len(self.tiles)
        return t


class Prog:
    def __init__(self, n_layers=DEPTH, debug=(), phases=None):
        self.n_layers = n_layers
        self.debug = set(debug)
        self.phases = phases
        nc = bass.Bass("TRN2", target_bir_lowering=False)
        self.nc = nc
        kb = self.kb = KB(nc)
        IN = "ExternalInput"
        NL = n_layers
        na = max(1, (NL + 1) // 2)
        nl = max(1, NL // 2)
        self.in_shapes = {
            "x": ([T, D], F32), "p": ([NL, T, PLE], F32), "positions": ([1, T], I32),
            "mla_w_in": ([na, D, QL + KVL + RD], F32), "mla_q_norm": ([na, QL], F32), "mla_kv_norm": ([na, KVL], F32),
            "mla_w_uq": ([na, QL, H * 192], F32), "mla_w_ukv": ([na, KVL, H * 256], F32), "mla_w_o": ([na, D, D], F32),
            "lru_w_in": ([nl, D, 2 * D], F32), "lru_conv_w": ([nl, 4, D], F32), "lru_conv_b": ([nl, D], F32),
            "lru_w_a": ([nl, 4, 256, 256], F32), "lru_b_a": ([nl, D], F32), "lru_w_x": ([nl, 4, 256, 256], F32),
            "lru_b_x": ([nl, D], F32), "lru_lambda": ([nl, D], F32), "lru_w_out": ([nl, D, D], F32),
            "ln1_g": ([NL, D], F32), "ln1_b": ([NL, D], F32), "ln2_g": ([NL, D], F32), "ln2_b": ([NL, D], F32),
            "moe_w_router": ([NL, D, NE], F32), "moe_b_router": ([NL, NE], F32),
            "moe_w_up": ([NL, NE, D, 2 * DFF], F32), "moe_b_up": ([NL, NE, 2 * DFF], F32),
            "moe_w_down": ([NL, NE, DFF, D], F32), "moe_b_down": ([NL, NE, D], F32),
            "ple_w_gate": ([NL, D, D], F32), "ple_w_proj": ([NL, PLE, D], F32),
            "c_ident_f": ([128, 128], F32), "c_ident_b": ([128, 128], BF16), "c_ones_f": ([128, 128], F32),
            "c_mask_b": ([128, 128], BF16), "c_rt_b": ([64, 128], BF16), "c_invf": ([64, 1], F32),
            "c_u_b": ([128, 128], BF16), "c_ones_b": ([128, 128], BF16), "c_ebase": ([128, NE], F32),
        }
        self.used_inputs = []

        class LazyD(dict):
            def __missing__(dself, name):
                shape, dt = self.in_shapes[name]
                t = kb.dram(name, shape, dt, kind=IN)
                dself[name] = t
                self.used_inputs.append(name)
                return t

        d = self.d = LazyD()

        def scr(name, shape, dt=F32):
            kind = "ExternalOutput" if name in self.debug else "Internal"
            d[name] = kb.dram(name, shape, dt, kind=kind)

        scr("xT", [D, T]); scr("x1T", [D, T]); scr("x1b", [D, T], BF16)
        scr("cs", [2, 64, T])
        scr("qn", [H, 128, T], BF16); scr("qr", [H, 64, T], BF16); scr("kn", [H, 128, T], BF16)
        scr("kr", [64, T], BF16); scr("v", [T, H * 128], BF16); scr("oT", [H, 128, T], BF16)
        scr("y", [T, D]); scr("xg", [NSLOT + 128, D], BF16); scr("ys", [NSLOT + 128, D], BF16)
        d["out"] = kb.dram("out", [T, D], F32, kind="ExternalOutput")
        self.ln_nl = NL

        self.ident_f = kb.sb("ident_f", [128, 128], F32)
        self.ident_b = kb.sb("ident_b", [128, 128], BF16)
        self.ones_f = kb.sb("ones_f", [128, 128], F32)
        self.mask_b = kb.sb("mask_b", [128, 128], BF16)
        self.rt_b = kb.sb("rt_b", [64, 128], BF16)
        self.u_b = kb.sb("u_b", [128, 128], BF16)
        self.ones_b = kb.sb("ones_b", [128, 128], BF16)
        self.ebase = kb.sb("ebase", [128, NE], F32)
        self.slots_all = kb.sb("slots_all", [128, T // 128, 4], I32)
        self.gk_all = kb.sb("gk_all", [128, T // 128, 4], F32)
        self.cnt = kb.sb("cnt", [128, NE], F32)
        for t_, n_ in ((self.ident_f, "c_ident_f"), (self.ident_b, "c_ident_b"), (self.ones_f, "c_ones_f"),
                       (self.mask_b, "c_mask_b"), (self.rt_b, "c_rt_b"), (self.u_b, "c_u_b"),
                       (self.ones_b, "c_ones_b"), (self.ebase, "c_ebase")):
            kb.dma("sp", t_[:], d[n_][:], reads=[d[n_]], writes=[t_])
        self.lncols = kb.sb("lncols", [128, 4 * 32], F32)
        self.eng_rr = 0

        self.build()
        if not d["out"].res.w:
            zt = kb.sb("zt", [128, D], F32)
            kb.op("dve", I("memset", zt[:], 0.0), reads=[], writes=[zt])
            kb.dma("sp", d["out"][0:128, :], zt[:], reads=[zt], joins=[d["out"]])
        outs = list(d["out"].res.w)
        for n in self.debug:
            outs += list(d[n].res.w)
        kb.q["sp"].append((outs, None, None))
        kb.emit()
        kb.close()

    def cp_eng(self):
        self.eng_rr ^= 1
        return "act" if self.eng_rr else "dve"

    def copy(self, eng, out_t, out_ap, in_t, in_ap):
        if eng == "act":
            self.kb.op("act", I("activation", out=out_ap, in_=in_ap, func=AF.Identity), reads=[in_t], joins=[out_t])
        else:
            if eng == "dve":
                self.kb.op(eng, I("tensor_scalar", out_ap, in_ap, 1.0, None, op0=ALU.mult), reads=[in_t], joins=[out_t])
            else:
                self.kb.op(eng, I("tensor_copy", out=out_ap, in_=in_ap), reads=[in_t], joins=[out_t])

    def mm(self, out_t, out_ap, pairs, reads):
        n = len(pairs)
        fns = []
        for i, (l, r) in enumerate(pairs):
            fns.append(I("matmul", out_ap, lhsT=l, rhs=r, start=(i == 0), stop=(i == n - 1)))
        self.kb.op("pe", fns, reads=reads, writes=[out_t])

    def load_cols(self, dst_t, dst_ap, src_t, src_ap, n, ps_t, tmp_t):
        kb = self.kb
        kb.dma("sp", tmp_t[0:n, :], src_ap, reads=[src_t], writes=[tmp_t])
        kb.op("pe", I("transpose", ps_t[:, 0:n], tmp_t[0:n, :], self.ident_f[0:n, 0:n]),
              reads=[tmp_t, self.ident_f], writes=[ps_t])
        self.copy("dve", dst_t, dst_ap, ps_t, ps_t[:, 0:n])

    def want(self, name):
        return self.phases is None or name in self.phases

    def build(self):
        if self.want("setup"):
            self.phase_setup()
        for l in range(self.n_layers):
            if l % 2 == 0:
                if self.want(f"proj{l}"):
                    self.phase_mla_proj(l)
                if self.want(f"attn{l}"):
                    self.phase_attn(l)
                if self.want(f"mix{l}"):
                    self.phase_mix_ln1(l, "mla")
            else:
                if self.want(f"mix{l}"):
                    self.phase_mix_ln1(l, "lru")
            if self.want(f"moe{l}"):
                self.phase_moe(l)
            if self.want(f"comb{l}"):
                self.phase_combine(l)
            if self.want(f"ple{l}"):
                self.phase_ln2_ple(l, last=(l == self.n_layers - 1))

    def phase_setup(self):
        kb, d = self.kb, self.d
        kb.begin_phase()
        ps = Rot([kb.ps(f"su_ps{i}", [128, 512]) for i in range(4)])
        tmp = kb.sb("su_tmp", [128, 128], F32)
        for v, nm in enumerate(("ln1_g", "ln1_b", "ln2_g", "ln2_b")):
            src = d[nm][:].rearrange("l (c p) -> (l c) p", p=128)
            nrow = self.ln_nl * 8
            self.load_cols(self.lncols, self.lncols[:, v * 32:v * 32 + nrow], d[nm], src, nrow, ps.next(), tmp)
        invf = kb.sb("su_invf", [64, 1], F32)
        kb.dma("sp", invf[:], d["c_invf"][:], reads=[d["c_invf"]], writes=[invf])
        pos_i = kb.sb("su_posi", [64, T], I32)
        kb.dma("sp", pos_i[:], d["positions"][0:1, :].partition_broadcast(64), reads=[d["positions"]], writes=[pos_i])
        ang = kb.sb("su_ang", [64, T], F32)
        kb.op("dve", I("tensor_copy", out=ang[:], in_=pos_i[:]), reads=[pos_i], writes=[ang])
        kb.op("dve", I("tensor_scalar", ang[:], ang[:], invf[:, 0:1], None, op0=ALU.mult), reads=[ang, invf], writes=[ang])
        kf_i = kb.sb("su_kfi", [64, T], I32)
        kf = kb.sb("su_kf", [64, T], F32)
        r = kb.sb("su_r", [64, T], F32)
        fx = kb.sb("su_fx", [64, T], F32)
        C1 = 6.28125
        C2 = TWO_PI - C1
        for which, shift in ((1, 0.0), (0, math.pi / 2)):
            kb.op("dve", I("tensor_scalar", kf[:], ang[:], shift, 1.0 / TWO_PI, op0=ALU.add, op1=ALU.mult), reads=[ang], writes=[kf])
            kb.op("dve", I("tensor_copy", out=kf_i[:], in_=kf[:]), reads=[kf], writes=[kf_i])
            kb.op("dve", I("tensor_copy", out=kf[:], in_=kf_i[:]), reads=[kf_i], writes=[kf])
            kb.op("dve", I("scalar_tensor_tensor", out=r[:], in0=kf[:], scalar=-C1, in1=ang[:], op0=ALU.mult, op1=ALU.add), reads=[kf, ang], writes=[r])
            kb.op("dve", I("scalar_tensor_tensor", out=r[:], in0=kf[:], scalar=-C2, in1=r[:], op0=ALU.mult, op1=ALU.add), reads=[kf, r], writes=[r])
            if shift != 0.0:
                kb.op("dve", I("tensor_scalar", r[:], r[:], shift, None, op0=ALU.add), reads=[r], writes=[r])
            kb.op("dve", I("tensor_scalar", fx[:], r[:], math.pi, -TWO_PI, op0=ALU.is_gt, op1=ALU.mult), reads=[r], writes=[fx])
            kb.op("dve", I("tensor_tensor", out=r[:], in0=r[:], in1=fx[:], op=ALU.add), reads=[r, fx], writes=[r])
            kb.op("dve", I("tensor_scalar", fx[:], r[:], -math.pi, TWO_PI, op0=ALU.is_lt, op1=ALU.mult), reads=[r], writes=[fx])
            kb.op("dve", I("tensor_tensor", out=r[:], in0=r[:], in1=fx[:], op=ALU.add), reads=[r, fx], writes=[r])
            kb.op("dve", I("tensor_scalar", r[:], r[:], math.pi, -math.pi, op0=ALU.min, op1=ALU.max), reads=[r], writes=[r])
            kb.op("act", I("activation", out=fx[:], in_=r[:], func=AF.Sin), reads=[r], writes=[fx])
            kb.dma("sp", d["cs"][which], fx[:], reads=[fx], joins=[d["cs"]])
        zt = kb.sb("su_zt", [128, 6, D], BF16)
        kb.op("dve", I("memset", zt[:], 0.0), reads=[], writes=[zt])
        for r0 in range(0, NSLOT + 128, 768):
            nr = min(768, NSLOT + 128 - r0)
            kb.dma("sp", d["xg"][r0:r0 + nr, :].rearrange("(b p) f -> p b f", p=128), zt[:, 0:nr // 128, :], reads=[zt], joins=[d["xg"]])
        xin = Rot([kb.sb(f"su_xin{i}", [128, 4, D], F32) for i in range(2)])
        xo = Rot([kb.sb(f"su_xo{i}", [128, KC, TT], F32) for i in range(2)])
        xTv = d["xT"][:].rearrange("(c p) t -> p c t", p=128)
        for j in range(NT):
            xi = xin.next()
            kb.dma("sp", xi[:], d["x"][j * TT:(j + 1) * TT, :].rearrange("(b p) f -> p b f", p=128), reads=[d["x"]], writes=[xi])
            xo_ = xo.next()
            for c in range(KC):
                pt = ps.next()
                fns = [I("transpose", pt[:, b * 128:(b + 1) * 128], xi[:, b, c * 128:(c + 1) * 128], self.ident_f[:]) for b in range(4)]
                kb.op("pe", fns, reads=[xi, self.ident_f], writes=[pt])
                self.copy(self.cp_eng(), xo_, xo_[:, c, :], pt, pt[:])
            kb.dma("sp", xTv[:, :, j * TT:(j + 1) * TT], xo_[:], reads=[xo_], joins=[d["xT"]])
        kb.end_phase()

    def rope(self, src_ps_t, src_ps_ap, cs_t, out_t, out_ap, ps_rot, tmp_f, tmp_b, tmp2, n=TT):
        kb = self.kb
        if STOP == 311:
            return
        kb.op("act", I("activation", out=tmp_f[0:64, :n], in_=src_ps_ap, func=AF.Identity), reads=[src_ps_t], writes=[tmp_f])
        if STOP == 312:
            return
        kb.op("act", I("activation", out=tmp_b[0:64, :n], in_=src_ps_ap, func=AF.Identity), reads=[src_ps_t], writes=[tmp_b])
        if STOP == 31:
            return
        rp = ps_rot.next()
        self.mm(rp, rp[:, :n], [(self.rt_b[:, :], tmp_b[0:64, :n])], reads=[self.rt_b, tmp_b])
        if STOP == 32:
            return
        kb.op("dve", I("tensor_tensor", out=tmp2[0:64, :n], in0=rp[0:64, :n], in1=cs_t[0:64, 1, :n], op=ALU.mult), reads=[rp, cs_t], writes=[tmp2])
        kb.op("dve", I("tensor_tensor", out=tmp_f[0:64, :n], in0=tmp_f[0:64, :n], in1=cs_t[0:64, 0, :n], op=ALU.mult), reads=[tmp_f, cs_t], writes=[tmp_f])
        kb.op("dve", I("tensor_tensor", out=out_ap, in0=tmp_f[0:64, :n], in1=tmp2[0:64, :n], op=ALU.add), reads=[tmp_f, tmp2], writes=[out_t])

    def rope_a(self, src_ps_t, src_ps_ap, tmp_f, tmp_b, n=TT):
        kb = self.kb
        kb.op("act", I("activation", out=tmp_f[0:64, :n], in_=src_ps_ap, func=AF.Identity), reads=[src_ps_t], writes=[tmp_f])
        kb.op("act", I("activation", out=tmp_b[0:64, :n], in_=src_ps_ap, func=AF.Identity), reads=[src_ps_t], writes=[tmp_b])

    def rope_b(self, cs_t, out_t, out_ap, ps_rot, tmp_f, tmp_b, tmp2, n=TT):
        kb = self.kb
        rp = ps_rot.next()
        self.mm(rp, rp[:, :n], [(self.rt_b[:, :], tmp_b[0:64, :n])], reads=[self.rt_b, tmp_b])
        kb.op("dve", I("tensor_tensor", out=tmp2[0:64, :n], in0=rp[0:64, :n], in1=cs_t[0:64, 1, :n], op=ALU.mult), reads=[rp, cs_t], writes=[tmp2])
        kb.op("dve", I("tensor_tensor", out=tmp_f[0:64, :n], in0=tmp_f[0:64, :n], in1=cs_t[0:64, 0, :n], op=ALU.mult), reads=[tmp_f, cs_t], writes=[tmp_f])
        kb.op("dve", I("tensor_tensor", out=out_ap, in0=tmp_f[0:64, :n], in1=tmp2[0:64, :n], op=ALU.add), reads=[tmp_f, tmp2], writes=[out_t])

    def phase_mla_proj(self, l):
        kb, d = self.kb, self.d
        j_ = l // 2
        kb.begin_phase()
        w_in = kb.sb("mp_win", [128, KC, QL + KVL + 128], BF16)
        w_uq = kb.sb("mp_wuq", [128, 6, H * 192 + 64], BF16)
        w_ukv = kb.sb("mp_wukv", [128, 2, H * 256], BF16)
        kb.op("dve", I("memset", w_in[:, :, 1088:1152], 0.0), reads=[], joins=[w_in])
        kb.op("dve", I("memset", w_uq[:, :, H * 192:H * 192 + 64], 0.0), reads=[], joins=[w_uq])
        kb.dma("pool", w_in[:, :, 0:1088], d["mla_w_in"][j_].rearrange("(c p) f -> p c f", p=128), reads=[d["mla_w_in"]], joins=[w_in])
        kb.dma("pool", w_uq[:, :, 0:H * 192], d["mla_w_uq"][j_].rearrange("(c p) f -> p c f", p=128), reads=[d["mla_w_uq"]], joins=[w_uq])
        kb.dma("pool", w_ukv[:], d["mla_w_ukv"][j_].rearrange("(c p) f -> p c f", p=128), reads=[d["mla_w_ukv"]], writes=[w_ukv])
        ps = Rot([kb.ps(f"mp_ps{i}", [128, 512]) for i in range(6)])
        tmp = kb.sb("mp_tmp", [128, 128], F32)
        ncol = kb.sb("mp_ncol", [128, 8], F32)
        self.load_cols(ncol, ncol[:, 0:6], d["mla_q_norm"], d["mla_q_norm"][j_].rearrange("(c p) -> c p", p=128), 6, ps.next(), tmp)
        self.load_cols(ncol, ncol[:, 6:8], d["mla_kv_norm"], d["mla_kv_norm"][j_].rearrange("(c p) -> c p", p=128), 2, ps.next(), tmp)

        if STOP == 1:
            kb.end_phase(); return
        xa = Rot([kb.sb(f"mp_xa{i}", [128, KC, TT], F32) for i in range(1)])
        xb = Rot([kb.sb(f"mp_xb{i}", [128, KC, TT], BF16) for i in range(2)])
        lat = kb.sb("mp_lat", [128, 8, TT], F32)
        sq = kb.sb("mp_sq", [128, 8, TT], F32)
        rstd = [kb.sb(f"mp_rstd{i}", [128, TT], F32) for i in range(2)]
        cqn = kb.sb("mp_cqn", [128, 8, TT], BF16)
        cs = Rot([kb.sb(f"mp_cs{i}", [64, 2, TT], F32) for i in range(2)])
        tfs = Rot([kb.sb(f"mp_tf{i}", [64, TT], F32) for i in range(2)])
        tbs = Rot([kb.sb(f"mp_tb{i}", [64, TT], BF16) for i in range(2)])
        t2s = Rot([kb.sb(f"mp_t2{i}", [64, TT], F32) for i in range(2)])
        kr_o = Rot([kb.sb(f"mp_kro{i}", [64, TT], BF16) for i in range(2)])
        qn_o = Rot([kb.sb(f"mp_qno{i}", [128, H, TT], BF16) for i in range(1)])
        qr_o = Rot([kb.sb(f"mp_qro{i}", [64, H, TT], BF16) for i in range(1)])
        kn_o = Rot([kb.sb(f"mp_kno{i}", [128, H, TT], BF16) for i in range(1)])
        v_o = Rot([kb.sb(f"mp_vo{i}", [128, 4, H * 128], BF16) for i in range(1)])
        xTv = d["xT"][:].rearrange("(c p) t -> p c t", p=128)
        wv = w_ukv[:].rearrange("p c (h two e) -> p c h two e", two=2, e=128)
        for j in range(NT):
            ts = slice(j * TT, (j + 1) * TT)
            xa_ = xa.next(); xb_ = xb.next()
            kb.dma("sp", xa_[:], xTv[:, :, ts], reads=[d["xT"]], writes=[xa_])
            for c in range(KC):
                self.copy(self.cp_eng(), xb_, xb_[:, c, :], xa_, xa_[:, c, :])
            cs_ = cs.next()
            kb.dma("sp", cs_[:], d["cs"][:, :, ts].rearrange("a p t -> p a t"), reads=[d["cs"]], writes=[cs_])
            for oc in range(8):
                pt = ps.next()
                self.mm(pt, pt[:], [(w_in[:, kc, oc * 128:(oc + 1) * 128], xb_[:, kc, :]) for kc in range(KC)], reads=[w_in, xb_])
                kb.op("act", I("activation", out=lat[:, oc, :], in_=pt[:], func=AF.Identity), reads=[pt], joins=[lat])
                kb.op("act", I("activation", out=sq[:, oc, :], in_=pt[:], func=AF.Square), reads=[pt], joins=[sq])
            if STOP == 2:
                break
            ptk = ps.next()
            self.mm(ptk, ptk[:, :], [(w_in[:, kc, 1024:1152], xb_[:, kc, :]) for kc in range(KC)], reads=[w_in, xb_])
            kr_ = kr_o.next()
            self.rope(ptk, ptk[0:64, :], cs_, kr_, kr_[:, :], ps, tfs.next(), tbs.next(), t2s.next())
            if STOP in (31, 32, 33, 311, 312):
                break
            kb.dma("sp", d["kr"][:, ts], kr_[:], reads=[kr_], joins=[d["kr"]])
            if STOP == 3:
                break
            for which, (c0, c1, dim) in enumerate(((0, 6, QL), (6, 8, KVL))):
                pt = ps.next()
                self.mm(pt, pt[:], [(self.ones_f[:], sq[:, c, :]) for c in range(c0, c1)], reads=[self.ones_f, sq])
                rs_ = rstd[which]
                kb.op("act", I("activation", out=rs_[:], in_=pt[:], func=AF.Sqrt, scale=1.0 / dim, bias=RMS_EPS), reads=[pt], writes=[rs_])
                kb.op("dve", I("reciprocal", out=rs_[:], in_=rs_[:]), reads=[rs_], writes=[rs_])
                for c in range(c0, c1):
                    kb.op("dve", I("scalar_tensor_tensor", out=cqn[:, c, :], in0=lat[:, c, :], scalar=ncol[:, c:c + 1], in1=rs_[:], op0=ALU.mult, op1=ALU.mult),
                          reads=[lat, ncol, rs_], joins=[cqn])
            if STOP == 4:
                break
            qn_ = qn_o.next(); qr_ = qr_o.next()
            pend = None
            for h in range(H):
                pt = ps.next()
                self.mm(pt, pt[:], [(w_uq[:, kc, h * 192:h * 192 + 128], cqn[:, kc, :]) for kc in range(6)], reads=[w_uq, cqn])
                self.copy(self.cp_eng(), qn_, qn_[:, h, :], pt, pt[:])
                pt2 = ps.next()
                self.mm(pt2, pt2[:, :], [(w_uq[:, kc, h * 192 + 128:h * 192 + 256], cqn[:, kc, :]) for kc in range(6)], reads=[w_uq, cqn])
                tf_, tb_, t2_ = tfs.next(), tbs.next(), t2s.next()
                self.rope_a(pt2, pt2[0:64, :], tf_, tb_)
                if pend is not None:
                    self.rope_b(*pend)
                pend = (cs_, qr_, qr_[:, h, :], ps, tf_, tb_, t2_)
            self.rope_b(*pend)
            kb.dma("sp", d["qn"][:, :, ts].rearrange("h p t -> p h t"), qn_[:], reads=[qn_], joins=[d["qn"]])
            kb.dma("sp", d["qr"][:, :, ts].rearrange("h p t -> p h t"), qr_[:], reads=[qr_], joins=[d["qr"]])
            if STOP == 5:
                break
            kn_ = kn_o.next()
            for h in range(H):
                pt = ps.next()
                self.mm(pt, pt[:], [(w_ukv[:, kc, h * 256:h * 256 + 128], cqn[:, 6 + kc, :]) for kc in range(2)], reads=[w_ukv, cqn])
                self.copy(self.cp_eng(), kn_, kn_[:, h, :], pt, pt[:])
            kb.dma("sp", d["kn"][:, :, ts].rearrange("h p t -> p h t"), kn_[:], reads=[kn_], joins=[d["kn"]])
            if STOP == 6:
                break
            v_ = v_o.next()
            for b in range(4):
                for hf in range(2):
                    pt = ps.next()
                    o_ap = pt[:].rearrange("p (a e) -> p a e", a=4)
                    self.mm(pt, o_ap, [(cqn[:, 6 + kc, b * 128:(b + 1) * 128], wv[:, kc, hf * 4:(hf + 1) * 4, 1, :]) for kc in range(2)], reads=[w_ukv, cqn])
                    self.copy(self.cp_eng(), v_, v_[:, b, hf * 512:(hf + 1) * 512], pt, pt[:])
            kb.dma("sp", d["v"][ts, :].rearrange("(b p) f -> p b f", p=128), v_[:], reads=[v_], joins=[d["v"]])
        kb.end_phase()

    def phase_attn(self, l):
        kb, d = self.kb, self.d
        kb.begin_phase()
        NQ = T // 128
        NST = 3
        kr = kb.sb("at_kr", [64, T], BF16)
        kb.dma("sp", kr[:], d["kr"][:], reads=[d["kr"]], writes=[kr])
        hd = [dict(kn=kb.sb(f"at_kn{i}", [128, T], BF16), qn=kb.sb(f"at_qn{i}", [128, T], BF16), qr=kb.sb(f"at_qr{i}", [64, T], BF16),
                   v=kb.sb(f"at_v{i}", [128, NQ, 128], BF16), oT=kb.sb(f"at_oT{i}", [128, T], BF16)) for i in range(2)]
        S = [kb.sb(f"at_S{i}", [128, T], F32) for i in range(NST)]
        P = [kb.sb(f"at_P{i}", [128, T], BF16) for i in range(NST)]
        PT = [kb.sb(f"at_PT{i}", [128, NQ, 128], BF16) for i in range(NST)]
        small = [kb.sb(f"at_sm{i}", [128, 4], F32) for i in range(NST)]
        osb = Rot([kb.sb(f"at_osb{i}", [128, 128], BF16) for i in range(3)])
        sc = Rot([kb.ps(f"at_sc{i}", [128, 512]) for i in range(3)])
        ptp = Rot([kb.ps(f"at_pt{i}", [128, 1024], BF16) for i in range(2)])
        ops = Rot([kb.ps(f"at_o{i}", [128, 512]) for i in range(2)])
        otp = kb.ps("at_oTp", [128, 1024], BF16)
        vview = d["v"][:].rearrange("(c p) (h e) -> h p c e", p=128, e=128)

        def load_head(h):
            t_ = hd[h % 2]
            kb.dma("sp", t_["kn"][:], d["kn"][h], reads=[d["kn"]], writes=[t_["kn"]])
            kb.dma("sp", t_["qn"][:], d["qn"][h], reads=[d["qn"]], writes=[t_["qn"]])
            kb.dma("sp", t_["qr"][:], d["qr"][h], reads=[d["qr"]], writes=[t_["qr"]])
            for q4 in range(4):
                kb.dma("sp", t_["v"][:, q4 * 8:(q4 + 1) * 8, :], vview[h][:, q4 * 8:(q4 + 1) * 8, :], reads=[d["v"]], joins=[t_["v"]])

        items = [(h, qi) for h in range(H) for qi in range(NQ)]

        def stage_a(i):
            h, qi = items[i]
            if qi == 0 and h == 0:
                load_head(0)
            if qi == 3 and h + 1 < H:
                load_head(h + 1)
            t_ = hd[h % 2]
            qn_, kn_, qr_ = t_["qn"], t_["kn"], t_["qr"]
            S_ = S[i % NST]
            qs = slice(qi * 128, (qi + 1) * 128)
            nk = (qi + 1) * 128
            nblk = (nk + 511) // 512
            for b in range(nblk):
                k0 = b * 512
                kw = min(512, nk - k0)
                pt = sc.next()
                last = (b == nblk - 1)
                fns = [I("matmul", pt[:, :kw], lhsT=qn_[:, qs], rhs=kn_[:, k0:k0 + kw], start=True, stop=False),
                       I("matmul", pt[:, :kw], lhsT=qr_[:, qs], rhs=kr[:, k0:k0 + kw], start=False, stop=(not last))]
                if last:
                    fns.append(I("matmul", pt[:, kw - 128:kw], lhsT=self.ident_b[:], rhs=self.mask_b[:], start=False, stop=True))
                kb.op("pe", fns, reads=[qn_, kn_, qr_, kr, self.ident_b, self.mask_b], writes=[pt])
                self.copy(self.cp_eng(), S_, S_[:, k0:k0 + kw], pt, pt[:, :kw])

        def stage_b(i):
            h, qi = items[i]
            nk = (qi + 1) * 128
            S_, P_, sm = S[i % NST], P[i % NST], small[i % NST]
            kb.op("dve", I("reduce_max", out=sm[:, 0:1], in_=S_[:, :nk], axis=AX.X), reads=[S_], writes=[sm])
            kb.op("dve", I("tensor_scalar", sm[:, 1:2], sm[:, 0:1], -ATT_SCALE, None, op0=ALU.mult), reads=[sm], writes=[sm])
            kb.op("act", I("activation", out=P_[:, :nk], in_=S_[:, :nk], func=AF.Exp, scale=ATT_SCALE, bias=sm[:, 1:2], accum_out=sm[:, 2:3]),
                  reads=[S_, sm], writes=[P_, sm])

        opsum = {}

        def stage_t(i):
            h, qi = items[i]
            nk = (qi + 1) * 128
            P_, PT_ = P[i % NST], PT[i % NST]
            nc_ = nk // 128
            for g0 in range(0, nc_, 8):
                g1 = min(nc_, g0 + 8)
                tp = ptp.next()
                fns = [I("transpose", tp[:, (c - g0) * 128:(c - g0 + 1) * 128], P_[:, c * 128:(c + 1) * 128], self.ident_b[:]) for c in range(g0, g1)]
                kb.op("pe", fns, reads=[P_, self.ident_b], writes=[tp])
                self.copy(self.cp_eng(), PT_, PT_[:, g0:g1, :], tp, tp[:, 0:(g1 - g0) * 128].rearrange("p (c e) -> p c e", e=128))

        def stage_pv(i):
            h, qi = items[i]
            v_ = hd[h % 2]["v"]
            PT_ = PT[i % NST]
            nc_ = qi + 1
            op_ = ops.next()
            opsum[i] = op_
            fns = [I("matmul", op_[:, 0:128], lhsT=PT_[:, c, :], rhs=v_[:, c, :], start=(c == 0), stop=(c == nc_ - 1)) for c in range(nc_)]
            kb.op("pe", fns, reads=[PT_, v_], writes=[op_])

        def stage_o(i):
            h, qi = items[i]
            oT_ = hd[h % 2]["oT"]
            sm = small[i % NST]
            op_ = opsum.pop(i)
            kb.op("dve", I("reciprocal", out=sm[:, 3:4], in_=sm[:, 2:3]), reads=[sm], writes=[sm])
            os_ = osb.next()
            kb.op("dve", I("tensor_scalar", os_[:], op_[:, 0:128], sm[:, 3:4], None, op0=ALU.mult), reads=[op_, sm], writes=[os_])
            g = qi % 8
            kb.op("pe", I("transpose", otp[:, g * 128:(g + 1) * 128], os_[:], self.ident_b[:]), reads=[os_, self.ident_b], joins=[otp])
            if g == 7:
                q0 = (qi - 7) * 128
                self.copy(self.cp_eng(), oT_, oT_[:, q0:q0 + 1024], otp, otp[:])
            if qi == NQ - 1:
                kb.dma("sp", d["oT"][h], oT_[:], reads=[oT_], joins=[d["oT"]])

        n = len(items)
        for step in range(n + 4):
            if 0 <= step - 2 < n:
                stage_t(step - 2)
            if step < n:
                stage_a(step)
            if 0 <= step - 2 < n:
                stage_pv(step - 2)
            if 0 <= step - 1 < n:
                stage_b(step - 1)
            if 0 <= step - 3 < n:
                stage_o(step - 3)
        kb.end_phase()

    def layernorm(self, pre, sq, ps, mean, rstd, tmp, gcol, bcol, out_f, out_b):
        kb = self.kb
        for c in range(KC):
            kb.op("act", I("activation", out=sq[:, c, :], in_=pre[:, c, :], func=AF.Square), reads=[pre], joins=[sq])
        p1 = ps.next()
        self.mm(p1, p1[:], [(self.ones_f[:], pre[:, c, :]) for c in range(KC)], reads=[self.ones_f, pre])
        p2 = ps.next()
        self.mm(p2, p2[:], [(self.ones_f[:], sq[:, c, :]) for c in range(KC)], reads=[self.ones_f, sq])
        kb.op("act", I("activation", out=mean[:], in_=p1[:], func=AF.Identity, scale=1.0 / D), reads=[p1], writes=[mean])
        kb.op("dve", I("tensor_tensor", out=tmp[:], in0=mean[:], in1=mean[:], op=ALU.mult), reads=[mean], writes=[tmp])
        kb.op("dve", I("scalar_tensor_tensor", out=rstd[:], in0=p2[:], scalar=1.0 / D, in1=tmp[:], op0=ALU.mult, op1=ALU.subtract), reads=[p2, tmp], writes=[rstd])
        kb.op("act", I("activation", out=rstd[:], in_=rstd[:], func=AF.Sqrt, bias=LN_EPS), reads=[rstd], writes=[rstd])
        kb.op("dve", I("reciprocal", out=rstd[:], in_=rstd[:]), reads=[rstd], writes=[rstd])
        for c in range(KC):
            kb.op("dve", I("tensor_tensor", out=sq[:, c, :], in0=pre[:, c, :], in1=mean[:], op=ALU.subtract), reads=[pre, mean], joins=[sq])
            kb.op("pool", I("tensor_tensor", out=sq[:, c, :], in0=sq[:, c, :], in1=rstd[:], op=ALU.mult), reads=[sq, rstd], joins=[sq])
            kb.op("act", I("activation", out=out_f[:, c, :], in_=sq[:, c, :], func=AF.Identity, scale=gcol[:, c:c + 1], bias=bcol[:, c:c + 1]),
                  reads=[sq, self.lncols], joins=[out_f])
            if out_b is not None:
                kb.op("pool", I("tensor_copy", out=out_b[:, c, :], in_=out_f[:, c, :]), reads=[out_f], joins=[out_b])

    def phase_mix_ln1(self, l, kind):
        kb, d = self.kb, self.d
        j_ = l // 2
        kb.begin_phase()
        ps = Rot([kb.ps(f"ml_ps{i}", [128, 512]) for i in range(7)])
        tmp = kb.sb("ml_tmp", [128, 128], F32)
        w_o = kb.sb("ml_wo", [128, KC, D], BF16)
        wsrc = d["mla_w_o"] if kind == "mla" else d["lru_w_out"]
        kb.dma("pool", w_o[:], wsrc[j_].rearrange("(c p) f -> p c f", p=128), reads=[wsrc], writes=[w_o])
        wr = kb.sb("ml_wr", [128, KC, NE], F32)
        kb.dma("sp", wr[:], d["moe_w_router"][l].rearrange("(c p) e -> p c e", p=128), reads=[d["moe_w_router"]], writes=[wr])
        br = kb.sb("ml_br", [1, NE], F32)
        kb.dma("sp", br[:], d["moe_b_router"][l:l + 1, :], reads=[d["moe_b_router"]], writes=[br])
        xa = Rot([kb.sb(f"ml_xa{i}", [128, KC, TT], F32) for i in range(1 if kind == "lru" else 2)])
        yin = Rot([kb.sb(f"ml_yin{i}", [128, KC, TT], BF16) for i in range(1 if kind == "lru" else 2)])
        pres = Rot([kb.sb(f"ml_pre{i}", [128, KC, TT], F32) for i in range(1 if kind == "lru" else 2)])
        sq = kb.sb("ml_sq", [128, KC, TT], F32)
        mean = kb.sb("ml_mean", [128, TT], F32)
        rstd = kb.sb("ml_rstd", [128, TT], F32)
        tmp2 = kb.sb("ml_tmp2", [128, TT], F32)
        x1f = Rot([kb.sb(f"ml_x1f{i}", [128, KC, TT], F32) for i in range(1 if kind == "lru" else 2)])
        x1b = Rot([kb.sb(f"ml_x1b{i}", [128, KC, TT], BF16) for i in range(1 if kind == "lru" else 2)])
        lg = kb.sb("ml_lg", [128, NE], F32)
        ee = kb.sb("ml_ee", [128, NE], F32)
        msk = kb.sb("ml_msk", [128, NE], F32)
        top8 = kb.sb("ml_top8", [128, 8], F32)
        sm = kb.sb("ml_sm", [128, 4], F32)
        mskb = kb.sb("ml_mskb", [128, NE], BF16)
        gts = kb.sb("ml_gts", [128, NE], F32)
        pos = kb.sb("ml_pos", [128, NE], F32)
        okm = kb.sb("ml_okm", [128, NE], F32)
        val = kb.sb("ml_val", [128, NE], F32)
        junk = kb.sb("ml_junk", [128, NE], F32)
        sl4 = kb.sb("ml_sl4", [128, 4], F32)
        x1tok = Rot([kb.sb(f"ml_xtok{i}", [128, 4, D], BF16) for i in range(1 if kind == "lru" else 2)])
        ptb = Rot([kb.ps(f"ml_ptb{i}", [128, 1024], BF16) for i in range(1)])
        kb.op("dve", I("memset", self.cnt[:], 0.0), reads=[], writes=[self.cnt])
        g1 = self.lncols[:, 0 * 32 + l * 8:0 * 32 + l * 8 + 8]
        b1 = self.lncols[:, 1 * 32 + l * 8:1 * 32 + l * 8 + 8]
        xTv = d["xT"][:].rearrange("(c p) t -> p c t", p=128)
        x1Tv = d["x1T"][:].rearrange("(c p) t -> p c t", p=128)
        x1bv = d["x1b"][:].rearrange("(c p) t -> p c t", p=128)
        if kind == "lru":
            L = self.lru_setup(l, ps, tmp)
        for j in range(NT):
            ts = slice(j * TT, (j + 1) * TT)
            xa_ = xa.next()
            kb.dma("sp", xa_[:], xTv[:, :, ts], reads=[d["xT"]], writes=[xa_])
            yin_ = yin.next()
            if kind == "mla":
                kb.dma("sp", yin_[:], d["oT"][:, :, ts].rearrange("h p t -> p h t"), reads=[d["oT"]], writes=[yin_])
            else:
                self.lru_tile(L, j, xa_, yin_, ps)
            pre = pres.next()
            for mc in range(KC):
                pt = ps.next()
                self.mm(pt, pt[:], [(w_o[:, kc, mc * 128:(mc + 1) * 128], yin_[:, kc, :]) for kc in range(KC)], reads=[w_o, yin_])
                kb.op("dve", I("scalar_tensor_tensor", out=pre[:, mc, :], in0=xa_[:, mc, :], scalar=ALPHA, in1=pt[:], op0=ALU.mult, op1=ALU.add),
                      reads=[xa_, pt], joins=[pre])
            x1f_, x1b_ = x1f.next(), x1b.next()
            self.layernorm(pre, sq, ps, mean, rstd, tmp2, g1, b1, x1f_, x1b_)
            kb.dma("sp", x1Tv[:, :, ts], x1f_[:], reads=[x1f_], joins=[d["x1T"]])
            xtok = x1tok.next()
            for b in range(4):
                tp = ptb.next()
                fns = [I("transpose", tp[:, kc * 128:(kc + 1) * 128], x1b_[:, kc, b * 128:(b + 1) * 128], self.ident_b[:]) for kc in range(KC)]
                kb.op("pe", fns, reads=[x1b_, self.ident_b], writes=[tp])
                self.copy(self.cp_eng(), xtok, xtok[:, b, :], tp, tp[:])
            for b in range(4):
                gb = j * 4 + b
                pt = ps.next()
                pairs = [(x1f_[:, kc, b * 128:(b + 1) * 128], wr[:, kc, :]) for kc in range(KC)]
                pairs.append((self.ones_f[0:1, 0:128], br[0:1, :]))
                self.mm(pt, pt[:, 0:NE], pairs, reads=[x1f_, wr, self.ones_f, br])
                self.copy("act", lg, lg[:], pt, pt[:, 0:NE])
                kb.op("dve", I("max", out=top8[:], in_=lg[:]), reads=[lg], writes=[top8])
                kb.op("dve", I("tensor_scalar", msk[:], lg[:], top8[:, 3:4], None, op0=ALU.is_ge), reads=[lg, top8], writes=[msk])
                kb.op("dve", I("tensor_scalar", mskb[:], lg[:], top8[:, 3:4], None, op0=ALU.is_ge), reads=[lg, top8], writes=[mskb])
                kb.op("dve", I("tensor_scalar", sm[:, 0:1], top8[:, 0:1], -1.0, None, op0=ALU.mult), reads=[top8], writes=[sm])
                kb.op("act", I("activation", out=ee[:], in_=lg[:], func=AF.Exp, bias=sm[:, 0:1]), reads=[lg, sm], writes=[ee])
                kb.op("dve", I("tensor_tensor", out=ee[:], in0=ee[:], in1=msk[:], op=ALU.mult), reads=[ee, msk], writes=[ee])
                kb.op("dve", I("reduce_sum", out=sm[:, 1:2], in_=ee[:], axis=AX.X), reads=[ee], writes=[sm])
                kb.op("dve", I("reciprocal", out=sm[:, 2:3], in_=sm[:, 1:2]), reads=[sm], writes=[sm])
                kb.op("dve", I("tensor_scalar", gts[:], ee[:], sm[:, 2:3], None, op0=ALU.mult), reads=[ee, sm], writes=[gts])
                pr = ps.next()
                self.mm(pr, pr[:, 0:NE], [(self.u_b[:], mskb[:])], reads=[self.u_b, mskb])
                pc = ps.next()
                self.mm(pc, pc[:, 0:NE], [(self.ones_b[:], mskb[:])], reads=[self.ones_b, mskb])
                kb.op("dve", I("tensor_tensor", out=pos[:], in0=pr[:, 0:NE], in1=self.cnt[:], op=ALU.add), reads=[pr, self.cnt], writes=[pos])
                kb.op("dve", I("tensor_tensor", out=self.cnt[:], in0=pc[:, 0:NE], in1=self.cnt[:], op=ALU.add), reads=[pc, self.cnt], writes=[self.cnt])
                kb.op("dve", I("tensor_scalar", okm[:], pos[:], float(CAP), None, op0=ALU.is_lt), reads=[pos], writes=[okm])
                kb.op("dve", I("tensor_tensor", out=okm[:], in0=okm[:], in1=msk[:], op=ALU.mult), reads=[okm, msk], writes=[okm])
                kb.op("dve", I("tensor_tensor", out=pos[:], in0=pos[:], in1=self.ebase[:], op=ALU.add), reads=[pos, self.ebase], writes=[pos])
                kb.op("dve", I("tensor_scalar", pos[:], pos[:], -1.0, BIGV, op0=ALU.mult, op1=ALU.add), reads=[pos], writes=[pos])
                kb.op("dve", I("tensor_tensor", out=val[:], in0=pos[:], in1=okm[:], op=ALU.mult), reads=[pos, okm], writes=[val])
                kb.op("dve", I("max", out=top8[:], in_=val[:]), reads=[val], writes=[top8])
                kb.op("dve", I("tensor_scalar", sl4[:], top8[:, 0:4], -1.0, BIGV, op0=ALU.mult, op1=ALU.add), reads=[top8], writes=[sl4])
                kb.op("dve", I("tensor_copy", out=self.slots_all[:, gb, :], in_=sl4[:]), reads=[sl4], joins=[self.slots_all])
                for k in range(4):
                    kb.op("dve", I("scalar_tensor_tensor", out=junk[:], in0=val[:], scalar=top8[:, k:k + 1], in1=gts[:], op0=ALU.is_equal, op1=ALU.mult,
                                   accum_out=self.gk_all[:, gb, k:k + 1]), reads=[val, top8, gts], writes=[junk], joins=[self.gk_all])
                for k in range(4):
                    kb.idma(d["xg"][:, :], bass.IndirectOffsetOnAxis(self.slots_all[:, gb, k:k + 1], 0), xtok[:, b, :], None, NSLOT - 1,
                            reads=[xtok, self.slots_all], joins=[d["xg"]])
        kb.end_phase()

    def lru_setup(self, l, ps, tmp):
        kb, d = self.kb, self.d
        j_ = l // 2
        L = {}
        L["w_in"] = kb.sb("lr_win", [128, KC, 2 * D], BF16)
        kb.dma("pool", L["w_in"][:], d["lru_w_in"][j_].rearrange("(c p) f -> p c f", p=128), reads=[d["lru_w_in"]], writes=[L["w_in"]])
        for nm, src in (("w_a", "lru_w_a"), ("w_x", "lru_w_x")):
            L[nm] = kb.sb("lr_" + nm, [128, 4, 2, 256], BF16)
            kb.dma("pool", L[nm][:], d[src][j_].rearrange("n (c p) f -> p n c f", p=128), reads=[d[src]], writes=[L[nm]])
        cols = L["cols"] = kb.sb("lr_cols", [128, 80], F32)
        self.load_cols(cols, cols[:, 0:32], d["lru_conv_w"], d["lru_conv_w"][j_].rearrange("k (c p) -> (k c) p", p=128), 32, ps.next(), tmp)
        for i, nm in enumerate(("lru_conv_b", "lru_b_a", "lru_b_x", "lru_lambda")):
            self.load_cols(cols, cols[:, 32 + 8 * i:40 + 8 * i], d[nm], d[nm][j_].rearrange("(c p) -> c p", p=128), 8, ps.next(), tmp)
        kb.op("act", I("activation", out=cols[:, 64:72], in_=cols[:, 56:64], func=AF.Exp, scale=-1.0), reads=[cols], writes=[cols])
        kb.op("act", I("activation", out=cols[:, 64:72], in_=cols[:, 64:72], func=AF.Ln, bias=1.0), reads=[cols], writes=[cols])
        kb.op("dve", I("tensor_scalar", cols[:, 64:72], cols[:, 64:72], -8.0, None, op0=ALU.mult), reads=[cols], writes=[cols])
        kb.op("dve", I("tensor_scalar", cols[:, 72:80], cols[:, 64:72], 2.0, None, op0=ALU.mult), reads=[cols], writes=[cols])
        L["halo"] = kb.sb("lr_halo", [128, KC, 4], F32)
        kb.op("dve", I("memset", L["halo"][:], 0.0), reads=[], writes=[L["halo"]])
        L["carry"] = kb.sb("lr_carry", [128, KC], F32)
        kb.op("dve", I("memset", L["carry"][:], 0.0), reads=[], writes=[L["carry"]])
        L["u"] = Rot([kb.sb(f"lr_u{i}", [128, 3 + TT], F32) for i in range(2)])
        L["gate"] = Rot([kb.sb(f"lr_gate{i}", [128, TT], BF16) for i in range(4)])
        L["uc"] = Rot([kb.sb(f"lr_uc{i}", [128, TT], F32) for i in range(4)])
        L["ucb"] = Rot([kb.sb(f"lr_ucb{i}", [128, TT], BF16) for i in range(4)])
        L["a"] = Rot([kb.sb(f"lr_a{i}", [128, TT], F32) for i in range(2)])
        L["a2"] = Rot([kb.sb(f"lr_a2{i}", [128, TT], F32) for i in range(2)])
        L["i"] = Rot([kb.sb(f"lr_i{i}", [128, TT], F32) for i in range(2)])
        L["r"] = Rot([kb.sb(f"lr_r{i}", [128, TT], F32) for i in range(2)])
        L["h"] = Rot([kb.sb(f"lr_h{i}", [128, TT], F32) for i in range(2)])
        L["xb"] = kb.sb("lr_xb", [128, KC, TT], BF16)
        return L

    def lru_tile(self, L, j, xa_, y_out, ps):
        kb = self.kb
        cols, halo, carry, w_in, xb = L["cols"], L["halo"], L["carry"], L["w_in"], L["xb"]
        for c in range(KC):
            self.copy(self.cp_eng(), xb, xb[:, c, :], xa_, xa_[:, c, :])
        chunks = {}

        def s1(n):
            chunk = chunks[n] = {}
            for oc in (2 * n, 2 * n + 1):
                pt = ps.next()
                self.mm(pt, pt[:], [(w_in[:, kc, oc * 128:(oc + 1) * 128], xb[:, kc, :]) for kc in range(KC)], reads=[w_in, xb])
                gate = L["gate"].next()
                kb.op("act", I("activation", out=gate[:], in_=pt[:], func=AF.Gelu_apprx_tanh), reads=[pt], writes=[gate])
                pt2 = ps.next()
                self.mm(pt2, pt2[:], [(w_in[:, kc, D + oc * 128:D + (oc + 1) * 128], xb[:, kc, :]) for kc in range(KC)], reads=[w_in, xb])
                u = L["u"].next()
                kb.op("dve", I("tensor_copy", out=u[:, 0:3], in_=halo[:, oc, 0:3]), reads=[halo], writes=[u])
                kb.op("act", I("activation", out=u[:, 3:3 + TT], in_=pt2[:], func=AF.Identity), reads=[pt2], joins=[u])
                kb.op("dve", I("tensor_copy", out=halo[:, oc, 0:3], in_=u[:, TT:TT + 3]), reads=[u], writes=[halo])
                uc = L["uc"].next()
                ucb = L["ucb"].next()
                kb.op("dve", I("tensor_scalar", uc[:], u[:, 0:TT], cols[:, oc:oc + 1], cols[:, 32 + oc:33 + oc], op0=ALU.mult, op1=ALU.add),
                      reads=[u, cols], writes=[uc])
                for k in range(1, 4):
                    kb.op("dve", I("scalar_tensor_tensor", out=uc[:], in0=u[:, k:k + TT], scalar=cols[:, k * 8 + oc:k * 8 + oc + 1], in1=uc[:], op0=ALU.mult, op1=ALU.add),
                          reads=[u, cols, uc], writes=[uc])
                kb.op("pool", I("tensor_copy", out=ucb[:], in_=uc[:]), reads=[uc], writes=[ucb])
                chunk[oc] = (gate, uc, ucb)

        def s2(n):
            chunk = chunks.pop(n)
            for oc in (2 * n, 2 * n + 1):
                co = (oc % 2) * 128
                a, a2, ii, rr = L["a"].next(), L["a2"].next(), L["i"].next(), L["r"].next()
                gate, uc, _ = chunk[oc]
                ucbs = [chunk[2 * n][2], chunk[2 * n + 1][2]]
                pa = ps.next()
                self.mm(pa, pa[:], [(L["w_a"][:, n, kc, co:co + 128], ucbs[kc][:]) for kc in range(2)], reads=[L["w_a"]] + ucbs)
                px = ps.next()
                self.mm(px, px[:], [(L["w_x"][:, n, kc, co:co + 128], ucbs[kc][:]) for kc in range(2)], reads=[L["w_x"]] + ucbs)
                kb.op("act", I("activation", out=rr[:], in_=pa[:], func=AF.Sigmoid, bias=cols[:, 40 + oc:41 + oc]), reads=[pa, cols], writes=[rr])
                kb.op("act", I("activation", out=ii[:], in_=px[:], func=AF.Sigmoid, bias=cols[:, 48 + oc:49 + oc]), reads=[px, cols], writes=[ii])
                kb.op("act", I("activation", out=a[:], in_=rr[:], func=AF.Exp, scale=cols[:, 64 + oc:65 + oc]), reads=[rr, cols], writes=[a])
                kb.op("act", I("activation", out=a2[:], in_=rr[:], func=AF.Exp, scale=cols[:, 72 + oc:73 + oc]), reads=[rr, cols], writes=[a2])
                kb.op("dve", I("tensor_scalar", a2[:], a2[:], -1.0, 1.0, op0=ALU.mult, op1=ALU.add), reads=[a2], writes=[a2])
                kb.op("act", I("activation", out=a2[:], in_=a2[:], func=AF.Sqrt), reads=[a2], writes=[a2])
                kb.op("dve", I("tensor_tensor", out=ii[:], in0=ii[:], in1=uc[:], op=ALU.mult), reads=[ii, uc], writes=[ii])
                kb.op("dve", I("tensor_tensor", out=ii[:], in0=ii[:], in1=a2[:], op=ALU.mult), reads=[ii, a2], writes=[ii])
                h = L["h"].next()
                kb.op("dve", I("tensor_tensor_scan", out=h[:], data0=a[:], data1=ii[:], initial=carry[:, oc:oc + 1], op0=ALU.mult, op1=ALU.add),
                      reads=[a, ii, carry], writes=[h])
                kb.op("dve", I("tensor_copy", out=carry[:, oc:oc + 1], in_=h[:, TT - 1:TT]), reads=[h], writes=[carry])
                kb.op("dve", I("tensor_tensor", out=y_out[:, oc, :], in0=h[:], in1=gate[:], op=ALU.mult), reads=[h, gate], joins=[y_out])

        s1(0)
        for n in range(4):
            if n + 1 < 4:
                s1(n + 1)
            s2(n)

    def phase_moe(self, l):
        kb, d = self.kb, self.d
        kb.begin_phase()
        NBLK = CAP // 128
        PARTS = ((0, 512), (512, CAP - 512))
        ps = Rot([kb.ps(f"mo_ps{i}", [128, 512]) for i in range(5)])
        ptb = Rot([kb.ps(f"mo_ptb{i}", [128, 1024], BF16) for i in range(3)])
        tmp = kb.sb("mo_tmp", [128, 128], F32)
        wu = Rot([kb.sb(f"mo_wu{i}", [128, KC, 2 * DFF], BF16) for i in range(2)])
        wd = Rot([kb.sb(f"mo_wd{i}", [128, KC, D], BF16) for i in range(2)])
        bdb = Rot([kb.sb(f"mo_bd{i}", [128, D], F32) for i in range(2)])
        bup = kb.sb("mo_bup", [128, NE * 16], F32)
        for e4 in range(0, NE, 8):
            self.load_cols(bup, bup[:, e4 * 16:(e4 + 8) * 16], d["moe_b_up"],
                           d["moe_b_up"][l, e4:e4 + 8, :].rearrange("e (c p) -> (e c) p", p=128), 128, ps.next(), tmp)
        xg = Rot([kb.sb(f"mo_xg{i}", [128, NBLK, D], BF16) for i in range(2)])
        xgT = kb.sb("mo_xgT", [128, KC, CAP], BF16)
        hT = kb.sb("mo_hT", [128, KC, CAP], BF16)
        yst = Rot([kb.sb(f"mo_ys{i}", [128, NBLK, D], BF16) for i in range(2)])
        g_ = Rot([kb.sb(f"mo_g{i}", [128, CAP], F32) for i in range(2)])
        sg_ = Rot([kb.sb(f"mo_sg{i}", [128, CAP], F32) for i in range(2)])
        u_ = Rot([kb.sb(f"mo_u{i}", [128, CAP], F32) for i in range(2)])
        bufs = {}

        def load(ex):
            wu_, wd_, bd_, xg_ = wu.next(), wd.next(), bdb.next(), xg.next()
            bufs[ex] = (wu_, wd_, bd_, xg_)
            rows = slice(ex * CAP, (ex + 1) * CAP)
            kb.dma("sp", xg_[:], d["xg"][rows, :].rearrange("(b p) f -> p b f", p=128), reads=[d["xg"]], writes=[xg_])
            kb.dma("sp", bd_[:], d["moe_b_down"][l, ex:ex + 1, :].partition_broadcast(128), reads=[d["moe_b_down"]], writes=[bd_])
            for q4 in range(4):
                kb.dma("pool", wu_[:, 2 * q4:2 * q4 + 2, :], d["moe_w_up"][l, ex, q4 * 256:(q4 + 1) * 256, :].rearrange("(c p) f -> p c f", p=128),
                       reads=[d["moe_w_up"]], joins=[wu_])
            for q2 in range(2):
                kb.dma("pool", wd_[:, 4 * q2:4 * q2 + 4, :], d["moe_w_down"][l, ex, q2 * 512:(q2 + 1) * 512, :].rearrange("(c p) f -> p c f", p=128),
                       reads=[d["moe_w_down"]], joins=[wd_])

        def transposes(ex):
            xg_ = bufs[ex][3]
            for kc in range(KC):
                tp = ptb.next()
                fns = [I("transpose", tp[:, b * 128:(b + 1) * 128], xg_[:, b, kc * 128:(kc + 1) * 128], self.ident_b[:]) for b in range(NBLK)]
                kb.op("pe", fns, reads=[xg_, self.ident_b], writes=[tp])
                self.copy(self.cp_eng(), xgT, xgT[:, kc, :], tp, tp[:, 0:CAP])

        def up(ex):
            wu_ = bufs[ex][0]
            for fc in range(KC):
                gg, sg, uu = g_.next(), sg_.next(), u_.next()
                cg = ex * 16 + fc
                cu = ex * 16 + 8 + fc
                for (s0, sw) in PARTS:
                    pg = ps.next()
                    self.mm(pg, pg[:, 0:sw], [(wu_[:, kc, fc * 128:(fc + 1) * 128], xgT[:, kc, s0:s0 + sw]) for kc in range(KC)], reads=[wu_, xgT])
                    kb.op("dve", I("tensor_scalar", gg[:, s0:s0 + sw], pg[:, 0:sw], bup[:, cg:cg + 1], 7.0, op0=ALU.add, op1=ALU.min), reads=[pg, bup], joins=[gg])
                    pu = ps.next()
                    self.mm(pu, pu[:, 0:sw], [(wu_[:, kc, DFF + fc * 128:DFF + (fc + 1) * 128], xgT[:, kc, s0:s0 + sw]) for kc in range(KC)], reads=[wu_, xgT])
                    kb.op("act", I("activation", out=uu[:, s0:s0 + sw], in_=pu[:, 0:sw], func=AF.Identity, bias=bup[:, cu:cu + 1]), reads=[pu, bup], joins=[uu])
                kb.op("act", I("activation", out=sg[:], in_=gg[:], func=AF.Sigmoid, scale=1.702), reads=[gg], writes=[sg])
                kb.op("pool", I("tensor_scalar", uu[:], uu[:], 7.0, -7.0, op0=ALU.min, op1=ALU.max), reads=[uu], writes=[uu])
                kb.op("pool", I("tensor_tensor", out=gg[:], in0=gg[:], in1=sg[:], op=ALU.mult), reads=[gg, sg], writes=[gg])
                kb.op("dve", I("scalar_tensor_tensor", out=hT[:, fc, :], in0=uu[:], scalar=1.0, in1=gg[:], op0=ALU.add, op1=ALU.mult), reads=[uu, gg], joins=[hT])

        def down(ex):
            _, wd_, bd_, _ = bufs[ex]
            rows = slice(ex * CAP, (ex + 1) * CAP)
            ys_ = yst.next()
            for b in range(NBLK):
                for hf in range(2):
                    pt = ps.next()
                    pairs = [(hT[:, fc, b * 128:(b + 1) * 128], wd_[:, fc, hf * 512:(hf + 1) * 512]) for fc in range(KC)]
                    self.mm(pt, pt[:], pairs, reads=[hT, wd_])
                    kb.op("dve", I("scalar_tensor_tensor", out=ys_[:, b, hf * 512:(hf + 1) * 512], in0=pt[:], scalar=1.0, in1=bd_[:, hf * 512:(hf + 1) * 512],
                                   op0=ALU.mult, op1=ALU.add), reads=[pt, bd_], joins=[ys_])
            kb.dma("sp", d["ys"][rows, :].rearrange("(b p) f -> p b f", p=128), ys_[:], reads=[ys_], joins=[d["ys"]])

        load(0)
        transposes(0)
        for ex in range(NE):
            if ex + 1 < NE:
                load(ex + 1)
            up(ex)
            if ex + 1 < NE:
                transposes(ex + 1)
            down(ex)
        kb.end_phase()

    def phase_combine(self, l):
        kb, d = self.kb, self.d
        kb.begin_phase()
        rows = [Rot([kb.sb(f"cb_r{k}_{i}", [128, D], BF16) for i in range(2)]) for k in range(4)]
        for k in range(4):
            for t_ in rows[k].tiles:
                kb.op("dve", I("memset", t_[:], 0.0), reads=[], writes=[t_])
        yt = Rot([kb.sb(f"cb_y{i}", [128, 4, D], F32) for i in range(2)])
        for j in range(NT):
            ts = slice(j * TT, (j + 1) * TT)
            yt_ = yt.next()
            for b in range(4):
                gb = j * 4 + b
                rk = [rows[k].next() for k in range(4)]
                for k in range(4):
                    kb.idma(rk[k][:], None, d["ys"][:, :], bass.IndirectOffsetOnAxis(self.slots_all[:, gb, k:k + 1], 0), NSLOT - 1,
                            reads=[d["ys"], self.slots_all], writes=[rk[k]])
                kb.op("dve", I("tensor_scalar", yt_[:, b, :], rk[0][:], self.gk_all[:, gb, 0:1], None, op0=ALU.mult), reads=[rk[0], self.gk_all], joins=[yt_])
                for k in range(1, 4):
                    kb.op("dve", I("scalar_tensor_tensor", out=yt_[:, b, :], in0=rk[k][:], scalar=self.gk_all[:, gb, k:k + 1], in1=yt_[:, b, :], op0=ALU.mult, op1=ALU.add),
                          reads=[rk[k], self.gk_all, yt_], joins=[yt_])
            kb.dma("sp", d["y"][ts, :].rearrange("(b p) f -> p b f", p=128), yt_[:], reads=[yt_], joins=[d["y"]])
        kb.end_phase()

    def phase_ln2_ple(self, l, last):
        kb, d = self.kb, self.d
        kb.begin_phase()
        ps = Rot([kb.ps(f"lp_ps{i}", [128, 512]) for i in range(7)])
        wg = kb.sb("lp_wg", [128, KC, D], BF16)
        kb.dma("pool", wg[:], d["ple_w_gate"][l].rearrange("(c p) f -> p c f", p=128), reads=[d["ple_w_gate"]], writes=[wg])
        wp = kb.sb("lp_wp", [128, 2, D], BF16)
        kb.dma("pool", wp[:], d["ple_w_proj"][l].rearrange("(c p) f -> p c f", p=128), reads=[d["ple_w_proj"]], writes=[wp])
        yt = Rot([kb.sb(f"lp_yt{i}", [128, 4, D], F32) for i in range(2)])
        x1f = Rot([kb.sb(f"lp_x1f{i}", [128, KC, TT], F32) for i in range(1)])
        pin = Rot([kb.sb(f"lp_pin{i}", [128, 4, PLE], F32) for i in range(2)])
        pTb = kb.sb("lp_pTb", [128, 2, TT], BF16)
        pres = Rot([kb.sb(f"lp_pre{i}", [128, KC, TT], F32) for i in range(2)])
        sq = kb.sb("lp_sq", [128, KC, TT], F32)
        mean = kb.sb("lp_mean", [128, TT], F32)
        rstd = kb.sb("lp_rstd", [128, TT], F32)
        tmp2 = kb.sb("lp_tmp2", [128, TT], F32)
        x2f = kb.sb("lp_x2f", [128, KC, TT], F32)
        x2b = kb.sb("lp_x2b", [128, KC, TT], BF16)
        sgm = Rot([kb.sb(f"lp_sg{i}", [128, TT], F32) for i in range(2)])
        xo = Rot([kb.sb(f"lp_xo{i}", [128, KC, TT], F32) for i in range(1)])
        otok = Rot([kb.sb(f"lp_ot{i}", [128, 4, D], F32) for i in range(1)]) if last else None
        g2 = self.lncols[:, 2 * 32 + l * 8:2 * 32 + l * 8 + 8]
        b2 = self.lncols[:, 3 * 32 + l * 8:3 * 32 + l * 8 + 8]
        x1Tv = d["x1T"][:].rearrange("(c p) t -> p c t", p=128)
        xTv = d["xT"][:].rearrange("(c p) t -> p c t", p=128)
        for j in range(NT):
            ts = slice(j * TT, (j + 1) * TT)
            yt_, x1f_, pin_ = yt.next(), x1f.next(), pin.next()
            kb.dma("sp", yt_[:], d["y"][ts, :].rearrange("(b p) f -> p b f", p=128), reads=[d["y"]], writes=[yt_])
            kb.dma("sp", x1f_[:], x1Tv[:, :, ts], reads=[d["x1T"]], writes=[x1f_])
            kb.dma("sp", pin_[:], d["p"][l, ts, :].rearrange("(b p) f -> p b f", p=128), reads=[d["p"]], writes=[pin_])
            for c in range(2):
                pt = ps.next()
                fns = [I("transpose", pt[:, b * 128:(b + 1) * 128], pin_[:, b, c * 128:(c + 1) * 128], self.ident_f[:]) for b in range(4)]
                kb.op("pe", fns, reads=[pin_, self.ident_f], writes=[pt])
                self.copy(self.cp_eng(), pTb, pTb[:, c, :], pt, pt[:])
            pre = pres.next()
            for mc in range(KC):
                pt = ps.next()
                fns = [I("transpose", pt[:, b * 128:(b + 1) * 128], yt_[:, b, mc * 128:(mc + 1) * 128], self.ident_f[:]) for b in range(4)]
                kb.op("pe", fns, reads=[yt_, self.ident_f], writes=[pt])
                kb.op("dve", I("scalar_tensor_tensor", out=pre[:, mc, :], in0=x1f_[:, mc, :], scalar=ALPHA, in1=pt[:], op0=ALU.mult, op1=ALU.add),
                      reads=[x1f_, pt], joins=[pre])
            self.layernorm(pre, sq, ps, mean, rstd, tmp2, g2, b2, x2f, x2b)
            xo_ = xo.next()
            for mc in range(KC):
                pg = ps.next()
                self.mm(pg, pg[:], [(wg[:, kc, mc * 128:(mc + 1) * 128], x2b[:, kc, :]) for kc in range(KC)], reads=[wg, x2b])
                pp = ps.next()
                self.mm(pp, pp[:], [(wp[:, kc, mc * 128:(mc + 1) * 128], pTb[:, kc, :]) for kc in range(2)], reads=[wp, pTb])
                sg = sgm.next()
                kb.op("act", I("activation", out=sg[:], in_=pg[:], func=AF.Sigmoid), reads=[pg], writes=[sg])
                kb.op("dve", I("tensor_tensor", out=sg[:], in0=sg[:], in1=pp[:], op=ALU.mult), reads=[sg, pp], writes=[sg])
                kb.op("pool", I("tensor_tensor", out=xo_[:, mc, :], in0=x2f[:, mc, :], in1=sg[:], op=ALU.add), reads=[x2f, sg], joins=[xo_])
            if not last:
                kb.dma("sp", xTv[:, :, ts], xo_[:], reads=[xo_], joins=[d["xT"]])
            else:
                if "xT" in self.debug:
                    kb.dma("sp", xTv[:, :, ts], xo_[:], reads=[xo_], joins=[d["xT"]])
                ot = otok.next()
                for b in range(4):
                    for hf in range(2):
                        pt = ps.next()
                        fns = [I("transpose", pt[:, c4 * 128:(c4 + 1) * 128], xo_[:, hf * 4 + c4, b * 128:(b + 1) * 128], self.ident_f[:]) for c4 in range(4)]
                        kb.op("pe", fns, reads=[xo_, self.ident_f], writes=[pt])
                        self.copy(self.cp_eng(), ot, ot[:, b, hf * 512:(hf + 1) * 512], pt, pt[:])
                kb.dma("sp", d["out"][ts, :].rearrange("(b p) f -> p b f", p=128), ot[:], reads=[ot], joins=[d["out"]])
        kb.end_phase()


def make_consts():
    bf = ml_dtypes.bfloat16
    c = {}
    c["c_ident_f"] = np.eye(128, dtype=np.float32)
    c["c_ident_b"] = np.eye(128, dtype=np.float32).astype(bf)
    c["c_ones_f"] = np.ones((128, 128), np.float32)
    q = np.arange(128)[:, None]
    k = np.arange(128)[None, :]
    c["c_mask_b"] = np.where(k <= q, 0.0, MASK_NEG).astype(np.float32).astype(bf)
    rt = np.zeros((64, 128), np.float32)
    for m in range(32):
        rt[m + 32, m] = -1.0
        rt[m, m + 32] = 1.0
    c["c_rt_b"] = rt.astype(bf)
    c["c_u_b"] = (np.arange(128)[:, None] < np.arange(128)[None, :]).astype(np.float32).astype(bf)
    c["c_ones_b"] = np.ones((128, 128), np.float32).astype(bf)
    c["c_ebase"] = np.tile((np.arange(NE, dtype=np.float32) * CAP)[None, :], (128, 1)).astype(np.float32)
    half = 32
    invf = np.exp(-math.log(10000.0) * np.arange(half, dtype=np.float32) / half).astype(np.float32)
    c["c_invf"] = np.concatenate([invf, invf]).reshape(64, 1).astype(np.float32)
    return c


_PROG_CACHE = {}


def get_prog(n_layers=DEPTH, debug=()):
    key = (n_layers, tuple(sorted(debug)))
    if key not in _PROG_CACHE:
        _PROG_CACHE[key] = Prog(n_layers, debug)
    return _PROG_CACHE[key]


WEIGHT_NAMES = ["mla_w_in", "mla_q_norm", "mla_kv_norm", "mla_w_uq", "mla_w_ukv", "mla_w_o",
                "lru_w_in", "lru_conv_w", "lru_conv_b", "lru_w_a", "lru_b_a", "lru_w_x", "lru_b_x", "lru_lambda", "lru_w_out",
                "ln1_g", "ln1_b", "ln2_g", "ln2_b", "moe_w_router", "moe_b_router", "moe_w_up", "moe_b_up",
                "moe_w_down", "moe_b_down", "ple_w_gate", "ple_w_proj"]


def make_in_map(inputs, b, consts, prog, shared=None):
    m = {}
    for n in prog.used_inputs:
        shape = prog.in_shapes[n][0]
        if n == "x":
            m[n] = np.ascontiguousarray(np.asarray(inputs["x"])[b], dtype=np.float32)
        elif n == "p":
            m[n] = np.ascontiguousarray(np.asarray(inputs["p"])[:shape[0], b], dtype=np.float32)
        elif n == "positions":
            m[n] = np.ascontiguousarray(np.asarray(inputs["positions"])[b:b + 1], dtype=np.int32)
        elif n.startswith("c_"):
            m[n] = consts[n]
        else:
            if shared is not None and n in shared:
                m[n] = shared[n]
            else:
                a = np.ascontiguousarray(np.asarray(inputs[n])[:shape[0]], dtype=np.float32)
                if shared is not None:
                    shared[n] = a
                m[n] = a
    return m


def kernel(**inputs):
    prog = get_prog()
    consts = make_consts()
    nb = np.asarray(inputs["x"]).shape[0]
    shared = {}
    in_maps = [make_in_map(inputs, b, consts, prog, shared) for b in range(nb)]
    res = run_bass_kernel_spmd(prog.nc, in_maps, core_ids=list(range(nb)))
    return np.stack([np.asarray(r["out"], dtype=np.float32) for r in res.results], axis=0)
```
